# Optimizing a Trainium2 kernel written in Bass

```python
import math
import jax, jax.numpy as jnp
from jax import lax
import numpy as np

D_MODEL = 1024
BATCH = 8
SEQ = 4096
DEPTH = 1

EPS = 1e-6
HG_HEADS = 4
HG_DK = 128
HG_DV = 128
HG_CHUNK = 64
HG_W = HG_HEADS * HG_DK
HG_VW = HG_HEADS * HG_DV
ATT_HEADS = 8
ATT_DH = 64
ATT_W = ATT_HEADS * ATT_DH
IDX_HEADS = 4
IDX_DH = 64
TOPK_MAX = 256
Q_BLOCK = 128
REL_BUCKETS = 32
REL_MAX_DIST = 128
PEER_HEADS = 8
PEER_NKEYS = 128
PEER_EXPERTS = PEER_NKEYS * PEER_NKEYS
PEER_QDIM = 256
PEER_HALF = PEER_QDIM // 2
PEER_TOPK = 16
PEER_TOKEN_BLOCK = 128
IN_SIZES = (HG_W, HG_W, HG_VW, HG_VW, ATT_W, ATT_DH, ATT_DH, IDX_HEADS * IDX_DH, IDX_DH, IDX_HEADS, D_MODEL, D_MODEL)
IN_WIDTH = sum(IN_SIZES)

kernel_name = "hybrid_hgrn2_dsa_peer_block"


def rmsnorm(x, g):
    xf = x.astype(jnp.float32)
    y = xf * lax.rsqrt(jnp.mean(xf * xf, axis=-1, keepdims=True) + EPS)
    return (y * g.astype(jnp.float32)).astype(x.dtype)


def layernorm(x, g, b):
    xf = x.astype(jnp.float32)
    mu = jnp.mean(xf, axis=-1, keepdims=True)
    var = jnp.mean(jnp.square(xf - mu), axis=-1, keepdims=True)
    y = (xf - mu) * lax.rsqrt(var + EPS)
    return (y * g.astype(jnp.float32) + b.astype(jnp.float32)).astype(x.dtype)


def split_cols(p, sizes):
    offs = np.cumsum(np.array(sizes))[:-1].tolist()
    return jnp.split(p, offs, axis=-1)


def t5_bucket(dist):
    n = jnp.maximum(dist, 0)
    max_exact = REL_BUCKETS // 2
    nf = jnp.maximum(n, 1).astype(jnp.float32)
    large = max_exact + (jnp.log(nf / max_exact) / math.log(REL_MAX_DIST / max_exact)
                         * (REL_BUCKETS - max_exact)).astype(jnp.int32)
    large = jnp.minimum(large, REL_BUCKETS - 1)
    return jnp.where(n < max_exact, n, large)


def hgrn2_mixer(q_raw, f_raw, i_raw, og_raw, lb, gn):
    B, S, _ = q_raw.shape
    H, DK, DV, C = HG_HEADS, HG_DK, HG_DV, HG_CHUNK
    n = S // C
    f32 = jnp.float32
    q = jax.nn.silu(q_raw.astype(f32))
    fg = lb + (1.0 - lb) * jax.nn.sigmoid(f_raw.astype(f32))
    k = 1.0 - fg
    logf = jnp.log(fg)
    v = i_raw.astype(f32)

    def to_chunks(a, d):
        return a.reshape(B, n, C, H, d).transpose(1, 0, 3, 2, 4)

    qc, kc, lc, vc = to_chunks(q, DK), to_chunks(k, DK), to_chunks(logf, DK), to_chunks(v, DV)
    tri = jnp.tril(jnp.ones((C, C), dtype=bool))[None, None, :, :, None]

    def step(state, inp):
        qb, kb, lfb, vb = inp
        b = jnp.cumsum(lfb, axis=2)
        inter = jnp.einsum('bhtd,bhde->bhte', qb * jnp.exp(b), state)
        diff = b[:, :, :, None, :] - b[:, :, None, :, :]
        decay = jnp.where(tri, jnp.exp(jnp.where(tri, diff, 0.0)), 0.0)
        A = jnp.einsum('bhtd,bhsd,bhtsd->bhts', qb, kb, decay)
        intra = jnp.einsum('bhts,bhse->bhte', A, vb)
        b_last = b[:, :, -1:, :]
        new_state = (jnp.exp(b_last[:, :, 0, :])[..., None] * state
                     + jnp.einsum('bhsd,bhse->bhde', kb * jnp.exp(b_last - b), vb))
        return new_state, inter + intra

    s0 = jnp.zeros((B, H, DK, DV), f32)
    _, o = lax.scan(step, s0, (qc, kc, lc, vc))
    o = o.transpose(1, 0, 3, 2, 4).reshape(B, S, H, DV)
    o = o * lax.rsqrt(jnp.mean(o * o, axis=-1, keepdims=True) + EPS) * gn.astype(f32)
    o = o.reshape(B, S, H * DV) * jax.nn.silu(og_raw.astype(f32))
    return o.astype(q_raw.dtype)


def dsa_mixer(q_raw, k_raw, v_raw, iq_raw, ik_raw, iw_raw, ik_g, ik_b, rel_table):
    B, S, _ = q_raw.shape
    f32 = jnp.float32
    nblk = S // Q_BLOCK
    ktop = min(TOPK_MAX, S // 4)
    ik = layernorm(ik_raw, ik_g, ik_b).astype(f32)
    iw = iw_raw.astype(f32) * (IDX_HEADS ** -0.5)
    qs = q_raw.reshape(B, nblk, Q_BLOCK, ATT_HEADS, ATT_DH).transpose(1, 0, 2, 3, 4)
    iqs = iq_raw.reshape(B, nblk, Q_BLOCK, IDX_HEADS, IDX_DH).transpose(1, 0, 2, 3, 4)
    iws = iw.reshape(B, nblk, Q_BLOCK, IDX_HEADS).transpose(1, 0, 2, 3)
    spos = jnp.arange(S)
    gather = jax.vmap(lambda tab, idx: tab[idx])

    def block(args):
        bi, qb, iqb, iwb = args
        tpos = bi * Q_BLOCK + jnp.arange(Q_BLOCK)
        rel = jnp.einsum('bqjd,bsd->bqjs', iqb.astype(f32), ik) * (IDX_DH ** -0.5)
        score = jnp.einsum('bqj,bqjs->bqs', iwb, jax.nn.relu(rel))
        causal = spos[None, :] <= tpos[:, None]
        score = jnp.where(causal[None], score, -jnp.inf)
        top_score, idx = lax.top_k(score, ktop)
        valid = jnp.isfinite(top_score)
        kg = gather(k_raw, idx)
        vg = gather(v_raw, idx)
        bias = rel_table[t5_bucket(tpos[None, :, None] - idx)].astype(f32)
        logits = (jnp.einsum('bqhd,bqkd->bqhk', qb, kg).astype(f32) * (ATT_DH ** -0.5)
                  + bias.transpose(0, 1, 3, 2))
        logits = jnp.where(valid[:, :, None, :], logits, -jnp.inf)
        p = jax.nn.softmax(logits, axis=-1)
        return jnp.einsum('bqhk,bqkd->bqhd', p.astype(vg.dtype), vg)

    o = lax.map(block, (jnp.arange(nblk), qs, iqs, iws))
    return o.transpose(1, 0, 2, 3, 4).reshape(B, S, ATT_W)


def peer_ffn(x, w_q, sub_keys, u, v):
    B, S, D = x.shape
    TB, H, K = PEER_TOKEN_BLOCK, PEER_HEADS, PEER_TOPK
    xt = x.reshape(B * S // TB, TB, D)

    def block(xb):
        q = (xb @ w_q).reshape(TB, H, 2, PEER_HALF)
        s = jnp.einsum('thpc,phnc->thpn', q, sub_keys).astype(jnp.float32)
        s1, i1 = lax.top_k(s[:, :, 0], K)
        s2, i2 = lax.top_k(s[:, :, 1], K)
        cand = (s1[..., :, None] + s2[..., None, :]).reshape(TB, H, K * K)
        cidx = (i1[..., :, None] * PEER_NKEYS + i2[..., None, :]).reshape(TB, H, K * K)
        top_s, pos = lax.top_k(cand, K)
        eidx = jnp.take_along_axis(cidx, pos, axis=-1).reshape(TB, H * K)
        g = jax.nn.softmax(top_s, axis=-1).reshape(TB, H * K)
        ug = u[eidx]
        vg = v[eidx]
        h = jnp.einsum('tkd,td->tk', ug, xb)
        a = jax.nn.gelu(h, approximate=False) * g.astype(h.dtype)
        return jnp.einsum('tk,tkd->td', a, vg)

    return lax.map(block, xt).reshape(B, S, D)


def setup_inputs(seed: int = 0) -> dict:
    key = jax.random.key(seed)
    ks = jax.random.split(key, 20)
    f32 = jnp.float32
    nrm = lambda k, shape, scale: jax.random.normal(k, shape, f32) * scale
    return {
        "x": nrm(ks[0], (BATCH, SEQ, D_MODEL), 1.0),
        "norm_mix": 1.0 + nrm(ks[1], (DEPTH, D_MODEL), 0.02),
        "w_in": nrm(ks[2], (DEPTH, D_MODEL, IN_WIDTH), D_MODEL ** -0.5),
        "hg_lb": nrm(ks[3], (DEPTH + 1, HG_W), 0.1),
        "hg_norm": 1.0 + nrm(ks[4], (DEPTH, HG_HEADS, HG_DV), 0.02),
        "idx_k_norm_g": 1.0 + nrm(ks[5], (DEPTH, IDX_DH), 0.02),
        "idx_k_norm_b": nrm(ks[6], (DEPTH, IDX_DH), 0.01),
        "rel_bias": nrm(ks[7], (REL_BUCKETS, ATT_HEADS), 0.1),
        "w_up_a": nrm(ks[8], (DEPTH, HG_VW, D_MODEL), HG_VW ** -0.5),
        "w_up_b": nrm(ks[9], (DEPTH, ATT_W, D_MODEL), ATT_W ** -0.5),
        "w_out": nrm(ks[10], (DEPTH, D_MODEL, D_MODEL), D_MODEL ** -0.5),
        "norm_ffn": 1.0 + nrm(ks[11], (DEPTH, D_MODEL), 0.02),
        "peer_wq": nrm(ks[12], (DEPTH, D_MODEL, PEER_HEADS * PEER_QDIM), D_MODEL ** -0.5),
        "peer_keys": nrm(ks[13], (DEPTH, 2, PEER_HEADS, PEER_NKEYS, PEER_HALF), PEER_HALF ** -0.5),
        "peer_u": nrm(ks[14], (DEPTH, PEER_EXPERTS, D_MODEL), D_MODEL ** -0.5),
        "peer_v": nrm(ks[15], (DEPTH, PEER_EXPERTS, D_MODEL), PEER_HEADS ** -0.5),
        "norm_final": 1.0 + nrm(ks[16], (D_MODEL,), 0.02),
    }


def reference(x, norm_mix, w_in, hg_lb, hg_norm, idx_k_norm_g, idx_k_norm_b, rel_bias,
              w_up_a, w_up_b, w_out, norm_ffn, peer_wq, peer_keys, peer_u, peer_v, norm_final):
    lb_all = jnp.cumsum(jax.nn.softmax(hg_lb.astype(jnp.float32), axis=0), axis=0)
    for l in range(DEPTH):
        xn = rmsnorm(x, norm_mix[l])
        proj = xn @ w_in[l]
        (hq, hf, hi, hog, aq, ak, av, iq, ik, iw, ga, gb) = split_cols(proj, IN_SIZES)
        ya = hgrn2_mixer(hq, hf, hi, hog, lb_all[l], hg_norm[l])
        yb = dsa_mixer(aq, ak, av, iq, ik, iw, idx_k_norm_g[l], idx_k_norm_b[l], rel_bias)
        h = jax.nn.sigmoid(ga) * (ya @ w_up_a[l]) + jax.nn.sigmoid(gb) * (yb @ w_up_b[l])
        x = x + h @ w_out[l]
        x = x + peer_ffn(rmsnorm(x, norm_ffn[l]), peer_wq[l], peer_keys[l], peer_u[l], peer_v[l])
    return rmsnorm(x, norm_final)
```

```python
import math
from contextlib import ExitStack

import numpy as np
import ml_dtypes
import concourse.bass as bass
import concourse.mybir as mybir
from concourse.bass_utils import run_bass_kernel_spmd

F32 = mybir.dt.float32
BF16 = mybir.dt.bfloat16
I32 = mybir.dt.int32
U32 = mybir.dt.uint32
ALU = mybir.AluOpType
AF = mybir.ActivationFunctionType
AX = mybir.AxisListType

S = 4096
D = 1024
NBLK = 32
NCORES = 8
COL = dict(hq=0, hf=512, hi=1024, hog=1536, aq=2048, ak=2560, av=2624, iq=2688, ik=2944,
           iw=3008, ga=3012, gb=4036)
IN_WIDTH = 5060
EPS = 1e-6
NEG = -30000.0
NROUNDS = 16


class Sched:
    ENG = ("pe", "dve", "act", "pool", "sp")

    def __init__(self, nc):
        self.nc = nc
        self.q = {e: [] for e in self.ENG}
        self.cnt = {}
        self.seen = {e: {} for e in self.ENG}
        self.w = {}
        self.r = {}
        self.excl = set()

    def _split(self, reads, writes):
        reads = self._units(reads)
        writes = self._units(writes)
        ex = [u for u in reads if u[0] in self.excl]
        if ex:
            reads = [u for u in reads if u[0] not in self.excl]
            writes = writes + [u for u in ex if u not in writes]
        return reads, writes

    @staticmethod
    def _units(specs):
        out = []
        for s in specs:
            if isinstance(s, str):
                out.append((s, 0))
            elif len(s) == 2:
                out.append((s[0], s[1]))
            else:
                for i in range(s[1], s[2]):
                    out.append((s[0], i))
        return out

    def _deps(self, eng, reads, writes):
        deps = {}

        def add(ev, kind):
            if ev is None:
                return
            sem, val = ev
            if sem == "E:" + eng and eng == "pe":
                return
            if deps.get(sem, 0) < val:
                deps[sem] = val

        for u in reads:
            add(self.w.get(u), "raw")
        for u in writes:
            add(self.w.get(u), "waw")
            for sem, val in self.r.get(u, {}).items():
                add((sem, val), "war")
        waits = []
        seen = self.seen[eng]
        for sem, val in deps.items():
            if seen.get(sem, 0) < val:
                seen[sem] = val
                waits.append((sem, val))
        return waits

    def _register(self, ev, reads, writes):
        sem, val = ev
        for u in reads:
            d = self.r.setdefault(u, {})
            if d.get(sem, 0) < val:
                d[sem] = val
        for u in writes:
            self.w[u] = ev
            self.r[u] = {}

    def op(self, eng, fn, reads=(), writes=()):
        reads, writes = self._split(reads, writes)
        waits = self._deps(eng, reads, writes)
        sem = "E:" + eng
        self.cnt[sem] = self.cnt.get(sem, 0) + 1
        ev = (sem, self.cnt[sem])
        self.q[eng].append((fn, waits, (sem, 1)))
        self._register(ev, reads, writes)
        return ev

    def dma(self, queue, fn, sem, reads=(), writes=()):
        reads, writes = self._split(reads, writes)
        waits = self._deps(queue, reads, writes)
        sem = "D:" + sem
        prev = self.cnt.get(sem, 0)
        if prev and self.seen[queue].get(sem, 0) < prev:
            self.seen[queue][sem] = prev
            waits.append((sem, prev))
        self.cnt[sem] = self.cnt.get(sem, 0) + 16
        ev = (sem, self.cnt[sem])
        self.q[queue].append((fn, waits, (sem, 16)))
        self._register(ev, reads, writes)
        return ev

    def barrier(self, engs=None):
        for eng in (engs or self.ENG):
            waits = []
            for sem, val in self.cnt.items():
                if sem == "E:" + eng:
                    continue
                if self.seen[eng].get(sem, 0) < val:
                    self.seen[eng][sem] = val
                    waits.append((sem, val))
            if waits:
                self.q[eng].append((None, waits, None))

    def emit(self):
        nc = self.nc
        with ExitStack() as st:
            handles = {}
            for name in self.cnt:
                handles[name] = st.enter_context(nc.semaphore(name.replace(":", "_")))
            block = st.enter_context(nc.Block())
            engobjs = {"pe": block.tensor, "dve": block.vector, "act": block.scalar,
                       "pool": block.gpsimd, "sp": block.sync}

            def make(ename):
                lst = self.q[ename]

                def body(e):
                    for fn, waits, inc in lst:
                        for sem, val in waits:
                            e.wait_ge(handles[sem], val)
                        if fn is not None:
                            ins = fn(e)
                            ins.then_inc(handles[inc[0]], inc[1])
                return body

            for ename in self.ENG:
                if self.q[ename]:
                    engobjs[ename](make(ename))


class K:
    def __init__(self, nc, cfg):
        self.nc = nc
        self.cfg = cfg
        self.s = Sched(nc)
        self.uid = 0

    def mm(self, out, lhsT, rhs, start, stop, r, w):
        self.s.op("pe", lambda e: e.matmul(out, lhsT=lhsT, rhs=rhs, start=start, stop=stop), r, w)

    def tr(self, out, in_, ident, r, w):
        self.s.op("pe", lambda e: e.transpose(out=out, in_=in_, identity=ident), r, w)

    def act(self, out, in_, func, r, w, scale=1.0, bias=0.0, accum=None):
        if accum is None:
            self.s.op("act", lambda e: e.activation(out=out, in_=in_, func=func, bias=bias, scale=scale), r, w)
        else:
            self.s.op("act", lambda e: e.activation(out=out, in_=in_, func=func, bias=bias, scale=scale,
                                                    accum_out=accum), r, w)

    def tsc(self, eng, out, in0, s1, s2, op0, op1, r, w, accum=None):
        if op1 is None:
            self.s.op(eng, lambda e: e.tensor_scalar(out=out, in0=in0, scalar1=s1, scalar2=None, op0=op0), r, w)
        elif accum is None:
            self.s.op(eng, lambda e: e.tensor_scalar(out=out, in0=in0, scalar1=s1, scalar2=s2, op0=op0, op1=op1), r, w)
        else:
            self.s.op(eng, lambda e: e.tensor_scalar(out=out, in0=in0, scalar1=s1, scalar2=s2, op0=op0, op1=op1,
                                                     accum_out=accum), r, w)

    def stt(self, eng, out, in0, scalar, in1, op0, op1, r, w, accum=None):
        if accum is None:
            self.s.op(eng, lambda e: e.scalar_tensor_tensor(out=out, in0=in0, scalar=scalar, in1=in1, op0=op0, op1=op1), r, w)
        else:
            self.s.op(eng, lambda e: e.scalar_tensor_tensor(out=out, in0=in0, scalar=scalar, in1=in1, op0=op0, op1=op1,
                                                            accum_out=accum), r, w)

    def tt(self, eng, out, in0, in1, op, r, w):
        self.s.op(eng, lambda e: e.tensor_tensor(out=out, in0=in0, in1=in1, op=op), r, w)

    def tred(self, eng, out, in_, op, r, w):
        self.s.op(eng, lambda e: e.tensor_reduce(out=out, in_=in_, axis=AX.X, op=op), r, w)

    def cp(self, eng, out, in_, r, w):
        if eng == "act":
            self.s.op("act", lambda e: e.copy(out=out, in_=in_), r, w)
        else:
            self.s.op(eng, lambda e: e.tensor_copy(out=out, in_=in_), r, w)

    def recip(self, out, in_, r, w):
        self.s.op("dve", lambda e: e.reciprocal(out=out, in_=in_), r, w)

    def memset(self, eng, ap, val, w):
        self.s.op(eng, lambda e: e.memset(ap, val), (), w)

    def dma(self, q, out, in_, sem, r, w):
        self.s.dma(q, lambda e: e.dma_start(out=out, in_=in_), sem, r, w)

    def gen(self, eng, fn, r, w):
        self.s.op(eng, fn, r, w)


def t5_bucket_np(n):
    n = np.maximum(n, 0)
    nf = np.maximum(n, 1).astype(np.float32)
    large = 16 + (np.log(nf / np.float32(16)) / np.float32(math.log(128 / 16)) * np.float32(16)).astype(np.int32)
    large = np.minimum(large, 31)
    return np.where(n < 16, n, large)


def host_consts(rel_bias):
    c = {}
    c["c_ident"] = np.eye(128, dtype=np.float32)
    tl = np.arange(128)
    c["c_cmask"] = np.where(tl[None, :] <= tl[:, None], 0.0, -1e30).astype(np.float32)
    t64 = np.arange(64)
    c["c_tri"] = (t64[:, None] <= t64[None, :]).astype(np.float32)
    cm = np.ones((128, 512), np.float32)
    cm[:, ::64] = 0.0
    c["c_chunkmask"] = cm
    e65 = np.zeros((65, 64), np.float32)
    e65[64, :] = 1.0
    c["c_e65"] = e65
    sel = np.zeros((64, 256), np.float32)
    sel[np.arange(64), np.arange(64)] = 1.0
    sel[np.arange(64), 128 + 64 + np.arange(64)] = 1.0
    c["c_sel"] = sel
    c["c_iota16"] = np.tile(np.arange(16, dtype=np.float32)[None, :], (128, 1))
    sl = np.arange(128)[:, None]
    tt_ = np.arange(128)[None, :]
    bd = t5_bucket_np(tt_ - sl)
    bp = t5_bucket_np(128 + tt_ - sl)
    rb = np.asarray(rel_bias, np.float32)
    c["c_bd"] = np.ascontiguousarray(rb[bd].transpose(0, 2, 1))
    c["c_bp"] = np.ascontiguousarray(rb[bp].transpose(0, 2, 1))
    c["c_b31"] = np.ascontiguousarray(np.broadcast_to(rb[31][None, :, None], (128, 8, 128)))
    return c


CONST_SHAPES = {
    "c_ident": [128, 128], "c_cmask": [128, 128], "c_tri": [64, 64], "c_chunkmask": [128, 512],
    "c_e65": [65, 64], "c_sel": [64, 256], "c_iota16": [128, 16],
    "c_bd": [128, 8, 128], "c_bp": [128, 8, 128], "c_b31": [128, 8, 128],
}

INPUT_SHAPES = {
    "x": [S, D], "norm_mix": [1, D], "w_in": [D, IN_WIDTH], "hg_lb": [2, 512], "hg_norm": [1, 512],
    "idx_k_norm_g": [1, 64], "idx_k_norm_b": [1, 64], "w_up_a": [512, D], "w_up_b": [512, D],
    "w_out": [D, D], "norm_ffn": [1, D], "peer_wq": [D, 2048], "peer_keys": [16, 128, 128],
    "peer_u": [16384, D], "peer_v": [16384, D], "norm_final": [1, D],
}

SCRATCH = {
    "ya_s": ([128, 4, S], BF16), "yb_s": ([128, 4, S], BF16), "hT_s": ([128, 8, S], BF16),
    "uv_s": ([16384, 2048], BF16),
}


def build(cfg=None):
    cfg = cfg or {}
    phases = cfg.get("phases", ["A", "BC", "D", "E1", "E2"])
    inject = cfg.get("inject", [])
    taps = cfg.get("taps", [])
    nc = bass.Bass("TRN2", target_bir_lowering=False)
    k = K(nc, cfg)
    s = k.s
    T = {}
    for name, shp in INPUT_SHAPES.items():
        T[name] = nc.dram_tensor(name, shp, F32, kind="ExternalInput").ap()
    for name, shp in CONST_SHAPES.items():
        T[name] = nc.dram_tensor(name, shp, F32, kind="ExternalInput").ap()
    for name, (shp, dt) in SCRATCH.items():
        kind = "ExternalInput" if name in inject else ("ExternalOutput" if name in taps else "Internal")
        T[name] = nc.dram_tensor(name, shp, dt, kind=kind).ap()
    T["out"] = nc.dram_tensor("out", [S, D], F32, kind="ExternalOutput").ap()

    with ExitStack() as top, nc.allow_low_precision("bf16 matmul operands, fp32 accumulation"), \
            nc.allow_non_contiguous_dma("small strided constant loads"):
        def sbt(st, name, shape, dt):
            return st.enter_context(nc.sbuf_tensor(name, shape, dt))

        def pst(st, name, shape, dt):
            s.excl.add(name)
            return st.enter_context(nc.psum_tensor(name, shape, dt))

        identf = sbt(top, "identf", [128, 128], F32)
        identb = sbt(top, "identb", [128, 128], BF16)
        wst = sbt(top, "wst", [128, 2, 8, 128], F32)
        k.dma("sp", identf[:], T["c_ident"], "c0", [], ["identf"])
        k.cp("dve", identb[:], identf[:], ["identf"], ["identb"])
        wstate = {"n": 0}

        def load_w(dst, dst_key, src, C, n):
            for c0 in range(0, n, 128):
                wdt = min(128, n - c0)
                b = wstate["n"] % 2
                wstate["n"] += 1
                k.dma("sp", wst[:, b, 0:C, 0:wdt], src[:, c0:c0 + wdt].rearrange("(c p) n -> p c n", p=128),
                      "wst%d" % b, [], [("wst", b)])
                k.cp("pool", dst[:, :, c0:c0 + wdt], wst[:, b, 0:C, 0:wdt], [("wst", b)], [dst_key])

        with ExitStack() as mid:
            xnT = sbt(mid, "xnT", [128, 8, S], BF16)
            if "A" in phases:
                phase_A(k, T, sbt, pst, xnT, identb)
            if "BC" in phases:
                phase_BC(k, T, sbt, pst, xnT, identb, identf, load_w)
            if "D" in phases:
                phase_D(k, T, sbt, pst, xnT, identb, identf, load_w)
            if "E1" in phases:
                phase_E1(k, T, sbt, pst, xnT, load_w)
            s.barrier()
        if "E2" in phases:
            phase_E2(k, T, sbt, pst, identb, identf, load_w)
        s.barrier()
        s.emit()
    return nc


def phase_A(k, T, sbt, pst, xnT, identb):
    nc, s = k.nc, k.s
    with ExitStack() as st:
        xt = sbt(st, "A_xt", [128, 2, D], F32)
        gb = sbt(st, "A_gb", [128, D], F32)
        junk = sbt(st, "A_junk", [128, D], F32)
        ss = sbt(st, "A_ss", [128, 2], F32)
        rstd = sbt(st, "A_rstd", [128, 2], F32)
        xs = sbt(st, "A_xs", [128, 2, D], BF16)
        pt = pst(st, "A_pt", [128, 2, 8, 128], BF16)
        k.dma("sp", gb[:], T["norm_mix"].partition_broadcast(128), "c0", [], ["A_gb"])
        for b in range(k.cfg.get("nblk_a", NBLK)):
            p = b % 2
            k.dma("sp", xt[:, p, :], T["x"][b * 128:(b + 1) * 128, :], "A_x%d" % p, [], [("A_xt", p)])
            k.act(junk[:], xt[:, p, :], AF.Square, [("A_xt", p)], ["A_junk", ("A_ss", p)], accum=ss[:, p:p + 1])
            k.act(rstd[:, p:p + 1], ss[:, p:p + 1], AF.Ln, [("A_ss", p)], [("A_rstd", p)], scale=1.0 / D, bias=EPS)
            k.act(rstd[:, p:p + 1], rstd[:, p:p + 1], AF.Exp, [("A_rstd", p)], [("A_rstd", p)], scale=-0.5)
            k.stt("dve", xs[:, p, :], xt[:, p, :], rstd[:, p:p + 1], gb[:], ALU.mult, ALU.mult,
                  [("A_xt", p), ("A_rstd", p), "A_gb"], [("A_xs", p)])
            for c in range(8):
                k.tr(pt[:, p, c, :], xs[:, p, c * 128:(c + 1) * 128], identb[:], [("A_xs", p), "identb"], [("A_pt", p)])
            k.cp("act", xnT[:, :, b * 128:(b + 1) * 128], pt[:, p, :, :], [("A_pt", p)], [("xnT", b)])
        s.barrier()


NCV = 4


def convert_step(k, T, cvf, cvb, r):
    def src_of(q):
        tile_, which = q // 2, q % 2
        rs = slice(tile_ * 128, (tile_ + 1) * 128)
        return (T["peer_u"] if which == 0 else T["peer_v"])[rs, :], T["uv_s"][rs, which * 1024:(which + 1) * 1024]
    if r < 256:
        fb = r % NCV
        k.dma("sp", cvf[:, fb, :], src_of(r)[0], "cvf%d" % fb, [], [("cvf", fb)])
    q = r - (NCV - 1)
    if 0 <= q < 256:
        fb, cb = q % NCV, q % 2
        k.cp("act", cvb[:, cb, :], cvf[:, fb, :], [("cvf", fb)], [("cvb", cb)])
        k.dma("sp", src_of(q)[1], cvb[:, cb, :], "cvst%d" % cb, [("cvb", cb)], ["uv_s"])


def phase_BC(k, T, sbt, pst, xnT, identb, identf, load_w):
    nc, s = k.nc, k.s
    with ExitStack() as st:
        wB = sbt(st, "wB", [128, 8, 452], BF16)
        wC = sbt(st, "wC", [128, 8, 768], BF16)
        kT = sbt(st, "kT", [64, S], BF16)
        iknT = sbt(st, "iknT", [64, S], BF16)
        v_aug = sbt(st, "v_aug", [128, NBLK, 65], BF16)
        iws = sbt(st, "iws", [128, NBLK, 4], F32)
        gI = sbt(st, "gI", [128, 64], F32)
        bI = sbt(st, "bI", [128, 64], F32)
        cmask = sbt(st, "cmask", [128, 128], F32)
        e65 = sbt(st, "e65", [65, 64], F32)
        self_f = sbt(st, "sel_f", [64, 256], F32)
        sel_b = sbt(st, "sel_b", [64, 256], BF16)
        i8 = sbt(st, "i8", [128, 8, 128], BF16)
        bnd = sbt(st, "bnd", [128, 2, 8, 128], BF16)
        ikf = sbt(st, "ikf", [128, 64], F32)
        ikb = sbt(st, "ikb", [128, 64], BF16)
        stats = sbt(st, "stats", [128, 6], F32)
        mv = sbt(st, "mv", [128, 2], F32)
        irs = sbt(st, "irs", [128, 1], F32)
        qT = sbt(st, "qT", [64, 2, 8, 128], BF16)
        iqT = sbt(st, "iqT", [64, 2, 4, 128], BF16)
        score = sbt(st, "score", [128, S], F32)
        rl = sbt(st, "rl", [128, 2, 4, 256], F32)
        selneg = sbt(st, "selneg", [128, 2, S], BF16)
        cj = sbt(st, "cj", [128, S], BF16)
        lo = sbt(st, "lo", [128, 1], F32)
        hi = sbt(st, "hi", [128, 1], F32)
        mid = sbt(st, "mid", [128, 1], F32)
        cnt = sbt(st, "cnt", [128, 1], F32)
        wtab = sbt(st, "wtab", [128, NROUNDS + 2], F32)
        pw2 = sbt(st, "pw2", [128, NROUNDS + 2], F32)
        pT = sbt(st, "pT", [128, 2, 2, 512], BF16)
        oT = sbt(st, "oT", [65, 2, 512], F32)
        rb = sbt(st, "rb", [64, 2, 512], F32)
        ybn = sbt(st, "ybn", [64, 8, 128], BF16)
        ybo = sbt(st, "ybo", [128, 4, 128], BF16)
        pX = pst(st, "pX", [128, 2, 512], F32)
        pL = pst(st, "pL", [128, 2, 2, 512], F32)
        pO = pst(st, "pO", [128, 2, 512], F32)

        load_w(wB[:], "wB", T["w_in"][:, COL["ak"]:COL["ak"] + 452], 8, 452)
        load_w(wC[:, :, 0:512], "wC", T["w_in"][:, COL["aq"]:COL["aq"] + 512], 8, 512)
        load_w(wC[:, :, 512:768], "wC", T["w_in"][:, COL["iq"]:COL["iq"] + 256], 8, 256)
        k.dma("sp", gI[:], T["idx_k_norm_g"].partition_broadcast(128), "c0", [], ["gI"])
        k.dma("sp", bI[:], T["idx_k_norm_b"].partition_broadcast(128), "c0", [], ["bI"])
        k.dma("sp", cmask[:], T["c_cmask"], "c0", [], ["cmask"])
        k.dma("sp", e65[:], T["c_e65"], "c0", [], ["e65"])
        k.dma("sp", self_f[:], T["c_sel"], "c0", [], ["sel_f"])
        k.cp("dve", sel_b[:], self_f[:], ["sel_f"], ["sel_b"])
        for h in range(8):
            k.cp("dve", i8[:, h, :], identb[:], ["identb"], ["i8"])
        with ExitStack() as st2:
            bstage = sbt(st2, "bstage", [128, 3, 8, 128], F32)
            k.dma("sp", bstage[:, 0], T["c_bd"], "c0", [], ["bstage"])
            k.dma("sp", bstage[:, 1], T["c_bp"], "c0", [], ["bstage"])
            k.dma("sp", bstage[:, 2], T["c_b31"], "c0", [], ["bstage"])
            for j in range(2):
                k.tt("dve", bstage[:, j], bstage[:, j], bstage[:, 2], ALU.subtract, ["bstage"], ["bstage"])
                k.tsc("dve", bnd[:, j], bstage[:, j], 8.0, None, ALU.mult, None, ["bstage"], ["bnd"])
            s.barrier()
        k.memset("dve", v_aug[:, :, 64:65], 1.0, ["v_aug"])
        for r_ in range(NROUNDS + 2):
            k.memset("dve", pw2[:, r_:r_ + 1], 2.0 ** (-r_), ["pw2"])

        stage = k.cfg.get("bc_stage", 9)
        for b in range(k.cfg.get("nblk_b", NBLK) if stage >= 1 else 0):
            tb = slice(b * 128, (b + 1) * 128)
            for c in range(8):
                k.mm(pX[:, 0, 0:452], xnT[:, c, tb], wB[:, c, :], c == 0, c == 7, [("xnT", b), "wB"], [("pX", 0)])
            bv = k.cfg.get("b_var", 9)
            k.cp("act", v_aug[:, b, 0:64], pX[:, 0, 64:128], [("pX", 0)], ["v_aug"])
            if bv >= 2:
                k.cp("act", ikf[:], pX[:, 0, 384:448], [("pX", 0)], ["ikf"])
                k.tsc("dve", iws[:, b, :], pX[:, 0, 448:452], 0.0625, None, ALU.mult, None, [("pX", 0)], ["iws"])
            if bv >= 3:
                k.gen("dve", lambda e: e.bn_stats(out=stats[:], in_=ikf[:]), ["ikf"], ["stats"])
                k.gen("dve", lambda e: e.bn_aggr(out=mv[:], in_=stats[:]), ["stats"], ["mv"])
            if bv >= 4:
                k.act(irs[:], mv[:, 1:2], AF.Ln, ["mv"], ["irs"], bias=EPS)
                k.act(irs[:], irs[:], AF.Exp, ["irs"], ["irs"], scale=-0.5)
                k.tsc("dve", ikf[:], ikf[:], mv[:, 0:1], None, ALU.subtract, None, ["ikf", "mv"], ["ikf"])
                k.tsc("dve", ikf[:], ikf[:], irs[:, 0:1], None, ALU.mult, None, ["ikf", "irs"], ["ikf"])
                k.tt("dve", ikf[:], ikf[:], gI[:], ALU.mult, ["ikf", "gI"], ["ikf"])
                k.tt("dve", ikf[:], ikf[:], bI[:], ALU.add, ["ikf", "bI"], ["ikf"])
            if bv >= 5:
                ptk = pX[0:64, 1, 0:128]
                k.tr(ptk, ikf[:], identf[:], ["ikf", "identf"], [("pX", 1)])
                k.cp("act", iknT[:, tb], ptk, [("pX", 1)], [("iknT", b)])
        for c4 in range(8 if stage >= 2 else 0):
            ts_ = slice(c4 * 512, (c4 + 1) * 512)
            for c in range(8):
                k.mm(pX[0:64, 0, :], wB[:, c, 0:64], xnT[:, c, ts_], c == 0, c == 7,
                     [("xnT", 4 * c4, 4 * c4 + 4), "wB"], [("pX", 0)])
            k.cp("act", kT[:, ts_], pX[0:64, 0, :], [("pX", 0)], [("kT", 4 * c4, 4 * c4 + 4)])

        def c123(i):
            p = i % 2
            tb = slice(i * 128, (i + 1) * 128)
            L = (i + 1) * 128
            pq = pX[0:64, :, :].rearrange("p a (h t) -> p (a h) t", t=128)
            for h in range(8):
                for c in range(8):
                    k.mm(pq[:, h, :], wC[:, c, h * 64:(h + 1) * 64], xnT[:, c, tb], c == 0, c == 7,
                         [("xnT", i), "wC"], [("pX", h // 4)])
            k.cp("act", qT[:, p], pq, [("pX", 0), ("pX", 1)], [("qT", p)])
            for h in range(4):
                for c in range(8):
                    k.mm(pq[:, h, :], wC[:, c, 512 + h * 64:512 + (h + 1) * 64], xnT[:, c, tb], c == 0, c == 7,
                         [("xnT", i), "wC"], [("pX", 0)])
            k.cp("act", iqT[:, p], pq[:, 0:4, :], [("pX", 0)], [("iqT", p)])
            ps4 = pX[:].rearrange("p a (j w) -> p (a j) w", w=256)
            nch = (L + 255) // 256
            for ch in range(nch):
                s0 = ch * 256
                wk = min(256, L - s0)
                rp = ch % 2
                for j in range(4):
                    k.mm(ps4[:, j, 0:wk], iqT[:, p, j, :], iknT[:, s0:s0 + wk], True, True,
                         [("iqT", p), ("iknT", s0 // 128, (s0 + wk) // 128)], [("pX", j // 2)])
                k.act(rl[:, rp, :, 0:wk], ps4[:, :, 0:wk], AF.Relu, [("pX", 0), ("pX", 1)], [("rl", rp)])
                k.tsc("dve", score[:, s0:s0 + wk], rl[:, rp, 0, 0:wk], iws[:, i, 0:1], None, ALU.mult, None,
                      [("rl", rp), "iws"], ["score"])
                for j in range(1, 4):
                    k.stt("dve", score[:, s0:s0 + wk], rl[:, rp, j, 0:wk], iws[:, i, j:j + 1], score[:, s0:s0 + wk],
                          ALU.mult, ALU.add, [("rl", rp), "iws", "score"], ["score"])
            k.tred("dve", hi[:], score[:, 0:L], ALU.max, ["score"], ["hi"])
            k.tred("dve", lo[:], score[:, 0:L], ALU.min, ["score"], ["lo"])
            k.tsc("dve", lo[:], lo[:], -1.0, None, ALU.add, None, ["lo"], ["lo"])
            k.tt("dve", score[:, L - 128:L], score[:, L - 128:L], cmask[:], ALU.add, ["score", "cmask"], ["score"])
            if L > 256:
                k.tsc("dve", mid[:], lo[:], hi[:, 0:1], 0.5, ALU.add, ALU.mult, ["lo", "hi"], ["mid"])
                k.tsc("dve", hi[:], hi[:], lo[:, 0:1], 0.5, ALU.subtract, ALU.mult, ["lo", "hi"], ["hi"])
                k.tsc("dve", wtab[:], pw2[:], hi[:, 0:1], None, ALU.mult, None, ["pw2", "hi"], ["wtab"])
                for r_ in range(NROUNDS):
                    k.tsc("dve", cj[:, 0:L], score[:, 0:L], mid[:, 0:1], None, ALU.is_gt, ALU.add,
                          ["score", "mid"], ["cj", "cnt"], accum=cnt[:])
                    k.tsc("dve", cnt[:], cnt[:], 255.5, wtab[:, r_:r_ + 1], ALU.is_gt, ALU.mult, ["cnt", "wtab"], ["cnt"])
                    k.stt("dve", mid[:], mid[:], wtab[:, r_ + 1:r_ + 2], cnt[:], ALU.subtract, ALU.add,
                          ["mid", "wtab", "cnt"], ["mid"])
                k.tsc("dve", lo[:], mid[:], wtab[:, NROUNDS:NROUNDS + 1], None, ALU.subtract, None, ["mid", "wtab"], ["lo"])
            k.tsc("dve", selneg[:, p, 0:L], score[:, 0:L], lo[:, 0:1], NEG, ALU.is_le, ALU.mult,
                  ["score", "lo"], [("selneg", p)])

        def c4(i):
            p = i % 2

            def qk(j):
                lb_ = j % 2
                sj = slice(j * 128, (j + 1) * 128)
                near = (j == i) or (j == i - 1)
                for half in range(2):
                    hs = slice(4 * half, 4 * half + 4)
                    k.mm(pL[:, lb_, half, :], kT[:, sj], qT[:, p, hs, :], True, False,
                         [("kT", j), ("qT", p)], [("pL", lb_)])
                    k.mm(pL[:, lb_, half, :], selneg[:, p, sj], i8[:, hs, :], False, not near,
                         [("selneg", p), "i8"], [("pL", lb_)])
                    if near:
                        k.mm(pL[:, lb_, half, :], identb[:], bnd[:, 0 if j == i else 1, hs, :], False, True,
                             ["identb", "bnd"], [("pL", lb_)])

            qk(0)
            for j in range(i + 1):
                lb_ = j % 2
                if j + 1 <= i:
                    qk(j + 1)
                k.act(pT[:, lb_, :, :], pL[:, lb_, :, :], AF.Exp, [("pL", lb_)], [("pT", lb_)], scale=0.125)
                for half in range(2):
                    k.mm(pO[0:65, half, :], v_aug[:, j, :], pT[:, lb_, half, :], j == 0, j == i,
                         ["v_aug", ("pT", lb_)], ["pO"])
            k.cp("act", oT[:], pO[0:65, :, :], ["pO"], ["oT"])
            for half in range(2):
                k.mm(pX[0:64, half, :], e65[:], oT[:, half, :], True, True,
                     ["e65", "oT"], [("pX", half)])
            k.act(rb[:], pX[0:64, :, :], AF.Ln, [("pX", 0), ("pX", 1)], ["rb"])
            k.act(rb[:], rb[:], AF.Exp, ["rb"], ["rb"], scale=-1.0)
            k.tt("dve", ybn[:].rearrange("p (a h) t -> p a (h t)", a=2), oT[0:64, :, :], rb[:], ALU.mult, ["oT", "rb"], ["ybn"])
            yv = ybn[:].rearrange("p (c two) t -> p two c t", two=2)
            k.mm(pX[:, 0, :], sel_b[:, 0:128], yv[:, 0], True, False, ["sel_b", "ybn"], [("pX", 0)])
            k.mm(pX[:, 0, :], sel_b[:, 128:256], yv[:, 1], False, True, ["sel_b", "ybn"], [("pX", 0)])
            k.cp("act", ybo[:], pX[:, 0, :], [("pX", 0)], ["ybo"])
            k.dma("sp", T["yb_s"][:, :, i * 128:(i + 1) * 128], ybo[:], "ybo", ["ybo"], [])

        nblk = k.cfg.get("nblk_c", NBLK)
        if stage >= 3:
            c123(0)
        for i in range(nblk if stage >= 3 else 0):
            if i + 1 < nblk:
                c123(i + 1)
            if stage >= 4:
                c4(i)
        s.barrier()


def phase_D(k, T, sbt, pst, xnT, identb, identf, load_w):
    nc, s = k.nc, k.s
    NG = k.cfg.get("ngroups_d", 8)
    with ExitStack() as st:
        wD = sbt(st, "wD", [128, 8, 2048], BF16)
        lbs = sbt(st, "lbs", [128, 2, 4], F32)
        lbT = sbt(st, "lbT", [128, 4], F32)
        omlT = sbt(st, "omlT", [128, 4], F32)
        gnT = sbt(st, "gnT", [128, 4], F32)
        tri = sbt(st, "tri", [64, 64], F32)
        cmk = sbt(st, "cmk", [128, 512], F32)
        ones_b = sbt(st, "ones_b", [128, 128], BF16)
        v_sb = sbt(st, "v_sb", [64, 2, 8, 512], BF16)
        state_f = sbt(st, "state_f", [128, 4, 128], F32)
        state_b = sbt(st, "state_b", [128, 4, 128], BF16)
        NT = 10
        tf = sbt(st, "tf", [128, 2, NT, 512], F32)
        qeT = sbt(st, "qeT", [128, 2, 512], BF16)
        keT = sbt(st, "keT", [128, 2, 512], BF16)
        k2T = sbt(st, "k2T", [128, 2, 512], F32)
        ebl = sbt(st, "ebl", [128, 2, 8], F32)
        at_sb = sbt(st, "at_sb", [64, 2, 64], BF16)
        k2_sb = sbt(st, "k2_sb", [64, 2, 128], BF16)
        sq = sbt(st, "sq", [128, 512], BF16)
        yo = sbt(st, "yo", [128, 2, 512], BF16)
        cvf = sbt(st, "cvf", [128, NCV, 1024], F32)
        cvb = sbt(st, "cvb", [128, 2, 1024], BF16)
        cstep = {"r": 0}
        pP = pst(st, "pP", [128, 3, 512], F32)
        pOo = pst(st, "pOo", [128, 2, 512], F32)
        pM = pst(st, "pM", [128, 2, 512], F32)
        pV = pst(st, "pV", [128, 512], F32)

        for nm, c0 in (("hq", 0), ("hf", 512), ("hi", 1024), ("hog", 1536)):
            load_w(wD[:, :, c0:c0 + 512], "wD", T["w_in"][:, COL[nm]:COL[nm] + 512], 8, 512)
        k.dma("sp", lbs[:], T["hg_lb"].rearrange("r (h p) -> p r h", p=128), "c0", [], ["lbs"])
        k.dma("sp", gnT[:], T["hg_norm"].rearrange("o (h p) -> p (o h)", p=128), "c0", [], ["gnT"])
        k.dma("sp", tri[:], T["c_tri"], "c0", [], ["tri"])
        k.dma("sp", cmk[:], T["c_chunkmask"], "c0", [], ["cmk"])
        k.memset("dve", ones_b[:], 1.0, ["ones_b"])
        k.memset("dve", state_f[:], 0.0, [("state_f", 0, 4)])
        k.memset("dve", state_b[:], 0.0, [("state_b", 0, 4)])
        k.tt("dve", lbT[:], lbs[:, 1, :], lbs[:, 0, :], ALU.subtract, ["lbs"], ["lbT"])
        k.act(lbT[:], lbT[:], AF.Exp, ["lbT"], ["lbT"])
        k.tsc("dve", lbT[:], lbT[:], 1.0, None, ALU.add, None, ["lbT"], ["lbT"])
        k.recip(lbT[:], lbT[:], ["lbT"], ["lbT"])
        k.tsc("dve", omlT[:], lbT[:], -1.0, 1.0, ALU.mult, ALU.add, ["lbT"], ["omlT"])

        def vproj(g):
            gp = g % 2
            xk = ("xnT", 4 * g, 4 * g + 4)
            for c in range(8):
                tok = slice(g * 512 + c * 64, g * 512 + (c + 1) * 64)
                for kc in range(8):
                    k.mm(pV[0:64, :], xnT[:, kc, tok], wD[:, kc, 1024:1536], kc == 0, kc == 7, [xk, "wD"], ["pV"])
                k.cp("act", v_sb[:, gp, c, :], pV[0:64, :], ["pV"], [("v_sb", gp)])

        def prologue(g, h):
            g5 = slice(g * 512, (g + 1) * 512)
            xk = ("xnT", 4 * g, 4 * g + 4)
            u = (g * 4 + h) % 2
            t = lambda i, u=u: tf[:, u, i, :]
            tk = lambda i, u=u: ("tf", u * NT + i)
            for j, c0 in enumerate((0, 512, 1536)):
                for kc in range(8):
                    k.mm(pP[:, j, :], wD[:, kc, c0 + h * 128:c0 + (h + 1) * 128], xnT[:, kc, g5], kc == 0, kc == 7,
                         [xk, "wD"], [("pP", j)])
                    yield
            k.act(t(0), pP[:, 1, :], AF.Exp, [("pP", 1)], [tk(0)], scale=-1.0)
            yield
            k.act(t(0), t(0), AF.Ln, [tk(0)], [tk(0)], bias=1.0)
            yield
            k.act(t(0), t(0), AF.Exp, [tk(0)], [tk(0)], scale=-1.0)
            yield
            k.tsc("dve", t(0), t(0), omlT[:, h:h + 1], lbT[:, h:h + 1], ALU.mult, ALU.add,
                  [tk(0), "omlT", "lbT"], [tk(0)])
            yield
            k.act(t(1), t(0), AF.Ln, [tk(0)], [tk(1)])
            yield
            k.tsc("dve", t(2), t(0), -1.0, 1.0, ALU.mult, ALU.add, [tk(0)], [tk(2)])
            yield
            k.gen("dve", lambda e, o=t(3), d0=cmk[:], d1=t(1): e.tensor_tensor_scan(
                out=o, data0=d0, data1=d1, initial=0.0, op0=ALU.mult, op1=ALU.add),
                ["cmk", tk(1)], [tk(3)])
            yield
            k.act(t(4), t(3), AF.Exp, [tk(3)], [tk(4)])
            yield
            k.act(t(5), t(3), AF.Exp, [tk(3)], [tk(5)], scale=-1.0)
            yield
            for c in range(8):
                cs = slice(c * 64, (c + 1) * 64)
                k.act(tf[:, u, 6, cs], tf[:, u, 3, cs], AF.Exp, [tk(3)], [tk(6)], scale=-1.0,
                      bias=tf[:, u, 3, c * 64 + 63:c * 64 + 64])
                yield
            bl = tf[:, u, 3, :].rearrange("p (c t) -> p c t", t=64)[:, :, 63]
            k.act(ebl[:, u, :], bl, AF.Exp, [tk(3)], [("ebl", u)])
            yield
            k.tt("dve", k2T[:, u, :], t(2), t(6), ALU.mult, [tk(2), tk(6)], [("k2T", u)])
            yield
            k.tt("dve", keT[:, u, :], t(2), t(5), ALU.mult, [tk(2), tk(5)], [("keT", u)])
            yield
            k.act(t(7), pP[:, 0, :], AF.Exp, [("pP", 0)], [tk(7)], scale=-1.0)
            yield
            k.act(t(7), t(7), AF.Ln, [tk(7)], [tk(7)], bias=1.0)
            yield
            k.act(t(7), t(7), AF.Exp, [tk(7)], [tk(7)], scale=-1.0)
            yield
            k.tt("dve", t(7), t(7), pP[:, 0, :], ALU.mult, [tk(7), ("pP", 0)], [tk(7)])
            yield
            k.tt("dve", qeT[:, u, :], t(7), t(4), ALU.mult, [tk(7), tk(4)], [("qeT", u)])
            yield
            k.act(t(8), pP[:, 2, :], AF.Exp, [("pP", 2)], [tk(8)], scale=-1.0)
            yield
            k.act(t(8), t(8), AF.Ln, [tk(8)], [tk(8)], bias=1.0)
            yield
            k.act(t(8), t(8), AF.Exp, [tk(8)], [tk(8)], scale=-1.0)
            yield
            k.tt("dve", t(8), t(8), pP[:, 2, :], ALU.mult, [tk(8), ("pP", 2)], [tk(8)])

        def chunks(g, h, pg=None, qg=None):
            def pull(n=1):
                if qg is not None:
                    next(qg, None)
                if pg is not None:
                    for _ in range(n):
                        next(pg, None)
            gp = g % 2
            u = (g * 4 + h) % 2
            hs = slice(h * 128, (h + 1) * 128)
            for c in range(8):
                cs = slice(c * 64, (c + 1) * 64)
                a2 = c % 2
                k.mm(pM[0:64, 0, 0:64], keT[:, u, cs], qeT[:, u, cs], True, True,
                     [("keT", u), ("qeT", u)], [("pM", 0)])
                k.tt("dve", at_sb[:, a2, :], pM[0:64, 0, 0:64], tri[:], ALU.mult, [("pM", 0), "tri"], [("at_sb", a2)])
                pull(2)
                k.mm(pOo[:, u, cs], state_b[:, h, :], qeT[:, u, cs], True, False,
                     [("state_b", h), ("qeT", u)], [("pOo", u)])
                k.mm(pOo[:, u, cs], v_sb[:, gp, c, hs], at_sb[:, a2, :], False, True,
                     [("v_sb", gp), ("at_sb", a2)], [("pOo", u)])
                k.tr(pM[0:64, 0, 128:256], k2T[:, u, cs], identf[:], [("k2T", u), "identf"], [("pM", 0)])
                k.cp("act", k2_sb[:, a2, :], pM[0:64, 0, 128:256], [("pM", 0)], [("k2_sb", a2)])
                pull(2)
                k.mm(pM[:, 1, 0:128], k2_sb[:, a2, :], v_sb[:, gp, c, hs], True, True,
                     [("k2_sb", a2), ("v_sb", gp)], [("pM", 1)])
                k.stt("dve", state_f[:, h, :], state_f[:, h, :], ebl[:, u, c:c + 1], pM[:, 1, 0:128],
                      ALU.mult, ALU.add, [("state_f", h), ("ebl", u), ("pM", 1)], [("state_f", h)])
                k.cp("act", state_b[:, h, :], state_f[:, h, :], [("state_f", h)], [("state_b", h)])
                pull(2)
                if k.cfg.get("convert", True) and NG == 8:
                    convert_step(k, T, cvf, cvb, cstep["r"])
                    cstep["r"] += 1

        def post(g, h):
            g5 = slice(g * 512, (g + 1) * 512)
            u = (g * 4 + h) % 2
            t = lambda i, u=u: tf[:, u, i, :]
            tk = lambda i, u=u: ("tf", u * NT + i)
            k.act(sq[:], pOo[:, u, :], AF.Square, [("pOo", u)], ["sq"])
            yield
            k.mm(pV[:], ones_b[:], sq[:], True, True, ["ones_b", "sq"], ["pV"])
            yield
            k.act(t(9), pV[:], AF.Ln, ["pV"], [tk(9)], scale=1.0 / 128, bias=EPS)
            yield
            k.act(t(9), t(9), AF.Exp, [tk(9)], [tk(9)], scale=-0.5)
            yield
            k.stt("dve", t(9), pOo[:, u, :], gnT[:, h:h + 1], t(9), ALU.mult, ALU.mult,
                  [("pOo", u), "gnT", tk(9)], [tk(9)])
            yield
            k.tt("dve", yo[:, u, :], t(9), t(8), ALU.mult, [tk(9), tk(8)], [("yo", u)])
            yield
            k.dma("sp", T["ya_s"][:, h, g5], yo[:, u, :], "yo%d" % u, [("yo", u)], [])

        units = [(g, h) for g in range(NG) for h in range(4)]
        vproj(0)
        for _ in prologue(*units[0]):
            pass
        qg = None
        for n, (g, h) in enumerate(units):
            pg = None
            if n + 1 < len(units):
                g1, h1 = units[n + 1]
                if h1 == 0:
                    vproj(g1)
                pg = prologue(g1, h1)
            chunks(g, h, pg, qg)
            if qg is not None:
                for _ in qg:
                    pass
            if pg is not None:
                for _ in pg:
                    pass
            qg = post(g, h)
        for _ in qg:
            pass
        if k.cfg.get("convert", True) and NG == 8:
            while cstep["r"] < 256 + NCV:
                convert_step(k, T, cvf, cvb, cstep["r"])
                cstep["r"] += 1
        s.barrier()


def phase_E1(k, T, sbt, pst, xnT, load_w):
    nc, s = k.nc, k.s
    NG = k.cfg.get("ngroups_e1", 8)
    with ExitStack() as st:
        wG = sbt(st, "wG", [128, 8, 2048], BF16)
        wU = sbt(st, "wU", [128, 2, 4, 1024], BF16)
        yab = sbt(st, "yab", [128, 2, 2, 4, 512], BF16)
        sg = sbt(st, "sg", [128, 2, 2, 512], F32)
        tmp = sbt(st, "e1tmp", [128, 2, 2, 512], F32)
        hTg = sbt(st, "hTg", [128, 2, 8, 512], BF16)
        pE = pst(st, "pE", [128, 2, 4, 512], F32)
        load_w(wG[:, :, 0:1024], "wG", T["w_in"][:, COL["ga"]:COL["ga"] + 1024], 8, 1024)
        load_w(wG[:, :, 1024:2048], "wG", T["w_in"][:, COL["gb"]:COL["gb"] + 1024], 8, 1024)
        load_w(wU[:, 0], "wU", T["w_up_a"], 4, 1024)
        load_w(wU[:, 1], "wU", T["w_up_b"], 4, 1024)
        for g in range(NG):
            gp = g % 2
            g5 = slice(g * 512, (g + 1) * 512)
            xk = ("xnT", 4 * g, 4 * g + 4)
            k.dma("sp", yab[:, gp, 0], T["ya_s"][:, :, g5], "yab%d" % gp, [], [("yab", gp)])
            k.dma("sp", yab[:, gp, 1], T["yb_s"][:, :, g5], "yab%d" % gp, [], [("yab", gp)])
            for nn in range(8):
                pb = nn % 2
                ns = slice(nn * 128, (nn + 1) * 128)
                for ab in range(2):
                    for kc in range(8):
                        k.mm(pE[:, pb, ab, :], wG[:, kc, ab * 1024 + nn * 128:ab * 1024 + (nn + 1) * 128], xnT[:, kc, g5],
                             kc == 0, kc == 7, [xk, "wG"], [("pE", pb * 4 + ab)])
                    for c4 in range(4):
                        k.mm(pE[:, pb, 2 + ab, :], wU[:, ab, c4, ns], yab[:, gp, ab, c4, :], c4 == 0, c4 == 3,
                             [("yab", gp), "wU"], [("pE", pb * 4 + 2 + ab)])
                for ab in range(2):
                    k.act(sg[:, pb, ab, :], pE[:, pb, ab, :], AF.Exp, [("pE", pb * 4 + ab)], [("sg", pb * 2 + ab)], scale=-1.0)
                    k.act(sg[:, pb, ab, :], sg[:, pb, ab, :], AF.Ln, [("sg", pb * 2 + ab)], [("sg", pb * 2 + ab)], bias=1.0)
                    k.act(sg[:, pb, ab, :], sg[:, pb, ab, :], AF.Exp, [("sg", pb * 2 + ab)], [("sg", pb * 2 + ab)], scale=-1.0)
                    k.tt("dve", tmp[:, pb, ab, :], sg[:, pb, ab, :], pE[:, pb, 2 + ab, :], ALU.mult,
                         [("sg", pb * 2 + ab), ("pE", pb * 4 + 2 + ab)], [("e1tmp", pb * 2 + ab)])
                k.tt("dve", hTg[:, gp, nn, :], tmp[:, pb, 0, :], tmp[:, pb, 1, :], ALU.add,
                     [("e1tmp", pb * 2), ("e1tmp", pb * 2 + 1)], [("hTg", gp)])
            k.dma("sp", T["hT_s"][:, :, g5], hTg[:, gp], "hTg%d" % gp, [("hTg", gp)], [])
        s.barrier()


def peer_routing_alloc(sbt, st, pfx="r"):
    B = {}
    B["vals"] = sbt(st, pfx + "vals", [128, 16, 16], F32)
    B["idxs"] = sbt(st, pfx + "idxs", [128, 16, 16], U32)
    B["idxf"] = sbt(st, pfx + "idxf", [128, 16, 16], F32)
    B["s2"] = sbt(st, pfx + "s2", [128, 128], F32)
    B["cand"] = sbt(st, pfx + "cand", [128, 8, 256], F32)
    B["cand2"] = sbt(st, pfx + "cand2", [128, 8, 256], F32)
    B["tops"] = sbt(st, pfx + "tops", [128, 8, 16], F32)
    B["pos"] = sbt(st, pfx + "pos", [128, 8, 16], U32)
    B["ipos"] = sbt(st, pfx + "ipos", [128, 8, 16], U32)
    B["jpos"] = sbt(st, pfx + "jpos", [128, 8, 16], U32)
    B["iposf"] = sbt(st, pfx + "iposf", [128, 8, 16], F32)
    B["jposf"] = sbt(st, pfx + "jposf", [128, 8, 16], F32)
    B["eq"] = sbt(st, pfx + "eq", [128, 8, 16, 16], F32)
    B["sel1"] = sbt(st, pfx + "sel1", [128, 8, 16], F32)
    B["sel2"] = sbt(st, pfx + "sel2", [128, 8, 16], F32)
    B["gsum"] = sbt(st, pfx + "gsum", [128, 8], F32)
    return B


def peer_routing(k, B, s_sb, s_key, iota16, eidx, eidx_key, gw, gw_key, pfx="r"):
    vals, idxs, idxf, s2, cand, cand2 = B["vals"], B["idxs"], B["idxf"], B["s2"], B["cand"], B["cand2"]
    tops, pos, ipos, jpos, iposf, jposf = B["tops"], B["pos"], B["ipos"], B["jpos"], B["iposf"], B["jposf"]
    eq, sel1, sel2, gsum = B["eq"], B["sel1"], B["sel2"], B["gsum"]
    K_ = pfx
    for l in range(16):
        k.gen("dve", lambda e, l=l: e.max(out=vals[:, l, 0:8], in_=s_sb[:, l, :]), [s_key], [K_ + "vals"])
        k.gen("dve", lambda e, l=l: e.max_index(out=idxs[:, l, 0:8], in_max=vals[:, l, 0:8], in_values=s_sb[:, l, :]),
              [s_key, K_ + "vals"], [K_ + "idxs"])
        k.gen("dve", lambda e, l=l: e.match_replace(out=s2[:], in_to_replace=vals[:, l, 0:8], in_values=s_sb[:, l, :],
                                                    imm_value=-1e30), [s_key, K_ + "vals"], [K_ + "s2"])
        k.gen("dve", lambda e, l=l: e.max(out=vals[:, l, 8:16], in_=s2[:]), [K_ + "s2"], [K_ + "vals"])
        k.gen("dve", lambda e, l=l: e.max_index(out=idxs[:, l, 8:16], in_max=vals[:, l, 8:16], in_values=s2[:]),
              [K_ + "s2", K_ + "vals"], [K_ + "idxs"])
        yield
    k.cp("dve", idxf[:], idxs[:], [K_ + "idxs"], [K_ + "idxf"])
    v4 = vals[:].rearrange("p (h two) i -> p h two i", two=2)
    x4 = idxf[:].rearrange("p (h two) i -> p h two i", two=2)
    c4 = cand[:].rearrange("p h (i j) -> p h i j", j=16)
    k.tt("dve", c4, v4[:, :, 0, :].unsqueeze(3).broadcast_to([128, 8, 16, 16]),
         v4[:, :, 1, :].unsqueeze(2).broadcast_to([128, 8, 16, 16]), ALU.add, [K_ + "vals"], [K_ + "cand"])
    for h in range(8):
        k.gen("dve", lambda e, h=h: e.max(out=tops[:, h, 0:8], in_=cand[:, h, :]), [K_ + "cand"], [K_ + "tops"])
        k.gen("dve", lambda e, h=h: e.max_index(out=pos[:, h, 0:8], in_max=tops[:, h, 0:8], in_values=cand[:, h, :]),
              [K_ + "cand", K_ + "tops"], [K_ + "pos"])
        k.gen("dve", lambda e, h=h: e.match_replace(out=cand2[:, h, :], in_to_replace=tops[:, h, 0:8],
                                                    in_values=cand[:, h, :], imm_value=-1e30),
              [K_ + "cand", K_ + "tops"], [K_ + "cand2"])
        k.gen("dve", lambda e, h=h: e.max(out=tops[:, h, 8:16], in_=cand2[:, h, :]), [K_ + "cand2"], [K_ + "tops"])
        k.gen("dve", lambda e, h=h: e.max_index(out=pos[:, h, 8:16], in_max=tops[:, h, 8:16], in_values=cand2[:, h, :]),
              [K_ + "cand2", K_ + "tops"], [K_ + "pos"])
        yield
    k.tsc("dve", ipos[:], pos[:], 4, None, ALU.logical_shift_right, None, [K_ + "pos"], [K_ + "ipos"])
    k.tsc("dve", jpos[:], pos[:], 15, None, ALU.bitwise_and, None, [K_ + "pos"], [K_ + "jpos"])
    k.cp("dve", iposf[:], ipos[:], [K_ + "ipos"], [K_ + "iposf"])
    k.cp("dve", jposf[:], jpos[:], [K_ + "jpos"], [K_ + "jposf"])
    io4 = iota16[:].unsqueeze(1).unsqueeze(1).broadcast_to([128, 8, 16, 16])
    for (pf_, xi, sel) in ((iposf, 0, sel1), (jposf, 1, sel2)):
        k.tt("dve", eq[:], io4, pf_[:].unsqueeze(3).broadcast_to([128, 8, 16, 16]), ALU.is_equal,
             ["iota16", K_ + "iposf", K_ + "jposf"], [K_ + "eq"])
        k.tt("dve", eq[:], eq[:], x4[:, :, xi, :].unsqueeze(2).broadcast_to([128, 8, 16, 16]), ALU.mult,
             [K_ + "eq", K_ + "idxf"], [K_ + "eq"])
        k.tred("dve", sel[:], eq[:], ALU.add, [K_ + "eq"], [K_ + "sel"])
    yield
    k.stt("dve", sel1[:], sel1[:], 128.0, sel2[:], ALU.mult, ALU.add, [K_ + "sel"], [K_ + "sel"])
    k.cp("dve", eidx.rearrange("p (h i) -> p h i", i=16), sel1[:], [K_ + "sel"], [eidx_key])
    k.tt("dve", gw, tops[:], tops[:, :, 0:1].broadcast_to([128, 8, 16]), ALU.subtract, [K_ + "tops"], [gw_key])
    k.act(gw, gw, AF.Exp, [gw_key], [gw_key])
    k.tred("dve", gsum[:], gw, ALU.add, [gw_key], [K_ + "gsum"])
    k.recip(gsum[:], gsum[:], [K_ + "gsum"], [K_ + "gsum"])
    k.tt("dve", gw, gw, gsum[:].unsqueeze(2).broadcast_to([128, 8, 16]), ALU.mult, [gw_key, K_ + "gsum"], [gw_key])


def phase_E2(k, T, sbt, pst, identb, identf, load_w):
    nc, s = k.nc, k.s
    NB_ = k.cfg.get("nblk_e2", NBLK)
    NU = 11
    GS = 4
    with ExitStack() as st:
        wO = sbt(st, "wO", [128, 8, 1024], BF16)
        wQ = sbt(st, "wQ", [128, 8, 2048], BF16)
        keysT = sbt(st, "keysT", [128, 16, 128], BF16)
        g2 = sbt(st, "g2", [128, 1024], F32)
        g3 = sbt(st, "g3", [128, 1024], F32)
        iota16 = sbt(st, "iota16", [128, 16], F32)
        load_w(wO[:], "wO", T["w_out"], 8, 1024)
        load_w(wQ[:], "wQ", T["peer_wq"], 8, 2048)
        k.dma("sp", g2[:], T["norm_ffn"].partition_broadcast(128), "c0", [], ["g2"])
        k.dma("sp", g3[:], T["norm_final"].partition_broadcast(128), "c0", [], ["g3"])
        k.dma("sp", iota16[:], T["c_iota16"], "c0", [], ["iota16"])
        with ExitStack() as st2:
            kst = sbt(st2, "kst", [128, 2, 128], F32)
            pK = pst(st2, "pK", [128, 128], F32)
            for l in range(16):
                h_, p_ = l // 2, l % 2
                kb = l % 2
                k.dma("sp", kst[:, kb, :], T["peer_keys"][p_ * 8 + h_], "kst%d" % kb, [], [("kst", kb)])
                k.tr(pK[:], kst[:, kb, :], identf[:], [("kst", kb), "identf"], ["pK"])
                k.cp("act", keysT[:, l, :], pK[:], ["pK"], ["keysT"])
            s.barrier()

        hTb = sbt(st, "hTb", [128, 2, 8, 128], BF16)
        xb = sbt(st, "xb", [128, 1, 2, 512], F32)
        x1 = sbt(st, "x1", [128, 2, 2, 512], F32)
        junk = sbt(st, "junk", [128, 1024], BF16)
        junkb = sbt(st, "junkb", [128, 1024], BF16)
        junkb2 = sbt(st, "junkb2", [128, 1024], BF16)
        prodb = sbt(st, "prodb", [128, 3, 1024], BF16)
        ssq = sbt(st, "ssq", [128, 4], F32)
        xn2f = sbt(st, "xn2f", [128, 1024], F32)
        xn2b = sbt(st, "xn2b", [128, 2, 1024], BF16)
        xn2T = sbt(st, "xn2T", [128, 8, 128], BF16)
        qTs = sbt(st, "qTs", [128, 8, 128], BF16)
        s_sb = sbt(st, "s_sb", [128, 16, 128], F32)
        eidx = sbt(st, "eidx", [128, 2, 128], I32)
        gw = sbt(st, "gw", [128, 2, 8, 16], F32)
        uvg = sbt(st, "uvg", [128, NU, 2048], BF16)
        dg = sbt(st, "dg", [128, 4, 128], BF16)
        hcol = sbt(st, "hcol", [128, 128], F32)
        acol = sbt(st, "acol", [128, 128], F32)
        ob = sbt(st, "ob", [128, 2, 512], F32)
        RB = peer_routing_alloc(sbt, st)
        pY = pst(st, "pY", [128, 2, 512], F32)
        pG = pst(st, "pG", [128, 2, 512], F32)
        pT2 = pst(st, "pT2", [128, 8, 128], BF16)
        pQ = pst(st, "pQ", [128, 8, 128], F32)

        def front(b):
            p = b % 2
            tb = slice(b * 128, (b + 1) * 128)
            k.dma("sp", hTb[:, p], T["hT_s"][:, :, tb], "hTb%d" % p, [], [("hTb", p)])
            k.dma("sp", xb[:, 0], T["x"][tb, :].rearrange("t (a n) -> t a n", a=2), "xb0", [], [("xb", 0)])
            for half in range(2):
                for c in range(8):
                    k.mm(pY[:, half, :], hTb[:, p, c, :], wO[:, c, half * 512:(half + 1) * 512], c == 0, c == 7,
                         [("hTb", p), "wO"], ["pY"])
                yield
            k.tt("dve", x1[:, p], xb[:, 0], pY[:], ALU.add, [("xb", 0), "pY"], [("x1", p)])
            x1f = x1[:, p].rearrange("p a n -> p (a n)")
            k.act(junk[:], x1f, AF.Square, [("x1", p)], ["junk", ("ssq", p)], accum=ssq[:, p:p + 1])
            k.act(ssq[:, p:p + 1], ssq[:, p:p + 1], AF.Ln, [("ssq", p)], [("ssq", p)], scale=1.0 / D, bias=EPS)
            k.act(ssq[:, p:p + 1], ssq[:, p:p + 1], AF.Exp, [("ssq", p)], [("ssq", p)], scale=-0.5)
            k.stt("dve", xn2f[:], x1f, ssq[:, p:p + 1], g2[:], ALU.mult, ALU.mult, [("x1", p), ("ssq", p), "g2"], ["xn2f"])
            k.cp("act", xn2b[:, p, :], xn2f[:], ["xn2f"], [("xn2b", p)])
            for c in range(8):
                k.tr(pT2[:, c, :], xn2b[:, p, c * 128:(c + 1) * 128], identb[:], [("xn2b", p), "identb"], ["pT2"])
            k.cp("act", xn2T[:], pT2[:], ["pT2"], ["xn2T"])
            yield
            for l0 in (0, 8):
                for l in range(8):
                    for kc in range(8):
                        k.mm(pQ[:, l, :], wQ[:, kc, (l0 + l) * 128:(l0 + l + 1) * 128], xn2T[:, kc, :], kc == 0, kc == 7,
                             ["xn2T", "wQ"], [("pQ", l // 4)])
                    yield
                for hb in range(2):
                    ls = slice(hb * 4, hb * 4 + 4)
                    k.cp("act", qTs[:, ls, :], pQ[:, ls, :], [("pQ", hb)], [("qTs", hb)])
                yield
                for l in range(8):
                    k.mm(pQ[:, l, :], qTs[:, l, :], keysT[:, l0 + l, :], True, True, [("qTs", l // 4), "keysT"], [("pQ", l // 4)])
                for hb in range(2):
                    ls = slice(hb * 4, hb * 4 + 4)
                    k.cp("act", s_sb[:, l0 + hb * 4:l0 + hb * 4 + 4, :], pQ[:, ls, :], [("pQ", hb)], ["s_sb"])
                yield
            yield from peer_routing(k, RB, s_sb, "s_sb", iota16, eidx[:, p, :], ("eidx", p), gw[:, p], ("gw", p))

        def gath(b, fg):
            p = b % 2
            tb = slice(b * 128, (b + 1) * 128)
            x1f = x1[:, p].rearrange("p a n -> p (a n)")
            gwf = gw[:, p].rearrange("p h i -> p (h i)")
            for kk in range(128):
                ub = kk % NU
                db = kk % 4
                hk = ("hcol", kk % 8)
                ak = ("acol", kk % 8)
                k.s.dma("pool", lambda e, kk=kk, ub=ub, p=p: e.indirect_dma_start(
                    out=uvg[:, ub, :], out_offset=None, in_=T["uv_s"][:, :],
                    in_offset=bass.IndirectOffsetOnAxis(ap=eidx[:, p, kk:kk + 1], axis=0)),
                    "uvg%d" % ub, [("eidx", p), "uv_s"], [("uvg", ub)])
                k.stt("dve", junkb[:], uvg[:, ub, 0:1024], 1.0, xn2b[:, p, :], ALU.mult, ALU.mult,
                      [("uvg", ub), ("xn2b", p)], ["junkb", hk], accum=hcol[:, kk:kk + 1])
                k.act(acol[:, kk:kk + 1], hcol[:, kk:kk + 1], AF.Gelu, [hk], [ak])
                k.act(acol[:, kk:kk + 1], acol[:, kk:kk + 1], AF.Copy, [ak, ("gw", p)], [ak], scale=gwf[:, kk:kk + 1])
                k.act(dg[:, db, :], identb[:], AF.Copy, ["identb", ak], [("dg", db)], scale=acol[:, kk:kk + 1])
                for half in range(2):
                    k.mm(pG[:, half, :], dg[:, db, :], uvg[:, ub, 1024 + half * 512:1024 + (half + 1) * 512],
                         kk == 0, kk == 127, [("dg", db), ("uvg", ub)], ["pG"])
                if fg is not None:
                    next(fg, None)
            if fg is not None:
                for _ in fg:
                    pass
            k.tt("dve", x1[:, p], x1[:, p], pG[:], ALU.add, [("x1", p), "pG"], [("x1", p)])
            k.act(junk[:], x1f, AF.Square, [("x1", p)], ["junk", ("ssq", 2)], accum=ssq[:, 2:3])
            k.act(ssq[:, 2:3], ssq[:, 2:3], AF.Ln, [("ssq", 2)], [("ssq", 2)], scale=1.0 / D, bias=EPS)
            k.act(ssq[:, 2:3], ssq[:, 2:3], AF.Exp, [("ssq", 2)], [("ssq", 2)], scale=-0.5)
            k.stt("dve", ob[:].rearrange("p a n -> p (a n)"), x1f, ssq[:, 2:3], g3[:], ALU.mult, ALU.mult,
                  [("x1", p), ("ssq", 2), "g3"], ["ob"])
            k.dma("sp", T["out"][tb, :].rearrange("t (a n) -> t a n", a=2), ob[:], "ob", ["ob"], [])

        for _ in front(0):
            pass
        for b in range(NB_):
            gath(b, front(b + 1) if b + 1 < NB_ else None)
        s.barrier()


_NC_CACHE = {}


def _core_inputs(inp, b, consts):
    d = {
        "x": np.ascontiguousarray(inp["x"][b], dtype=np.float32),
        "norm_mix": np.asarray(inp["norm_mix"], np.float32).reshape(1, D),
        "w_in": np.ascontiguousarray(np.asarray(inp["w_in"], np.float32)[0]),
        "hg_lb": np.asarray(inp["hg_lb"], np.float32).reshape(2, 512),
        "hg_norm": np.asarray(inp["hg_norm"], np.float32).reshape(1, 512),
        "idx_k_norm_g": np.asarray(inp["idx_k_norm_g"], np.float32).reshape(1, 64),
        "idx_k_norm_b": np.asarray(inp["idx_k_norm_b"], np.float32).reshape(1, 64),
        "w_up_a": np.ascontiguousarray(np.asarray(inp["w_up_a"], np.float32)[0]),
        "w_up_b": np.ascontiguousarray(np.asarray(inp["w_up_b"], np.float32)[0]),
        "w_out": np.ascontiguousarray(np.asarray(inp["w_out"], np.float32)[0]),
        "norm_ffn": np.asarray(inp["norm_ffn"], np.float32).reshape(1, D),
        "peer_wq": np.ascontiguousarray(np.asarray(inp["peer_wq"], np.float32)[0]),
        "peer_keys": np.ascontiguousarray(np.asarray(inp["peer_keys"], np.float32)[0]).reshape(16, 128, 128),
        "peer_u": np.ascontiguousarray(np.asarray(inp["peer_u"], np.float32)[0]),
        "peer_v": np.ascontiguousarray(np.asarray(inp["peer_v"], np.float32)[0]),
        "norm_final": np.asarray(inp["norm_final"], np.float32).reshape(1, D),
    }
    d.update(consts)
    return d


def kernel(**inputs):
    if "nc" not in _NC_CACHE:
        _NC_CACHE["nc"] = build()
    nc = _NC_CACHE["nc"]
    consts = host_consts(np.asarray(inputs["rel_bias"], np.float32))
    shared = _core_inputs(inputs, 0, consts)
    in_maps = []
    for b in range(NCORES):
        d = dict(shared)
        d["x"] = np.ascontiguousarray(np.asarray(inputs["x"])[b], dtype=np.float32)
        in_maps.append(d)
    res = run_bass_kernel_spmd(nc, in_maps, core_ids=list(range(NCORES)))
    out = np.stack([np.asarray(r["out"], dtype=np.float32) for r in res.results], axis=0)
    return out
```

```python
import math
from contextlib import ExitStack

import numpy as np
import ml_dtypes
import concourse.bass as bass
import concourse.mybir as mybir
from concourse.bass_utils import run_bass_kernel_spmd

F32 = mybir.dt.float32
BF16 = mybir.dt.bfloat16
I32 = mybir.dt.int32
U32 = mybir.dt.uint32
ALU = mybir.AluOpType
AF = mybir.ActivationFunctionType
AX = mybir.AxisListType

S = 4096
D = 1024
NBLK = 32
NCORES = 8
COL = dict(hq=0, hf=512, hi=1024, hog=1536, aq=2048, ak=2560, av=2624, iq=2688, ik=2944,
           iw=3008, ga=3012, gb=4036)
IN_WIDTH = 5060
EPS = 1e-6
NEG = -30000.0
NROUNDS = 16


class Sched:
    ENG = ("pe", "dve", "act", "pool", "sp")

    def __init__(self, nc):
        self.nc = nc
        self.q = {e: [] for e in self.ENG}
        self.cnt = {}
        self.seen = {e: {} for e in self.ENG}
        self.w = {}
        self.r = {}
        self.excl = set()

    def _split(self, reads, writes):
        reads = self._units(reads)
        writes = self._units(writes)
        ex = [u for u in reads if u[0] in self.excl]
        if ex:
            reads = [u for u in reads if u[0] not in self.excl]
            writes = writes + [u for u in ex if u not in writes]
        return reads, writes

    @staticmethod
    def _units(specs):
        out = []
        for s in specs:
            if isinstance(s, str):
                out.append((s, 0))
            elif len(s) == 2:
                out.append((s[0], s[1]))
            else:
                for i in range(s[1], s[2]):
                    out.append((s[0], i))
        return out

    def _deps(self, eng, reads, writes):
        deps = {}

        def add(ev, kind):
            if ev is None:
                return
            sem, val = ev
            if sem == "E:" + eng and eng == "pe":
                return
            if deps.get(sem, 0) < val:
                deps[sem] = val

        for u in reads:
            add(self.w.get(u), "raw")
        for u in writes:
            add(self.w.get(u), "waw")
            for sem, val in self.r.get(u, {}).items():
                add((sem, val), "war")
        waits = []
        seen = self.seen[eng]
        for sem, val in deps.items():
            if seen.get(sem, 0) < val:
                seen[sem] = val
                waits.append((sem, val))
        return waits

    def _register(self, ev, reads, writes):
        sem, val = ev
        for u in reads:
            d = self.r.setdefault(u, {})
            if d.get(sem, 0) < val:
                d[sem] = val
        for u in writes:
            self.w[u] = ev
            self.r[u] = {}

    def op(self, eng, fn, reads=(), writes=()):
        reads, writes = self._split(reads, writes)
        waits = self._deps(eng, reads, writes)
        sem = "E:" + eng
        self.cnt[sem] = self.cnt.get(sem, 0) + 1
        ev = (sem, self.cnt[sem])
        self.q[eng].append((fn, waits, (sem, 1)))
        self._register(ev, reads, writes)
        return ev

    def dma(self, queue, fn, sem, reads=(), writes=()):
        reads, writes = self._split(reads, writes)
        waits = self._deps(queue, reads, writes)
        sem = "D:" + sem
        prev = self.cnt.get(sem, 0)
        if prev and self.seen[queue].get(sem, 0) < prev:
            self.seen[queue][sem] = prev
            waits.append((sem, prev))
        self.cnt[sem] = self.cnt.get(sem, 0) + 16
        ev = (sem, self.cnt[sem])
        self.q[queue].append((fn, waits, (sem, 16)))
        self._register(ev, reads, writes)
        return ev

    def barrier(self, engs=None):
        for eng in (engs or self.ENG):
            waits = []
            for sem, val in self.cnt.items():
                if sem == "E:" + eng:
                    continue
                if self.seen[eng].get(sem, 0) < val:
                    self.seen[eng][sem] = val
                    waits.append((sem, val))
            if waits:
                self.q[eng].append((None, waits, None))

    def emit(self):
        nc = self.nc
        with ExitStack() as st:
            handles = {}
            for name in self.cnt:
                handles[name] = st.enter_context(nc.semaphore(name.replace(":", "_")))
            block = st.enter_context(nc.Block())
            engobjs = {"pe": block.tensor, "dve": block.vector, "act": block.scalar,
                       "pool": block.gpsimd, "sp": block.sync}

            def make(ename):
                lst = self.q[ename]

                def body(e):
                    for fn, waits, inc in lst:
                        for sem, val in waits:
                            e.wait_ge(handles[sem], val)
                        if fn is not None:
                            ins = fn(e)
                            ins.then_inc(handles[inc[0]], inc[1])
                return body

            for ename in self.ENG:
                if self.q[ename]:
                    engobjs[ename](make(ename))


class K:
    def __init__(self, nc, cfg):
        self.nc = nc
        self.cfg = cfg
        self.s = Sched(nc)
        self.uid = 0

    def mm(self, out, lhsT, rhs, start, stop, r, w):
        self.s.op("pe", lambda e: e.matmul(out, lhsT=lhsT, rhs=rhs, start=start, stop=stop), r, w)

    def tr(self, out, in_, ident, r, w):
        self.s.op("pe", lambda e: e.transpose(out=out, in_=in_, identity=ident), r, w)

    def act(self, out, in_, func, r, w, scale=1.0, bias=0.0, accum=None):
        if accum is None:
            self.s.op("act", lambda e: e.activation(out=out, in_=in_, func=func, bias=bias, scale=scale), r, w)
        else:
            self.s.op("act", lambda e: e.activation(out=out, in_=in_, func=func, bias=bias, scale=scale,
                                                    accum_out=accum), r, w)

    def tsc(self, eng, out, in0, s1, s2, op0, op1, r, w, accum=None):
        if op1 is None:
            self.s.op(eng, lambda e: e.tensor_scalar(out=out, in0=in0, scalar1=s1, scalar2=None, op0=op0), r, w)
        elif accum is None:
            self.s.op(eng, lambda e: e.tensor_scalar(out=out, in0=in0, scalar1=s1, scalar2=s2, op0=op0, op1=op1), r, w)
        else:
            self.s.op(eng, lambda e: e.tensor_scalar(out=out, in0=in0, scalar1=s1, scalar2=s2, op0=op0, op1=op1,
                                                     accum_out=accum), r, w)

    def stt(self, eng, out, in0, scalar, in1, op0, op1, r, w, accum=None):
        if accum is None:
            self.s.op(eng, lambda e: e.scalar_tensor_tensor(out=out, in0=in0, scalar=scalar, in1=in1, op0=op0, op1=op1), r, w)
        else:
            self.s.op(eng, lambda e: e.scalar_tensor_tensor(out=out, in0=in0, scalar=scalar, in1=in1, op0=op0, op1=op1,
                                                            accum_out=accum), r, w)

    def tt(self, eng, out, in0, in1, op, r, w):
        self.s.op(eng, lambda e: e.tensor_tensor(out=out, in0=in0, in1=in1, op=op), r, w)

    def tred(self, eng, out, in_, op, r, w):
        self.s.op(eng, lambda e: e.tensor_reduce(out=out, in_=in_, axis=AX.X, op=op), r, w)

    def cp(self, eng, out, in_, r, w):
        if eng == "act":
            self.s.op("act", lambda e: e.copy(out=out, in_=in_), r, w)
        else:
            self.s.op(eng, lambda e: e.tensor_copy(out=out, in_=in_), r, w)

    def recip(self, out, in_, r, w):
        self.s.op("dve", lambda e: e.reciprocal(out=out, in_=in_), r, w)

    def memset(self, eng, ap, val, w):
        self.s.op(eng, lambda e: e.memset(ap, val), (), w)

    def dma(self, q, out, in_, sem, r, w):
        self.s.dma(q, lambda e: e.dma_start(out=out, in_=in_), sem, r, w)

    def gen(self, eng, fn, r, w):
        self.s.op(eng, fn, r, w)


def t5_bucket_np(n):
    n = np.maximum(n, 0)
    nf = np.maximum(n, 1).astype(np.float32)
    large = 16 + (np.log(nf / np.float32(16)) / np.float32(math.log(128 / 16)) * np.float32(16)).astype(np.int32)
    large = np.minimum(large, 31)
    return np.where(n < 16, n, large)


def host_consts(rel_bias):
    c = {}
    c["c_ident"] = np.eye(128, dtype=np.float32)
    tl = np.arange(128)
    c["c_cmask"] = np.where(tl[None, :] <= tl[:, None], 0.0, -1e30).astype(np.float32)
    t64 = np.arange(64)
    c["c_tri"] = (t64[:, None] <= t64[None, :]).astype(np.float32)
    cm = np.ones((128, 512), np.float32)
    cm[:, ::64] = 0.0
    c["c_chunkmask"] = cm
    e65 = np.zeros((65, 64), np.float32)
    e65[64, :] = 1.0
    c["c_e65"] = e65
    sel = np.zeros((64, 256), np.float32)
    sel[np.arange(64), np.arange(64)] = 1.0
    sel[np.arange(64), 128 + 64 + np.arange(64)] = 1.0
    c["c_sel"] = sel
    c["c_iota16"] = np.tile(np.arange(16, dtype=np.float32)[None, :], (128, 1))
    sl = np.arange(128)[:, None]
    tt_ = np.arange(128)[None, :]
    bd = t5_bucket_np(tt_ - sl)
    bp = t5_bucket_np(128 + tt_ - sl)
    rb = np.asarray(rel_bias, np.float32)
    c["c_bd"] = np.ascontiguousarray(rb[bd].transpose(0, 2, 1))
    c["c_bp"] = np.ascontiguousarray(rb[bp].transpose(0, 2, 1))
    c["c_b31"] = np.ascontiguousarray(np.broadcast_to(rb[31][None, :, None], (128, 8, 128)))
    return c


CONST_SHAPES = {
    "c_ident": [128, 128], "c_cmask": [128, 128], "c_tri": [64, 64], "c_chunkmask": [128, 512],
    "c_e65": [65, 64], "c_sel": [64, 256], "c_iota16": [128, 16],
    "c_bd": [128, 8, 128], "c_bp": [128, 8, 128], "c_b31": [128, 8, 128],
}

INPUT_SHAPES = {
    "x": [S, D], "norm_mix": [1, D], "w_in": [D, IN_WIDTH], "hg_lb": [2, 512], "hg_norm": [1, 512],
    "idx_k_norm_g": [1, 64], "idx_k_norm_b": [1, 64], "w_up_a": [512, D], "w_up_b": [512, D],
    "w_out": [D, D], "norm_ffn": [1, D], "peer_wq": [D, 2048], "peer_keys": [16, 128, 128],
    "peer_u": [16384, D], "peer_v": [16384, D], "norm_final": [1, D],
}

SCRATCH = {
    "ya_s": ([128, 4, S], BF16), "yb_s": ([128, 4, S], BF16), "hT_s": ([128, 8, S], BF16),
    "uv_s": ([16384, 2048], BF16),
}


def build(cfg=None):
    cfg = cfg or {}
    phases = cfg.get("phases", ["A", "BC", "D", "E1", "E2"])
    inject = cfg.get("inject", [])
    taps = cfg.get("taps", [])
    nc = bass.Bass("TRN2", target_bir_lowering=False)
    k = K(nc, cfg)
    s = k.s
    T = {}
    for name, shp in INPUT_SHAPES.items():
        T[name] = nc.dram_tensor(name, shp, F32, kind="ExternalInput").ap()
    for name, shp in CONST_SHAPES.items():
        T[name] = nc.dram_tensor(name, shp, F32, kind="ExternalInput").ap()
    for name, (shp, dt) in SCRATCH.items():
        kind = "ExternalInput" if name in inject else ("ExternalOutput" if name in taps else "Internal")
        T[name] = nc.dram_tensor(name, shp, dt, kind=kind).ap()
    T["out"] = nc.dram_tensor("out", [S, D], F32, kind="ExternalOutput").ap()

    with ExitStack() as top, nc.allow_low_precision("bf16 matmul operands, fp32 accumulation"), \
            nc.allow_non_contiguous_dma("small strided constant loads"):
        def sbt(st, name, shape, dt):
            return st.enter_context(nc.sbuf_tensor(name, shape, dt))

        def pst(st, name, shape, dt):
            s.excl.add(name)
            return st.enter_context(nc.psum_tensor(name, shape, dt))

        identf = sbt(top, "identf", [128, 128], F32)
        identb = sbt(top, "identb", [128, 128], BF16)
        wst = sbt(top, "wst", [128, 2, 8, 128], F32)
        k.dma("sp", identf[:], T["c_ident"], "c0", [], ["identf"])
        k.cp("dve", identb[:], identf[:], ["identf"], ["identb"])
        wstate = {"n": 0}

        def load_w(dst, dst_key, src, C, n):
            for c0 in range(0, n, 128):
                wdt = min(128, n - c0)
                b = wstate["n"] % 2
                wstate["n"] += 1
                k.dma("sp" if b == 0 else "act", wst[:, b, 0:C, 0:wdt],
                      src[:, c0:c0 + wdt].rearrange("(c p) n -> p c n", p=128), "wst%d" % b, [], [("wst", b)])
                k.cp("dve", dst[:, :, c0:c0 + wdt], wst[:, b, 0:C, 0:wdt], [("wst", b)], [dst_key])

        with ExitStack() as mid:
            xnT = sbt(mid, "xnT", [128, 8, S], BF16)
            if "A" in phases:
                phase_A(k, T, sbt, pst, xnT, identb)
            if "BC" in phases:
                phase_BC(k, T, sbt, pst, xnT, identb, identf, load_w)
            if "D" in phases:
                phase_D(k, T, sbt, pst, xnT, identb, identf, load_w)
            if "E1" in phases:
                phase_E1(k, T, sbt, pst, xnT, load_w)
            s.barrier()
        if "E2" in phases:
            phase_E2(k, T, sbt, pst, identb, identf, load_w)
        s.barrier()
        s.emit()
    return nc


def phase_A(k, T, sbt, pst, xnT, identb):
    nc, s = k.nc, k.s
    with ExitStack() as st:
        xt = sbt(st, "A_xt", [128, 2, D], F32)
        gb = sbt(st, "A_gb", [128, D], F32)
        junk = sbt(st, "A_junk", [128, D], F32)
        ss = sbt(st, "A_ss", [128, 2], F32)
        rstd = sbt(st, "A_rstd", [128, 2], F32)
        xs = sbt(st, "A_xs", [128, 2, D], BF16)
        pt = pst(st, "A_pt", [128, 2, 8, 128], BF16)
        k.dma("sp", gb[:], T["norm_mix"].partition_broadcast(128), "c0", [], ["A_gb"])
        for b in range(k.cfg.get("nblk_a", NBLK)):
            p = b % 2
            k.dma("sp", xt[:, p, :], T["x"][b * 128:(b + 1) * 128, :], "A_x%d" % p, [], [("A_xt", p)])
            k.act(junk[:], xt[:, p, :], AF.Square, [("A_xt", p)], ["A_junk", ("A_ss", p)], accum=ss[:, p:p + 1])
            k.act(rstd[:, p:p + 1], ss[:, p:p + 1], AF.Ln, [("A_ss", p)], [("A_rstd", p)], scale=1.0 / D, bias=EPS)
            k.act(rstd[:, p:p + 1], rstd[:, p:p + 1], AF.Exp, [("A_rstd", p)], [("A_rstd", p)], scale=-0.5)
            k.stt("dve", xs[:, p, :], xt[:, p, :], rstd[:, p:p + 1], gb[:], ALU.mult, ALU.mult,
                  [("A_xt", p), ("A_rstd", p), "A_gb"], [("A_xs", p)])
            for c in range(8):
                k.tr(pt[:, p, c, :], xs[:, p, c * 128:(c + 1) * 128], identb[:], [("A_xs", p), "identb"], [("A_pt", p)])
            k.cp("act", xnT[:, :, b * 128:(b + 1) * 128], pt[:, p, :, :], [("A_pt", p)], [("xnT", b)])
        s.barrier()


NCV = 4


def convert_step(k, T, cvf, cvb, r):
    def src_of(q):
        tile_, which = q // 2, q % 2
        rs = slice(tile_ * 128, (tile_ + 1) * 128)
        return (T["peer_u"] if which == 0 else T["peer_v"])[rs, :], T["uv_s"][rs, which * 1024:(which + 1) * 1024]
    if r < 256:
        fb = r % NCV
        k.dma("sp", cvf[:, fb, :], src_of(r)[0], "cvf%d" % fb, [], [("cvf", fb)])
    q = r - (NCV - 1)
    if 0 <= q < 256:
        fb, cb = q % NCV, q % 2
        k.cp("act", cvb[:, cb, :], cvf[:, fb, :], [("cvf", fb)], [("cvb", cb)])
        k.dma("sp", src_of(q)[1], cvb[:, cb, :], "cvst%d" % cb, [("cvb", cb)], ["uv_s"])


def phase_BC(k, T, sbt, pst, xnT, identb, identf, load_w):
    nc, s = k.nc, k.s
    with ExitStack() as st:
        wB = sbt(st, "wB", [128, 8, 452], BF16)
        wC = sbt(st, "wC", [128, 8, 768], BF16)
        kT = sbt(st, "kT", [64, S], BF16)
        iknT = sbt(st, "iknT", [64, S], BF16)
        v_aug = sbt(st, "v_aug", [128, NBLK, 65], BF16)
        iws = sbt(st, "iws", [128, NBLK, 4], F32)
        gI = sbt(st, "gI", [128, 64], F32)
        bI = sbt(st, "bI", [128, 64], F32)
        cmask = sbt(st, "cmask", [128, 128], F32)
        e65 = sbt(st, "e65", [65, 64], F32)
        self_f = sbt(st, "sel_f", [64, 256], F32)
        sel_b = sbt(st, "sel_b", [64, 256], BF16)
        i8 = sbt(st, "i8", [128, 8, 128], BF16)
        bnd = sbt(st, "bnd", [128, 2, 8, 128], BF16)
        ikf = sbt(st, "ikf", [128, 64], F32)
        ikb = sbt(st, "ikb", [128, 64], BF16)
        stats = sbt(st, "stats", [128, 6], F32)
        mv = sbt(st, "mv", [128, 2], F32)
        irs = sbt(st, "irs", [128, 1], F32)
        qT = sbt(st, "qT", [64, 2, 8, 128], BF16)
        iqT = sbt(st, "iqT", [64, 2, 4, 128], BF16)
        score = sbt(st, "score", [128, S], F32)
        rl = sbt(st, "rl", [128, 2, 4, 256], F32)
        selneg = sbt(st, "selneg", [128, 2, S], BF16)
        cj = sbt(st, "cj", [128, S], BF16)
        lo = sbt(st, "lo", [128, 1], F32)
        hi = sbt(st, "hi", [128, 1], F32)
        mid = sbt(st, "mid", [128, 1], F32)
        cnt = sbt(st, "cnt", [128, 1], F32)
        wtab = sbt(st, "wtab", [128, NROUNDS + 2], F32)
        pw2 = sbt(st, "pw2", [128, NROUNDS + 2], F32)
        pT = sbt(st, "pT", [128, 2, 2, 512], BF16)
        oT = sbt(st, "oT", [65, 2, 512], F32)
        rb = sbt(st, "rb", [64, 2, 512], F32)
        ybn = sbt(st, "ybn", [64, 8, 128], BF16)
        ybo = sbt(st, "ybo", [128, 4, 128], BF16)
        pX = pst(st, "pX", [128, 2, 512], F32)
        pL = pst(st, "pL", [128, 2, 2, 512], F32)
        pO = pst(st, "pO", [128, 2, 512], F32)

        load_w(wB[:], "wB", T["w_in"][:, COL["ak"]:COL["ak"] + 452], 8, 452)
        load_w(wC[:, :, 0:512], "wC", T["w_in"][:, COL["aq"]:COL["aq"] + 512], 8, 512)
        load_w(wC[:, :, 512:768], "wC", T["w_in"][:, COL["iq"]:COL["iq"] + 256], 8, 256)
        k.dma("sp", gI[:], T["idx_k_norm_g"].partition_broadcast(128), "c0", [], ["gI"])
        k.dma("sp", bI[:], T["idx_k_norm_b"].partition_broadcast(128), "c0", [], ["bI"])
        k.dma("sp", cmask[:], T["c_cmask"], "c0", [], ["cmask"])
        k.dma("sp", e65[:], T["c_e65"], "c0", [], ["e65"])
        k.dma("sp", self_f[:], T["c_sel"], "c0", [], ["sel_f"])
        k.cp("dve", sel_b[:], self_f[:], ["sel_f"], ["sel_b"])
        for h in range(8):
            k.cp("dve", i8[:, h, :], identb[:], ["identb"], ["i8"])
        with ExitStack() as st2:
            bstage = sbt(st2, "bstage", [128, 3, 8, 128], F32)
            k.dma("sp", bstage[:, 0], T["c_bd"], "c0", [], ["bstage"])
            k.dma("sp", bstage[:, 1], T["c_bp"], "c0", [], ["bstage"])
            k.dma("sp", bstage[:, 2], T["c_b31"], "c0", [], ["bstage"])
            for j in range(2):
                k.tt("dve", bstage[:, j], bstage[:, j], bstage[:, 2], ALU.subtract, ["bstage"], ["bstage"])
                k.tsc("dve", bnd[:, j], bstage[:, j], 8.0, None, ALU.mult, None, ["bstage"], ["bnd"])
            s.barrier()
        k.memset("dve", v_aug[:, :, 64:65], 1.0, ["v_aug"])
        for r_ in range(NROUNDS + 2):
            k.memset("dve", pw2[:, r_:r_ + 1], 2.0 ** (-r_), ["pw2"])

        stage = k.cfg.get("bc_stage", 9)
        for b in range(k.cfg.get("nblk_b", NBLK) if stage >= 1 else 0):
            tb = slice(b * 128, (b + 1) * 128)
            for c in range(8):
                k.mm(pX[:, 0, 0:452], xnT[:, c, tb], wB[:, c, :], c == 0, c == 7, [("xnT", b), "wB"], [("pX", 0)])
            bv = k.cfg.get("b_var", 9)
            k.cp("act", v_aug[:, b, 0:64], pX[:, 0, 64:128], [("pX", 0)], ["v_aug"])
            if bv >= 2:
                k.cp("act", ikf[:], pX[:, 0, 384:448], [("pX", 0)], ["ikf"])
                k.tsc("dve", iws[:, b, :], pX[:, 0, 448:452], 0.0625, None, ALU.mult, None, [("pX", 0)], ["iws"])
            if bv >= 3:
                k.gen("dve", lambda e: e.bn_stats(out=stats[:], in_=ikf[:]), ["ikf"], ["stats"])
                k.gen("dve", lambda e: e.bn_aggr(out=mv[:], in_=stats[:]), ["stats"], ["mv"])
            if bv >= 4:
                k.act(irs[:], mv[:, 1:2], AF.Ln, ["mv"], ["irs"], bias=EPS)
                k.act(irs[:], irs[:], AF.Exp, ["irs"], ["irs"], scale=-0.5)
                k.tsc("dve", ikf[:], ikf[:], mv[:, 0:1], None, ALU.subtract, None, ["ikf", "mv"], ["ikf"])
                k.tsc("dve", ikf[:], ikf[:], irs[:, 0:1], None, ALU.mult, None, ["ikf", "irs"], ["ikf"])
                k.tt("dve", ikf[:], ikf[:], gI[:], ALU.mult, ["ikf", "gI"], ["ikf"])
                k.tt("dve", ikf[:], ikf[:], bI[:], ALU.add, ["ikf", "bI"], ["ikf"])
            if bv >= 5:
                ptk = pX[0:64, 1, 0:128]
                k.tr(ptk, ikf[:], identf[:], ["ikf", "identf"], [("pX", 1)])
                k.cp("act", iknT[:, tb], ptk, [("pX", 1)], [("iknT", b)])
        for c4 in range(8 if stage >= 2 else 0):
            ts_ = slice(c4 * 512, (c4 + 1) * 512)
            for c in range(8):
                k.mm(pX[0:64, 0, :], wB[:, c, 0:64], xnT[:, c, ts_], c == 0, c == 7,
                     [("xnT", 4 * c4, 4 * c4 + 4), "wB"], [("pX", 0)])
            k.cp("act", kT[:, ts_], pX[0:64, 0, :], [("pX", 0)], [("kT", 4 * c4, 4 * c4 + 4)])

        def c123(i):
            p = i % 2
            tb = slice(i * 128, (i + 1) * 128)
            L = (i + 1) * 128
            pq = pX[0:64, :, :].rearrange("p a (h t) -> p (a h) t", t=128)
            for h in range(8):
                for c in range(8):
                    k.mm(pq[:, h, :], wC[:, c, h * 64:(h + 1) * 64], xnT[:, c, tb], c == 0, c == 7,
                         [("xnT", i), "wC"], [("pX", h // 4)])
            k.cp("act", qT[:, p], pq, [("pX", 0), ("pX", 1)], [("qT", p)])
            for h in range(4):
                for c in range(8):
                    k.mm(pq[:, h, :], wC[:, c, 512 + h * 64:512 + (h + 1) * 64], xnT[:, c, tb], c == 0, c == 7,
                         [("xnT", i), "wC"], [("pX", 0)])
            k.cp("act", iqT[:, p], pq[:, 0:4, :], [("pX", 0)], [("iqT", p)])
            ps4 = pX[:].rearrange("p a (j w) -> p (a j) w", w=256)
            nch = (L + 255) // 256
            for ch in range(nch):
                s0 = ch * 256
                wk = min(256, L - s0)
                rp = ch % 2
                for j in range(4):
                    k.mm(ps4[:, j, 0:wk], iqT[:, p, j, :], iknT[:, s0:s0 + wk], True, True,
                         [("iqT", p), ("iknT", s0 // 128, (s0 + wk) // 128)], [("pX", j // 2)])
                k.act(rl[:, rp, :, 0:wk], ps4[:, :, 0:wk], AF.Relu, [("pX", 0), ("pX", 1)], [("rl", rp)])
                k.tsc("dve", score[:, s0:s0 + wk], rl[:, rp, 0, 0:wk], iws[:, i, 0:1], None, ALU.mult, None,
                      [("rl", rp), "iws"], ["score"])
                for j in range(1, 4):
                    k.stt("dve", score[:, s0:s0 + wk], rl[:, rp, j, 0:wk], iws[:, i, j:j + 1], score[:, s0:s0 + wk],
                          ALU.mult, ALU.add, [("rl", rp), "iws", "score"], ["score"])
            k.tred("dve", hi[:], score[:, 0:L], ALU.max, ["score"], ["hi"])
            k.tred("dve", lo[:], score[:, 0:L], ALU.min, ["score"], ["lo"])
            k.tsc("dve", lo[:], lo[:], -1.0, None, ALU.add, None, ["lo"], ["lo"])
            k.tt("dve", score[:, L - 128:L], score[:, L - 128:L], cmask[:], ALU.add, ["score", "cmask"], ["score"])
            if L > 256:
                k.tsc("dve", mid[:], lo[:], hi[:, 0:1], 0.5, ALU.add, ALU.mult, ["lo", "hi"], ["mid"])
                k.tsc("dve", hi[:], hi[:], lo[:, 0:1], 0.5, ALU.subtract, ALU.mult, ["lo", "hi"], ["hi"])
                k.tsc("dve", wtab[:], pw2[:], hi[:, 0:1], None, ALU.mult, None, ["pw2", "hi"], ["wtab"])
                for r_ in range(NROUNDS):
                    k.tsc("dve", cj[:, 0:L], score[:, 0:L], mid[:, 0:1], None, ALU.is_gt, ALU.add,
                          ["score", "mid"], ["cj", "cnt"], accum=cnt[:])
                    k.tsc("dve", cnt[:], cnt[:], 255.5, wtab[:, r_:r_ + 1], ALU.is_gt, ALU.mult, ["cnt", "wtab"], ["cnt"])
                    k.stt("dve", mid[:], mid[:], wtab[:, r_ + 1:r_ + 2], cnt[:], ALU.subtract, ALU.add,
                          ["mid", "wtab", "cnt"], ["mid"])
                k.tsc("dve", lo[:], mid[:], wtab[:, NROUNDS:NROUNDS + 1], None, ALU.subtract, None, ["mid", "wtab"], ["lo"])
            k.tsc("dve", selneg[:, p, 0:L], score[:, 0:L], lo[:, 0:1], NEG, ALU.is_le, ALU.mult,
                  ["score", "lo"], [("selneg", p)])

        def c4(i):
            p = i % 2

            def qk(j):
                lb_ = j % 2
                sj = slice(j * 128, (j + 1) * 128)
                near = (j == i) or (j == i - 1)
                for half in range(2):
                    hs = slice(4 * half, 4 * half + 4)
                    k.mm(pL[:, lb_, half, :], kT[:, sj], qT[:, p, hs, :], True, False,
                         [("kT", j), ("qT", p)], [("pL", lb_)])
                    k.mm(pL[:, lb_, half, :], selneg[:, p, sj], i8[:, hs, :], False, not near,
                         [("selneg", p), "i8"], [("pL", lb_)])
                    if near:
                        k.mm(pL[:, lb_, half, :], identb[:], bnd[:, 0 if j == i else 1, hs, :], False, True,
                             ["identb", "bnd"], [("pL", lb_)])

            qk(0)
            for j in range(i + 1):
                lb_ = j % 2
                if j + 1 <= i:
                    qk(j + 1)
                k.act(pT[:, lb_, :, :], pL[:, lb_, :, :], AF.Exp, [("pL", lb_)], [("pT", lb_)], scale=0.125)
                for half in range(2):
                    k.mm(pO[0:65, half, :], v_aug[:, j, :], pT[:, lb_, half, :], j == 0, j == i,
                         ["v_aug", ("pT", lb_)], ["pO"])
            k.cp("act", oT[:], pO[0:65, :, :], ["pO"], ["oT"])
            for half in range(2):
                k.mm(pX[0:64, half, :], e65[:], oT[:, half, :], True, True,
                     ["e65", "oT"], [("pX", half)])
            k.act(rb[:], pX[0:64, :, :], AF.Ln, [("pX", 0), ("pX", 1)], ["rb"])
            k.act(rb[:], rb[:], AF.Exp, ["rb"], ["rb"], scale=-1.0)
            k.tt("dve", ybn[:].rearrange("p (a h) t -> p a (h t)", a=2), oT[0:64, :, :], rb[:], ALU.mult, ["oT", "rb"], ["ybn"])
            yv = ybn[:].rearrange("p (c two) t -> p two c t", two=2)
            k.mm(pX[:, 0, :], sel_b[:, 0:128], yv[:, 0], True, False, ["sel_b", "ybn"], [("pX", 0)])
            k.mm(pX[:, 0, :], sel_b[:, 128:256], yv[:, 1], False, True, ["sel_b", "ybn"], [("pX", 0)])
            k.cp("act", ybo[:], pX[:, 0, :], [("pX", 0)], ["ybo"])
            k.dma("sp", T["yb_s"][:, :, i * 128:(i + 1) * 128], ybo[:], "ybo", ["ybo"], [])

        nblk = k.cfg.get("nblk_c", NBLK)
        if stage >= 3:
            c123(0)
        for i in range(nblk if stage >= 3 else 0):
            if i + 1 < nblk:
                c123(i + 1)
            if stage >= 4:
                c4(i)
        s.barrier()


def phase_D(k, T, sbt, pst, xnT, identb, identf, load_w):
    nc, s = k.nc, k.s
    NG = k.cfg.get("ngroups_d", 8)
    with ExitStack() as st:
        wD = sbt(st, "wD", [128, 8, 2048], BF16)
        lbs = sbt(st, "lbs", [128, 2, 4], F32)
        lbT = sbt(st, "lbT", [128, 4], F32)
        omlT = sbt(st, "omlT", [128, 4], F32)
        gnT = sbt(st, "gnT", [128, 4], F32)
        tri = sbt(st, "tri", [64, 64], F32)
        cmk = sbt(st, "cmk", [128, 512], F32)
        ones_b = sbt(st, "ones_b", [128, 128], BF16)
        v_sb = sbt(st, "v_sb", [64, 2, 8, 512], BF16)
        state_f = sbt(st, "state_f", [128, 4, 128], F32)
        state_b = sbt(st, "state_b", [128, 4, 128], BF16)
        NT = 10
        tf = sbt(st, "tf", [128, 2, NT, 512], F32)
        qeT = sbt(st, "qeT", [128, 2, 512], BF16)
        keT = sbt(st, "keT", [128, 2, 512], BF16)
        k2T = sbt(st, "k2T", [128, 2, 512], F32)
        ebl = sbt(st, "ebl", [128, 2, 8], F32)
        at_sb = sbt(st, "at_sb", [64, 2, 64], BF16)
        k2_sb = sbt(st, "k2_sb", [64, 2, 128], BF16)
        sq = sbt(st, "sq", [128, 512], BF16)
        yo = sbt(st, "yo", [128, 2, 512], BF16)
        cvf = sbt(st, "cvf", [128, NCV, 1024], F32)
        cvb = sbt(st, "cvb", [128, 2, 1024], BF16)
        cstep = {"r": 0}
        pP = pst(st, "pP", [128, 3, 512], F32)
        pOo = pst(st, "pOo", [128, 2, 512], F32)
        pM = pst(st, "pM", [128, 2, 512], F32)
        pV = pst(st, "pV", [128, 512], F32)

        for nm, c0 in (("hq", 0), ("hf", 512), ("hi", 1024), ("hog", 1536)):
            load_w(wD[:, :, c0:c0 + 512], "wD", T["w_in"][:, COL[nm]:COL[nm] + 512], 8, 512)
        k.dma("sp", lbs[:], T["hg_lb"].rearrange("r (h p) -> p r h", p=128), "c0", [], ["lbs"])
        k.dma("sp", gnT[:], T["hg_norm"].rearrange("o (h p) -> p (o h)", p=128), "c0", [], ["gnT"])
        k.dma("sp", tri[:], T["c_tri"], "c0", [], ["tri"])
        k.dma("sp", cmk[:], T["c_chunkmask"], "c0", [], ["cmk"])
        k.memset("dve", ones_b[:], 1.0, ["ones_b"])
        k.memset("dve", state_f[:], 0.0, [("state_f", 0, 4)])
        k.memset("dve", state_b[:], 0.0, [("state_b", 0, 4)])
        k.tt("dve", lbT[:], lbs[:, 1, :], lbs[:, 0, :], ALU.subtract, ["lbs"], ["lbT"])
        k.act(lbT[:], lbT[:], AF.Exp, ["lbT"], ["lbT"])
        k.tsc("dve", lbT[:], lbT[:], 1.0, None, ALU.add, None, ["lbT"], ["lbT"])
        k.recip(lbT[:], lbT[:], ["lbT"], ["lbT"])
        k.tsc("dve", omlT[:], lbT[:], -1.0, 1.0, ALU.mult, ALU.add, ["lbT"], ["omlT"])

        def vproj(g):
            gp = g % 2
            xk = ("xnT", 4 * g, 4 * g + 4)
            for c in range(8):
                tok = slice(g * 512 + c * 64, g * 512 + (c + 1) * 64)
                for kc in range(8):
                    k.mm(pV[0:64, :], xnT[:, kc, tok], wD[:, kc, 1024:1536], kc == 0, kc == 7, [xk, "wD"], ["pV"])
                k.cp("act", v_sb[:, gp, c, :], pV[0:64, :], ["pV"], [("v_sb", gp)])

        def prologue(g, h):
            g5 = slice(g * 512, (g + 1) * 512)
            xk = ("xnT", 4 * g, 4 * g + 4)
            u = (g * 4 + h) % 2
            t = lambda i, u=u: tf[:, u, i, :]
            tk = lambda i, u=u: ("tf", u * NT + i)
            for j, c0 in enumerate((0, 512, 1536)):
                for kc in range(8):
                    k.mm(pP[:, j, :], wD[:, kc, c0 + h * 128:c0 + (h + 1) * 128], xnT[:, kc, g5], kc == 0, kc == 7,
                         [xk, "wD"], [("pP", j)])
                    yield
            k.act(t(0), pP[:, 1, :], AF.Exp, [("pP", 1)], [tk(0)], scale=-1.0)
            yield
            k.act(t(0), t(0), AF.Ln, [tk(0)], [tk(0)], bias=1.0)
            yield
            k.act(t(0), t(0), AF.Exp, [tk(0)], [tk(0)], scale=-1.0)
            yield
            k.tsc("dve", t(0), t(0), omlT[:, h:h + 1], lbT[:, h:h + 1], ALU.mult, ALU.add,
                  [tk(0), "omlT", "lbT"], [tk(0)])
            yield
            k.act(t(1), t(0), AF.Ln, [tk(0)], [tk(1)])
            yield
            k.tsc("dve", t(2), t(0), -1.0, 1.0, ALU.mult, ALU.add, [tk(0)], [tk(2)])
            yield
            k.gen("dve", lambda e, o=t(3), d0=cmk[:], d1=t(1): e.tensor_tensor_scan(
                out=o, data0=d0, data1=d1, initial=0.0, op0=ALU.mult, op1=ALU.add),
                ["cmk", tk(1)], [tk(3)])
            yield
            k.act(t(4), t(3), AF.Exp, [tk(3)], [tk(4)])
            yield
            k.act(t(5), t(3), AF.Exp, [tk(3)], [tk(5)], scale=-1.0)
            yield
            for c in range(8):
                cs = slice(c * 64, (c + 1) * 64)
                k.act(tf[:, u, 6, cs], tf[:, u, 3, cs], AF.Exp, [tk(3)], [tk(6)], scale=-1.0,
                      bias=tf[:, u, 3, c * 64 + 63:c * 64 + 64])
                yield
            bl = tf[:, u, 3, :].rearrange("p (c t) -> p c t", t=64)[:, :, 63]
            k.act(ebl[:, u, :], bl, AF.Exp, [tk(3)], [("ebl", u)])
            yield
            k.tt("dve", k2T[:, u, :], t(2), t(6), ALU.mult, [tk(2), tk(6)], [("k2T", u)])
            yield
            k.tt("dve", keT[:, u, :], t(2), t(5), ALU.mult, [tk(2), tk(5)], [("keT", u)])
            yield
            k.act(t(7), pP[:, 0, :], AF.Exp, [("pP", 0)], [tk(7)], scale=-1.0)
            yield
            k.act(t(7), t(7), AF.Ln, [tk(7)], [tk(7)], bias=1.0)
            yield
            k.act(t(7), t(7), AF.Exp, [tk(7)], [tk(7)], scale=-1.0)
            yield
            k.tt("dve", t(7), t(7), pP[:, 0, :], ALU.mult, [tk(7), ("pP", 0)], [tk(7)])
            yield
            k.tt("dve", qeT[:, u, :], t(7), t(4), ALU.mult, [tk(7), tk(4)], [("qeT", u)])
            yield
            k.act(t(8), pP[:, 2, :], AF.Exp, [("pP", 2)], [tk(8)], scale=-1.0)
            yield
            k.act(t(8), t(8), AF.Ln, [tk(8)], [tk(8)], bias=1.0)
            yield
            k.act(t(8), t(8), AF.Exp, [tk(8)], [tk(8)], scale=-1.0)
            yield
            k.tt("dve", t(8), t(8), pP[:, 2, :], ALU.mult, [tk(8), ("pP", 2)], [tk(8)])

        def chunks(g, h, pg=None, qg=None):
            def pull(n=1):
                if qg is not None:
                    next(qg, None)
                if pg is not None:
                    for _ in range(n):
                        next(pg, None)
            gp = g % 2
            u = (g * 4 + h) % 2
            hs = slice(h * 128, (h + 1) * 128)
            for c in range(8):
                cs = slice(c * 64, (c + 1) * 64)
                a2 = c % 2
                k.mm(pM[0:64, 0, 0:64], keT[:, u, cs], qeT[:, u, cs], True, True,
                     [("keT", u), ("qeT", u)], [("pM", 0)])
                k.tt("dve", at_sb[:, a2, :], pM[0:64, 0, 0:64], tri[:], ALU.mult, [("pM", 0), "tri"], [("at_sb", a2)])
                pull(2)
                k.mm(pOo[:, u, cs], state_b[:, h, :], qeT[:, u, cs], True, False,
                     [("state_b", h), ("qeT", u)], [("pOo", u)])
                k.mm(pOo[:, u, cs], v_sb[:, gp, c, hs], at_sb[:, a2, :], False, True,
                     [("v_sb", gp), ("at_sb", a2)], [("pOo", u)])
                k.tr(pM[0:64, 0, 128:256], k2T[:, u, cs], identf[:], [("k2T", u), "identf"], [("pM", 0)])
                k.cp("act", k2_sb[:, a2, :], pM[0:64, 0, 128:256], [("pM", 0)], [("k2_sb", a2)])
                pull(2)
                k.mm(pM[:, 1, 0:128], k2_sb[:, a2, :], v_sb[:, gp, c, hs], True, True,
                     [("k2_sb", a2), ("v_sb", gp)], [("pM", 1)])
                k.stt("dve", state_f[:, h, :], state_f[:, h, :], ebl[:, u, c:c + 1], pM[:, 1, 0:128],
                      ALU.mult, ALU.add, [("state_f", h), ("ebl", u), ("pM", 1)], [("state_f", h)])
                k.cp("act", state_b[:, h, :], state_f[:, h, :], [("state_f", h)], [("state_b", h)])
                pull(2)
                if k.cfg.get("convert", True) and NG == 8:
                    convert_step(k, T, cvf, cvb, cstep["r"])
                    cstep["r"] += 1

        def post(g, h):
            g5 = slice(g * 512, (g + 1) * 512)
            u = (g * 4 + h) % 2
            t = lambda i, u=u: tf[:, u, i, :]
            tk = lambda i, u=u: ("tf", u * NT + i)
            k.act(sq[:], pOo[:, u, :], AF.Square, [("pOo", u)], ["sq"])
            yield
            k.mm(pV[:], ones_b[:], sq[:], True, True, ["ones_b", "sq"], ["pV"])
            yield
            k.act(t(9), pV[:], AF.Ln, ["pV"], [tk(9)], scale=1.0 / 128, bias=EPS)
            yield
            k.act(t(9), t(9), AF.Exp, [tk(9)], [tk(9)], scale=-0.5)
            yield
            k.stt("dve", t(9), pOo[:, u, :], gnT[:, h:h + 1], t(9), ALU.mult, ALU.mult,
                  [("pOo", u), "gnT", tk(9)], [tk(9)])
            yield
            k.tt("dve", yo[:, u, :], t(9), t(8), ALU.mult, [tk(9), tk(8)], [("yo", u)])
            yield
            k.dma("sp", T["ya_s"][:, h, g5], yo[:, u, :], "yo%d" % u, [("yo", u)], [])

        units = [(g, h) for g in range(NG) for h in range(4)]
        vproj(0)
        for _ in prologue(*units[0]):
            pass
        qg = None
        for n, (g, h) in enumerate(units):
            pg = None
            if n + 1 < len(units):
                g1, h1 = units[n + 1]
                if h1 == 0:
                    vproj(g1)
                pg = prologue(g1, h1)
            chunks(g, h, pg, qg)
            if qg is not None:
                for _ in qg:
                    pass
            if pg is not None:
                for _ in pg:
                    pass
            qg = post(g, h)
        for _ in qg:
            pass
        if k.cfg.get("convert", True) and NG == 8:
            while cstep["r"] < 256 + NCV:
                convert_step(k, T, cvf, cvb, cstep["r"])
                cstep["r"] += 1
        s.barrier()


def phase_E1(k, T, sbt, pst, xnT, load_w):
    nc, s = k.nc, k.s
    NG = k.cfg.get("ngroups_e1", 8)
    with ExitStack() as st:
        wG = sbt(st, "wG", [128, 8, 2048], BF16)
        wU = sbt(st, "wU", [128, 2, 4, 1024], BF16)
        yab = sbt(st, "yab", [128, 2, 2, 4, 512], BF16)
        sg = sbt(st, "sg", [128, 2, 2, 512], F32)
        tmp = sbt(st, "e1tmp", [128, 2, 2, 512], F32)
        hTg = sbt(st, "hTg", [128, 2, 8, 512], BF16)
        pE = pst(st, "pE", [128, 2, 4, 512], F32)
        load_w(wG[:, :, 0:1024], "wG", T["w_in"][:, COL["ga"]:COL["ga"] + 1024], 8, 1024)
        load_w(wG[:, :, 1024:2048], "wG", T["w_in"][:, COL["gb"]:COL["gb"] + 1024], 8, 1024)
        load_w(wU[:, 0], "wU", T["w_up_a"], 4, 1024)
        load_w(wU[:, 1], "wU", T["w_up_b"], 4, 1024)
        for g in range(NG):
            gp = g % 2
            g5 = slice(g * 512, (g + 1) * 512)
            xk = ("xnT", 4 * g, 4 * g + 4)
            k.dma("sp", yab[:, gp, 0], T["ya_s"][:, :, g5], "yab%d" % gp, [], [("yab", gp)])
            k.dma("sp", yab[:, gp, 1], T["yb_s"][:, :, g5], "yab%d" % gp, [], [("yab", gp)])
            for nn in range(8):
                pb = nn % 2
                ns = slice(nn * 128, (nn + 1) * 128)
                for ab in range(2):
                    for kc in range(8):
                        k.mm(pE[:, pb, ab, :], wG[:, kc, ab * 1024 + nn * 128:ab * 1024 + (nn + 1) * 128], xnT[:, kc, g5],
                             kc == 0, kc == 7, [xk, "wG"], [("pE", pb * 4 + ab)])
                    for c4 in range(4):
                        k.mm(pE[:, pb, 2 + ab, :], wU[:, ab, c4, ns], yab[:, gp, ab, c4, :], c4 == 0, c4 == 3,
                             [("yab", gp), "wU"], [("pE", pb * 4 + 2 + ab)])
                for ab in range(2):
                    k.act(sg[:, pb, ab, :], pE[:, pb, ab, :], AF.Exp, [("pE", pb * 4 + ab)], [("sg", pb * 2 + ab)], scale=-1.0)
                    k.act(sg[:, pb, ab, :], sg[:, pb, ab, :], AF.Ln, [("sg", pb * 2 + ab)], [("sg", pb * 2 + ab)], bias=1.0)
                    k.act(sg[:, pb, ab, :], sg[:, pb, ab, :], AF.Exp, [("sg", pb * 2 + ab)], [("sg", pb * 2 + ab)], scale=-1.0)
                    k.tt("dve", tmp[:, pb, ab, :], sg[:, pb, ab, :], pE[:, pb, 2 + ab, :], ALU.mult,
                         [("sg", pb * 2 + ab), ("pE", pb * 4 + 2 + ab)], [("e1tmp", pb * 2 + ab)])
                k.tt("dve", hTg[:, gp, nn, :], tmp[:, pb, 0, :], tmp[:, pb, 1, :], ALU.add,
                     [("e1tmp", pb * 2), ("e1tmp", pb * 2 + 1)], [("hTg", gp)])
            k.dma("sp", T["hT_s"][:, :, g5], hTg[:, gp], "hTg%d" % gp, [("hTg", gp)], [])
        s.barrier()


def peer_routing_alloc(sbt, st, pfx="r"):
    B = {}
    B["vals"] = sbt(st, pfx + "vals", [128, 16, 16], F32)
    B["idxs"] = sbt(st, pfx + "idxs", [128, 16, 16], U32)
    B["idxf"] = sbt(st, pfx + "idxf", [128, 16, 16], F32)
    B["s2"] = sbt(st, pfx + "s2", [128, 128], F32)
    B["cand"] = sbt(st, pfx + "cand", [128, 8, 256], F32)
    B["cand2"] = sbt(st, pfx + "cand2", [128, 8, 256], F32)
    B["tops"] = sbt(st, pfx + "tops", [128, 8, 16], F32)
    B["pos"] = sbt(st, pfx + "pos", [128, 8, 16], U32)
    B["ipos"] = sbt(st, pfx + "ipos", [128, 8, 16], U32)
    B["jpos"] = sbt(st, pfx + "jpos", [128, 8, 16], U32)
    B["iposf"] = sbt(st, pfx + "iposf", [128, 8, 16], F32)
    B["jposf"] = sbt(st, pfx + "jposf", [128, 8, 16], F32)
    B["eq"] = sbt(st, pfx + "eq", [128, 8, 16, 16], F32)
    B["sel1"] = sbt(st, pfx + "sel1", [128, 8, 16], F32)
    B["sel2"] = sbt(st, pfx + "sel2", [128, 8, 16], F32)
    B["gsum"] = sbt(st, pfx + "gsum", [128, 8], F32)
    return B


def peer_routing(k, B, s_sb, s_key, iota16, eidx, eidx_key, gw, gw_key, pfx="r"):
    vals, idxs, idxf, s2, cand, cand2 = B["vals"], B["idxs"], B["idxf"], B["s2"], B["cand"], B["cand2"]
    tops, pos, ipos, jpos, iposf, jposf = B["tops"], B["pos"], B["ipos"], B["jpos"], B["iposf"], B["jposf"]
    eq, sel1, sel2, gsum = B["eq"], B["sel1"], B["sel2"], B["gsum"]
    K_ = pfx
    for l in range(16):
        k.gen("dve", lambda e, l=l: e.max(out=vals[:, l, 0:8], in_=s_sb[:, l, :]), [s_key], [K_ + "vals"])
        k.gen("dve", lambda e, l=l: e.max_index(out=idxs[:, l, 0:8], in_max=vals[:, l, 0:8], in_values=s_sb[:, l, :]),
              [s_key, K_ + "vals"], [K_ + "idxs"])
        k.gen("dve", lambda e, l=l: e.match_replace(out=s2[:], in_to_replace=vals[:, l, 0:8], in_values=s_sb[:, l, :],
                                                    imm_value=-1e30), [s_key, K_ + "vals"], [K_ + "s2"])
        k.gen("dve", lambda e, l=l: e.max(out=vals[:, l, 8:16], in_=s2[:]), [K_ + "s2"], [K_ + "vals"])
        k.gen("dve", lambda e, l=l: e.max_index(out=idxs[:, l, 8:16], in_max=vals[:, l, 8:16], in_values=s2[:]),
              [K_ + "s2", K_ + "vals"], [K_ + "idxs"])
        yield
    k.cp("dve", idxf[:], idxs[:], [K_ + "idxs"], [K_ + "idxf"])
    v4 = vals[:].rearrange("p (h two) i -> p h two i", two=2)
    x4 = idxf[:].rearrange("p (h two) i -> p h two i", two=2)
    c4 = cand[:].rearrange("p h (i j) -> p h i j", j=16)
    k.tt("dve", c4, v4[:, :, 0, :].unsqueeze(3).broadcast_to([128, 8, 16, 16]),
         v4[:, :, 1, :].unsqueeze(2).broadcast_to([128, 8, 16, 16]), ALU.add, [K_ + "vals"], [K_ + "cand"])
    for h in range(8):
        k.gen("dve", lambda e, h=h: e.max(out=tops[:, h, 0:8], in_=cand[:, h, :]), [K_ + "cand"], [K_ + "tops"])
        k.gen("dve", lambda e, h=h: e.max_index(out=pos[:, h, 0:8], in_max=tops[:, h, 0:8], in_values=cand[:, h, :]),
              [K_ + "cand", K_ + "tops"], [K_ + "pos"])
        k.gen("dve", lambda e, h=h: e.match_replace(out=cand2[:, h, :], in_to_replace=tops[:, h, 0:8],
                                                    in_values=cand[:, h, :], imm_value=-1e30),
              [K_ + "cand", K_ + "tops"], [K_ + "cand2"])
        k.gen("dve", lambda e, h=h: e.max(out=tops[:, h, 8:16], in_=cand2[:, h, :]), [K_ + "cand2"], [K_ + "tops"])
        k.gen("dve", lambda e, h=h: e.max_index(out=pos[:, h, 8:16], in_max=tops[:, h, 8:16], in_values=cand2[:, h, :]),
              [K_ + "cand2", K_ + "tops"], [K_ + "pos"])
        yield
    k.tsc("dve", ipos[:], pos[:], 4, None, ALU.logical_shift_right, None, [K_ + "pos"], [K_ + "ipos"])
    k.tsc("dve", jpos[:], pos[:], 15, None, ALU.bitwise_and, None, [K_ + "pos"], [K_ + "jpos"])
    k.cp("dve", iposf[:], ipos[:], [K_ + "ipos"], [K_ + "iposf"])
    k.cp("dve", jposf[:], jpos[:], [K_ + "jpos"], [K_ + "jposf"])
    io4 = iota16[:].unsqueeze(1).unsqueeze(1).broadcast_to([128, 8, 16, 16])
    for (pf_, xi, sel) in ((iposf, 0, sel1), (jposf, 1, sel2)):
        k.tt("dve", eq[:], io4, pf_[:].unsqueeze(3).broadcast_to([128, 8, 16, 16]), ALU.is_equal,
             ["iota16", K_ + "iposf", K_ + "jposf"], [K_ + "eq"])
        k.tt("dve", eq[:], eq[:], x4[:, :, xi, :].unsqueeze(2).broadcast_to([128, 8, 16, 16]), ALU.mult,
             [K_ + "eq", K_ + "idxf"], [K_ + "eq"])
        k.tred("dve", sel[:], eq[:], ALU.add, [K_ + "eq"], [K_ + "sel"])
    yield
    k.stt("dve", sel1[:], sel1[:], 128.0, sel2[:], ALU.mult, ALU.add, [K_ + "sel"], [K_ + "sel"])
    k.cp("dve", eidx.rearrange("p (h i) -> p h i", i=16), sel1[:], [K_ + "sel"], [eidx_key])
    k.tt("dve", gw, tops[:], tops[:, :, 0:1].broadcast_to([128, 8, 16]), ALU.subtract, [K_ + "tops"], [gw_key])
    k.act(gw, gw, AF.Exp, [gw_key], [gw_key])
    k.tred("dve", gsum[:], gw, ALU.add, [gw_key], [K_ + "gsum"])
    k.recip(gsum[:], gsum[:], [K_ + "gsum"], [K_ + "gsum"])
    k.tt("dve", gw, gw, gsum[:].unsqueeze(2).broadcast_to([128, 8, 16]), ALU.mult, [gw_key, K_ + "gsum"], [gw_key])


def phase_E2(k, T, sbt, pst, identb, identf, load_w):
    nc, s = k.nc, k.s
    NB_ = k.cfg.get("nblk_e2", NBLK)
    NU = 11
    GS = 4
    with ExitStack() as st:
        wO = sbt(st, "wO", [128, 8, 1024], BF16)
        wQ = sbt(st, "wQ", [128, 8, 2048], BF16)
        keysT = sbt(st, "keysT", [128, 16, 128], BF16)
        g2 = sbt(st, "g2", [128, 1024], F32)
        g3 = sbt(st, "g3", [128, 1024], F32)
        iota16 = sbt(st, "iota16", [128, 16], F32)
        load_w(wO[:], "wO", T["w_out"], 8, 1024)
        load_w(wQ[:], "wQ", T["peer_wq"], 8, 2048)
        k.dma("sp", g2[:], T["norm_ffn"].partition_broadcast(128), "c0", [], ["g2"])
        k.dma("sp", g3[:], T["norm_final"].partition_broadcast(128), "c0", [], ["g3"])
        k.dma("sp", iota16[:], T["c_iota16"], "c0", [], ["iota16"])
        with ExitStack() as st2:
            kst = sbt(st2, "kst", [128, 2, 128], F32)
            pK = pst(st2, "pK", [128, 128], F32)
            for l in range(16):
                h_, p_ = l // 2, l % 2
                kb = l % 2
                k.dma("sp", kst[:, kb, :], T["peer_keys"][p_ * 8 + h_], "kst%d" % kb, [], [("kst", kb)])
                k.tr(pK[:], kst[:, kb, :], identf[:], [("kst", kb), "identf"], ["pK"])
                k.cp("act", keysT[:, l, :], pK[:], ["pK"], ["keysT"])
            s.barrier()

        hTb = sbt(st, "hTb", [128, 2, 8, 128], BF16)
        xb = sbt(st, "xb", [128, 1, 2, 512], F32)
        x1 = sbt(st, "x1", [128, 2, 2, 512], F32)
        junk = sbt(st, "junk", [128, 1024], BF16)
        junkb = sbt(st, "junkb", [128, 1024], BF16)
        junkb2 = sbt(st, "junkb2", [128, 1024], BF16)
        prodb = sbt(st, "prodb", [128, 3, 1024], BF16)
        ssq = sbt(st, "ssq", [128, 4], F32)
        xn2f = sbt(st, "xn2f", [128, 1024], F32)
        xn2b = sbt(st, "xn2b", [128, 2, 1024], BF16)
        xn2T = sbt(st, "xn2T", [128, 8, 128], BF16)
        qTs = sbt(st, "qTs", [128, 8, 128], BF16)
        s_sb = sbt(st, "s_sb", [128, 16, 128], F32)
        eidx = sbt(st, "eidx", [128, 2, 128], I32)
        gw = sbt(st, "gw", [128, 2, 8, 16], F32)
        uvg = sbt(st, "uvg", [128, NU, 2048], BF16)
        dg = sbt(st, "dg", [128, 4, 128], BF16)
        hcol = sbt(st, "hcol", [128, 128], F32)
        acol = sbt(st, "acol", [128, 128], F32)
        ob = sbt(st, "ob", [128, 2, 512], F32)
        RB = peer_routing_alloc(sbt, st)
        pY = pst(st, "pY", [128, 2, 512], F32)
        pG = pst(st, "pG", [128, 2, 512], F32)
        pT2 = pst(st, "pT2", [128, 8, 128], BF16)
        pQ = pst(st, "pQ", [128, 8, 128], F32)

        def front(b):
            p = b % 2
            tb = slice(b * 128, (b + 1) * 128)
            k.dma("sp", hTb[:, p], T["hT_s"][:, :, tb], "hTb%d" % p, [], [("hTb", p)])
            k.dma("sp", xb[:, 0], T["x"][tb, :].rearrange("t (a n) -> t a n", a=2), "xb0", [], [("xb", 0)])
            for half in range(2):
                for c in range(8):
                    k.mm(pY[:, half, :], hTb[:, p, c, :], wO[:, c, half * 512:(half + 1) * 512], c == 0, c == 7,
                         [("hTb", p), "wO"], ["pY"])
                yield
            k.tt("dve", x1[:, p], xb[:, 0], pY[:], ALU.add, [("xb", 0), "pY"], [("x1", p)])
            x1f = x1[:, p].rearrange("p a n -> p (a n)")
            k.act(junk[:], x1f, AF.Square, [("x1", p)], ["junk", ("ssq", p)], accum=ssq[:, p:p + 1])
            k.act(ssq[:, p:p + 1], ssq[:, p:p + 1], AF.Ln, [("ssq", p)], [("ssq", p)], scale=1.0 / D, bias=EPS)
            k.act(ssq[:, p:p + 1], ssq[:, p:p + 1], AF.Exp, [("ssq", p)], [("ssq", p)], scale=-0.5)
            k.stt("dve", xn2f[:], x1f, ssq[:, p:p + 1], g2[:], ALU.mult, ALU.mult, [("x1", p), ("ssq", p), "g2"], ["xn2f"])
            k.cp("act", xn2b[:, p, :], xn2f[:], ["xn2f"], [("xn2b", p)])
            for c in range(8):
                k.tr(pT2[:, c, :], xn2b[:, p, c * 128:(c + 1) * 128], identb[:], [("xn2b", p), "identb"], ["pT2"])
            k.cp("act", xn2T[:], pT2[:], ["pT2"], ["xn2T"])
            yield
            for l0 in (0, 8):
                for l in range(8):
                    for kc in range(8):
                        k.mm(pQ[:, l, :], wQ[:, kc, (l0 + l) * 128:(l0 + l + 1) * 128], xn2T[:, kc, :], kc == 0, kc == 7,
                             ["xn2T", "wQ"], [("pQ", l // 4)])
                    yield
                for hb in range(2):
                    ls = slice(hb * 4, hb * 4 + 4)
                    k.cp("act", qTs[:, ls, :], pQ[:, ls, :], [("pQ", hb)], [("qTs", hb)])
                yield
                for l in range(8):
                    k.mm(pQ[:, l, :], qTs[:, l, :], keysT[:, l0 + l, :], True, True, [("qTs", l // 4), "keysT"], [("pQ", l // 4)])
                for hb in range(2):
                    ls = slice(hb * 4, hb * 4 + 4)
                    k.cp("act", s_sb[:, l0 + hb * 4:l0 + hb * 4 + 4, :], pQ[:, ls, :], [("pQ", hb)], ["s_sb"])
                yield
            yield from peer_routing(k, RB, s_sb, "s_sb", iota16, eidx[:, p, :], ("eidx", p), gw[:, p], ("gw", p))

        def gath(b, fg):
            p = b % 2
            tb = slice(b * 128, (b + 1) * 128)
            x1f = x1[:, p].rearrange("p a n -> p (a n)")
            gwf = gw[:, p].rearrange("p h i -> p (h i)")
            for kk in range(128):
                ub = kk % NU
                db = kk % 4
                hk = ("hcol", kk % 8)
                ak = ("acol", kk % 8)
                k.s.dma("pool", lambda e, kk=kk, ub=ub, p=p: e.indirect_dma_start(
                    out=uvg[:, ub, :], out_offset=None, in_=T["uv_s"][:, :],
                    in_offset=bass.IndirectOffsetOnAxis(ap=eidx[:, p, kk:kk + 1], axis=0)),
                    "uvg%d" % ub, [("eidx", p), "uv_s"], [("uvg", ub)])
                k.stt("dve", junkb[:], uvg[:, ub, 0:1024], 1.0, xn2b[:, p, :], ALU.mult, ALU.mult,
                      [("uvg", ub), ("xn2b", p)], ["junkb", hk], accum=hcol[:, kk:kk + 1])
                k.act(acol[:, kk:kk + 1], hcol[:, kk:kk + 1], AF.Gelu, [hk], [ak])
                k.act(acol[:, kk:kk + 1], acol[:, kk:kk + 1], AF.Copy, [ak, ("gw", p)], [ak], scale=gwf[:, kk:kk + 1])
                k.act(dg[:, db, :], identb[:], AF.Copy, ["identb", ak], [("dg", db)], scale=acol[:, kk:kk + 1])
                for half in range(2):
                    k.mm(pG[:, half, :], dg[:, db, :], uvg[:, ub, 1024 + half * 512:1024 + (half + 1) * 512],
                         kk == 0, kk == 127, [("dg", db), ("uvg", ub)], ["pG"])
                if fg is not None:
                    next(fg, None)
            if fg is not None:
                for _ in fg:
                    pass
            k.tt("dve", x1[:, p], x1[:, p], pG[:], ALU.add, [("x1", p), "pG"], [("x1", p)])
            k.act(junk[:], x1f, AF.Square, [("x1", p)], ["junk", ("ssq", 2)], accum=ssq[:, 2:3])
            k.act(ssq[:, 2:3], ssq[:, 2:3], AF.Ln, [("ssq", 2)], [("ssq", 2)], scale=1.0 / D, bias=EPS)
            k.act(ssq[:, 2:3], ssq[:, 2:3], AF.Exp, [("ssq", 2)], [("ssq", 2)], scale=-0.5)
            k.stt("dve", ob[:].rearrange("p a n -> p (a n)"), x1f, ssq[:, 2:3], g3[:], ALU.mult, ALU.mult,
                  [("x1", p), ("ssq", 2), "g3"], ["ob"])
            k.dma("sp", T["out"][tb, :].rearrange("t (a n) -> t a n", a=2), ob[:], "ob", ["ob"], [])

        for _ in front(0):
            pass
        for b in range(NB_):
            gath(b, front(b + 1) if b + 1 < NB_ else None)
        s.barrier()


_NC_CACHE = {}


def _core_inputs(inp, b, consts):
    d = {
        "x": np.ascontiguousarray(inp["x"][b], dtype=np.float32),
        "norm_mix": np.asarray(inp["norm_mix"], np.float32).reshape(1, D),
        "w_in": np.ascontiguousarray(np.asarray(inp["w_in"], np.float32)[0]),
        "hg_lb": np.asarray(inp["hg_lb"], np.float32).reshape(2, 512),
        "hg_norm": np.asarray(inp["hg_norm"], np.float32).reshape(1, 512),
        "idx_k_norm_g": np.asarray(inp["idx_k_norm_g"], np.float32).reshape(1, 64),
        "idx_k_norm_b": np.asarray(inp["idx_k_norm_b"], np.float32).reshape(1, 64),
        "w_up_a": np.ascontiguousarray(np.asarray(inp["w_up_a"], np.float32)[0]),
        "w_up_b": np.ascontiguousarray(np.asarray(inp["w_up_b"], np.float32)[0]),
        "w_out": np.ascontiguousarray(np.asarray(inp["w_out"], np.float32)[0]),
        "norm_ffn": np.asarray(inp["norm_ffn"], np.float32).reshape(1, D),
        "peer_wq": np.ascontiguousarray(np.asarray(inp["peer_wq"], np.float32)[0]),
        "peer_keys": np.ascontiguousarray(np.asarray(inp["peer_keys"], np.float32)[0]).reshape(16, 128, 128),
        "peer_u": np.ascontiguousarray(np.asarray(inp["peer_u"], np.float32)[0]),
        "peer_v": np.ascontiguousarray(np.asarray(inp["peer_v"], np.float32)[0]),
        "norm_final": np.asarray(inp["norm_final"], np.float32).reshape(1, D),
    }
    d.update(consts)
    return d


def kernel(**inputs):
    if "nc" not in _NC_CACHE:
        _NC_CACHE["nc"] = build()
    nc = _NC_CACHE["nc"]
    consts = host_consts(np.asarray(inputs["rel_bias"], np.float32))
    shared = _core_inputs(inputs, 0, consts)
    in_maps = []
    for b in range(NCORES):
        d = dict(shared)
        d["x"] = np.ascontiguousarray(np.asarray(inputs["x"])[b], dtype=np.float32)
        in_maps.append(d)
    res = run_bass_kernel_spmd(nc, in_maps, core_ids=list(range(NCORES)))
    out = np.stack([np.asarray(r["out"], dtype=np.float32) for r in res.results], axis=0)
    return out
```

```python
import math
from contextlib import ExitStack

import numpy as np
import ml_dtypes
import concourse.bass as bass
import concourse.mybir as mybir
from concourse.bass_utils import run_bass_kernel_spmd

F32 = mybir.dt.float32
BF16 = mybir.dt.bfloat16
I32 = mybir.dt.int32
U32 = mybir.dt.uint32
ALU = mybir.AluOpType
AF = mybir.ActivationFunctionType
AX = mybir.AxisListType

S = 4096
D = 1024
NBLK = 32
NCORES = 8
COL = dict(hq=0, hf=512, hi=1024, hog=1536, aq=2048, ak=2560, av=2624, iq=2688, ik=2944,
           iw=3008, ga=3012, gb=4036)
IN_WIDTH = 5060
EPS = 1e-6
NEG = -30000.0
NROUNDS = 16


class Sched:
    ENG = ("pe", "dve", "act", "pool", "sp")

    def __init__(self, nc):
        self.nc = nc
        self.q = {e: [] for e in self.ENG}
        self.cnt = {}
        self.seen = {e: {} for e in self.ENG}
        self.w = {}
        self.r = {}
        self.excl = set()

    def _split(self, reads, writes):
        reads = self._units(reads)
        writes = self._units(writes)
        ex = [u for u in reads if u[0] in self.excl]
        if ex:
            reads = [u for u in reads if u[0] not in self.excl]
            writes = writes + [u for u in ex if u not in writes]
        return reads, writes

    @staticmethod
    def _units(specs):
        out = []
        for s in specs:
            if isinstance(s, str):
                out.append((s, 0))
            elif len(s) == 2:
                out.append((s[0], s[1]))
            else:
                for i in range(s[1], s[2]):
                    out.append((s[0], i))
        return out

    def _deps(self, eng, reads, writes):
        deps = {}

        def add(ev, kind):
            if ev is None:
                return
            sem, val = ev
            if sem == "E:" + eng and eng == "pe":
                return
            if deps.get(sem, 0) < val:
                deps[sem] = val

        for u in reads:
            add(self.w.get(u), "raw")
        for u in writes:
            add(self.w.get(u), "waw")
            for sem, val in self.r.get(u, {}).items():
                add((sem, val), "war")
        waits = []
        seen = self.seen[eng]
        for sem, val in deps.items():
            if seen.get(sem, 0) < val:
                seen[sem] = val
                waits.append((sem, val))
        return waits

    def _register(self, ev, reads, writes):
        sem, val = ev
        for u in reads:
            d = self.r.setdefault(u, {})
            if d.get(sem, 0) < val:
                d[sem] = val
        for u in writes:
            self.w[u] = ev
            self.r[u] = {}

    def op(self, eng, fn, reads=(), writes=()):
        reads, writes = self._split(reads, writes)
        waits = self._deps(eng, reads, writes)
        sem = "E:" + eng
        self.cnt[sem] = self.cnt.get(sem, 0) + 1
        ev = (sem, self.cnt[sem])
        self.q[eng].append((fn, waits, (sem, 1)))
        self._register(ev, reads, writes)
        return ev

    def dma(self, queue, fn, sem, reads=(), writes=()):
        reads, writes = self._split(reads, writes)
        waits = self._deps(queue, reads, writes)
        sem = "D:" + sem
        prev = self.cnt.get(sem, 0)
        if prev and self.seen[queue].get(sem, 0) < prev:
            self.seen[queue][sem] = prev
            waits.append((sem, prev))
        self.cnt[sem] = self.cnt.get(sem, 0) + 16
        ev = (sem, self.cnt[sem])
        self.q[queue].append((fn, waits, (sem, 16)))
        self._register(ev, reads, writes)
        return ev

    def barrier(self, engs=None):
        for eng in (engs or self.ENG):
            waits = []
            for sem, val in self.cnt.items():
                if sem == "E:" + eng:
                    continue
                if self.seen[eng].get(sem, 0) < val:
                    self.seen[eng][sem] = val
                    waits.append((sem, val))
            if waits:
                self.q[eng].append((None, waits, None))

    def emit(self):
        nc = self.nc
        with ExitStack() as st:
            handles = {}
            for name in self.cnt:
                handles[name] = st.enter_context(nc.semaphore(name.replace(":", "_")))
            block = st.enter_context(nc.Block())
            engobjs = {"pe": block.tensor, "dve": block.vector, "act": block.scalar,
                       "pool": block.gpsimd, "sp": block.sync}

            def make(ename):
                lst = self.q[ename]

                def body(e):
                    for fn, waits, inc in lst:
                        for sem, val in waits:
                            e.wait_ge(handles[sem], val)
                        if fn is not None:
                            ins = fn(e)
                            ins.then_inc(handles[inc[0]], inc[1])
                return body

            for ename in self.ENG:
                if self.q[ename]:
                    engobjs[ename](make(ename))


class K:
    def __init__(self, nc, cfg):
        self.nc = nc
        self.cfg = cfg
        self.s = Sched(nc)
        self.uid = 0

    def mm(self, out, lhsT, rhs, start, stop, r, w):
        self.s.op("pe", lambda e: e.matmul(out, lhsT=lhsT, rhs=rhs, start=start, stop=stop), r, w)

    def tr(self, out, in_, ident, r, w):
        self.s.op("pe", lambda e: e.transpose(out=out, in_=in_, identity=ident), r, w)

    def act(self, out, in_, func, r, w, scale=1.0, bias=0.0, accum=None):
        if accum is None:
            self.s.op("act", lambda e: e.activation(out=out, in_=in_, func=func, bias=bias, scale=scale), r, w)
        else:
            self.s.op("act", lambda e: e.activation(out=out, in_=in_, func=func, bias=bias, scale=scale,
                                                    accum_out=accum), r, w)

    def tsc(self, eng, out, in0, s1, s2, op0, op1, r, w, accum=None):
        if op1 is None:
            self.s.op(eng, lambda e: e.tensor_scalar(out=out, in0=in0, scalar1=s1, scalar2=None, op0=op0), r, w)
        elif accum is None:
            self.s.op(eng, lambda e: e.tensor_scalar(out=out, in0=in0, scalar1=s1, scalar2=s2, op0=op0, op1=op1), r, w)
        else:
            self.s.op(eng, lambda e: e.tensor_scalar(out=out, in0=in0, scalar1=s1, scalar2=s2, op0=op0, op1=op1,
                                                     accum_out=accum), r, w)

    def stt(self, eng, out, in0, scalar, in1, op0, op1, r, w, accum=None):
        if accum is None:
            self.s.op(eng, lambda e: e.scalar_tensor_tensor(out=out, in0=in0, scalar=scalar, in1=in1, op0=op0, op1=op1), r, w)
        else:
            self.s.op(eng, lambda e: e.scalar_tensor_tensor(out=out, in0=in0, scalar=scalar, in1=in1, op0=op0, op1=op1,
                                                            accum_out=accum), r, w)

    def tt(self, eng, out, in0, in1, op, r, w):
        self.s.op(eng, lambda e: e.tensor_tensor(out=out, in0=in0, in1=in1, op=op), r, w)

    def tred(self, eng, out, in_, op, r, w):
        self.s.op(eng, lambda e: e.tensor_reduce(out=out, in_=in_, axis=AX.X, op=op), r, w)

    def cp(self, eng, out, in_, r, w):
        if eng == "act":
            self.s.op("act", lambda e: e.copy(out=out, in_=in_), r, w)
        else:
            self.s.op(eng, lambda e: e.tensor_copy(out=out, in_=in_), r, w)

    def recip(self, out, in_, r, w):
        self.s.op("dve", lambda e: e.reciprocal(out=out, in_=in_), r, w)

    def memset(self, eng, ap, val, w):
        self.s.op(eng, lambda e: e.memset(ap, val), (), w)

    def dma(self, q, out, in_, sem, r, w):
        self.s.dma(q, lambda e: e.dma_start(out=out, in_=in_), sem, r, w)

    def gen(self, eng, fn, r, w):
        self.s.op(eng, fn, r, w)


def t5_bucket_np(n):
    n = np.maximum(n, 0)
    nf = np.maximum(n, 1).astype(np.float32)
    large = 16 + (np.log(nf / np.float32(16)) / np.float32(math.log(128 / 16)) * np.float32(16)).astype(np.int32)
    large = np.minimum(large, 31)
    return np.where(n < 16, n, large)


def host_consts(rel_bias):
    c = {}
    c["c_ident"] = np.eye(128, dtype=np.float32)
    tl = np.arange(128)
    c["c_cmask"] = np.where(tl[None, :] <= tl[:, None], 0.0, -1e30).astype(np.float32)
    t64 = np.arange(64)
    c["c_tri"] = (t64[:, None] <= t64[None, :]).astype(np.float32)
    cm = np.ones((128, 512), np.float32)
    cm[:, ::64] = 0.0
    c["c_chunkmask"] = cm
    e65 = np.zeros((65, 64), np.float32)
    e65[64, :] = 1.0
    c["c_e65"] = e65
    sel = np.zeros((64, 256), np.float32)
    sel[np.arange(64), np.arange(64)] = 1.0
    sel[np.arange(64), 128 + 64 + np.arange(64)] = 1.0
    c["c_sel"] = sel
    c["c_iota16"] = np.tile(np.arange(16, dtype=np.float32)[None, :], (128, 1))
    sl = np.arange(128)[:, None]
    tt_ = np.arange(128)[None, :]
    bd = t5_bucket_np(tt_ - sl)
    bp = t5_bucket_np(128 + tt_ - sl)
    rb = np.asarray(rel_bias, np.float32)
    c["c_bd"] = np.ascontiguousarray(rb[bd].transpose(0, 2, 1))
    c["c_bp"] = np.ascontiguousarray(rb[bp].transpose(0, 2, 1))
    c["c_b31"] = np.ascontiguousarray(np.broadcast_to(rb[31][None, :, None], (128, 8, 128)))
    return c


CONST_SHAPES = {
    "c_ident": [128, 128], "c_cmask": [128, 128], "c_tri": [64, 64], "c_chunkmask": [128, 512],
    "c_e65": [65, 64], "c_sel": [64, 256], "c_iota16": [128, 16],
    "c_bd": [128, 8, 128], "c_bp": [128, 8, 128], "c_b31": [128, 8, 128],
}

INPUT_SHAPES = {
    "x": [S, D], "norm_mix": [1, D], "w_in": [D, IN_WIDTH], "hg_lb": [2, 512], "hg_norm": [1, 512],
    "idx_k_norm_g": [1, 64], "idx_k_norm_b": [1, 64], "w_up_a": [512, D], "w_up_b": [512, D],
    "w_out": [D, D], "norm_ffn": [1, D], "peer_wq": [D, 2048], "peer_keys": [16, 128, 128],
    "peer_u": [16384, D], "peer_v": [16384, D], "norm_final": [1, D],
}

SCRATCH = {
    "ya_s": ([128, 4, S], BF16), "yb_s": ([128, 4, S], BF16), "hT_s": ([128, 8, S], BF16),
    "uv_s": ([16384, 2048], BF16),
}


def build(cfg=None):
    cfg = cfg or {}
    phases = cfg.get("phases", ["A", "BC", "D", "E1", "E2"])
    inject = cfg.get("inject", [])
    taps = cfg.get("taps", [])
    nc = bass.Bass("TRN2", target_bir_lowering=False)
    k = K(nc, cfg)
    s = k.s
    T = {}
    for name, shp in INPUT_SHAPES.items():
        T[name] = nc.dram_tensor(name, shp, F32, kind="ExternalInput").ap()
    for name, shp in CONST_SHAPES.items():
        T[name] = nc.dram_tensor(name, shp, F32, kind="ExternalInput").ap()
    for name, (shp, dt) in SCRATCH.items():
        kind = "ExternalInput" if name in inject else ("ExternalOutput" if name in taps else "Internal")
        T[name] = nc.dram_tensor(name, shp, dt, kind=kind).ap()
    T["out"] = nc.dram_tensor("out", [S, D], F32, kind="ExternalOutput").ap()

    with ExitStack() as top, nc.allow_low_precision("bf16 matmul operands, fp32 accumulation"), \
            nc.allow_non_contiguous_dma("small strided constant loads"):
        def sbt(st, name, shape, dt):
            return st.enter_context(nc.sbuf_tensor(name, shape, dt))

        def pst(st, name, shape, dt):
            s.excl.add(name)
            return st.enter_context(nc.psum_tensor(name, shape, dt))

        identf = sbt(top, "identf", [128, 128], F32)
        identb = sbt(top, "identb", [128, 128], BF16)
        wst = sbt(top, "wst", [128, 2, 8, 128], F32)
        k.dma("sp", identf[:], T["c_ident"], "c0", [], ["identf"])
        k.cp("dve", identb[:], identf[:], ["identf"], ["identb"])
        wstate = {"n": 0}

        def load_w(dst, dst_key, src, C, n):
            for c0 in range(0, n, 128):
                wdt = min(128, n - c0)
                b = wstate["n"] % 2
                wstate["n"] += 1
                k.dma("sp", wst[:, b, 0:C, 0:wdt], src[:, c0:c0 + wdt].rearrange("(c p) n -> p c n", p=128),
                      "wst%d" % b, [], [("wst", b)])
                k.cp("pool", dst[:, :, c0:c0 + wdt], wst[:, b, 0:C, 0:wdt], [("wst", b)], [dst_key])

        with ExitStack() as mid:
            xnT = sbt(mid, "xnT", [128, 8, S], BF16)
            if "A" in phases:
                phase_A(k, T, sbt, pst, xnT, identb)
            if "BC" in phases:
                phase_BC(k, T, sbt, pst, xnT, identb, identf, load_w)
            if "D" in phases:
                phase_D(k, T, sbt, pst, xnT, identb, identf, load_w)
            if "E1" in phases:
                phase_E1(k, T, sbt, pst, xnT, load_w)
            s.barrier()
        if "E2" in phases:
            phase_E2(k, T, sbt, pst, identb, identf, load_w)
        s.barrier()
        s.emit()
    return nc


def phase_A(k, T, sbt, pst, xnT, identb):
    nc, s = k.nc, k.s
    with ExitStack() as st:
        xt = sbt(st, "A_xt", [128, 2, D], F32)
        gb = sbt(st, "A_gb", [128, D], F32)
        junk = sbt(st, "A_junk", [128, D], F32)
        ss = sbt(st, "A_ss", [128, 2], F32)
        rstd = sbt(st, "A_rstd", [128, 2], F32)
        xs = sbt(st, "A_xs", [128, 2, D], BF16)
        pt = pst(st, "A_pt", [128, 2, 8, 128], BF16)
        k.dma("sp", gb[:], T["norm_mix"].partition_broadcast(128), "c0", [], ["A_gb"])
        for b in range(k.cfg.get("nblk_a", NBLK)):
            p = b % 2
            k.dma("sp", xt[:, p, :], T["x"][b * 128:(b + 1) * 128, :], "A_x%d" % p, [], [("A_xt", p)])
            k.act(junk[:], xt[:, p, :], AF.Square, [("A_xt", p)], ["A_junk", ("A_ss", p)], accum=ss[:, p:p + 1])
            k.act(rstd[:, p:p + 1], ss[:, p:p + 1], AF.Ln, [("A_ss", p)], [("A_rstd", p)], scale=1.0 / D, bias=EPS)
            k.act(rstd[:, p:p + 1], rstd[:, p:p + 1], AF.Exp, [("A_rstd", p)], [("A_rstd", p)], scale=-0.5)
            k.stt("dve", xs[:, p, :], xt[:, p, :], rstd[:, p:p + 1], gb[:], ALU.mult, ALU.mult,
                  [("A_xt", p), ("A_rstd", p), "A_gb"], [("A_xs", p)])
            for c in range(8):
                k.tr(pt[:, p, c, :], xs[:, p, c * 128:(c + 1) * 128], identb[:], [("A_xs", p), "identb"], [("A_pt", p)])
            k.cp("act", xnT[:, :, b * 128:(b + 1) * 128], pt[:, p, :, :], [("A_pt", p)], [("xnT", b)])
        s.barrier()


NCV = 4


def convert_step(k, T, cvf, cvb, r):
    def src_of(q):
        tile_, which = q // 2, q % 2
        rs = slice(tile_ * 128, (tile_ + 1) * 128)
        return (T["peer_u"] if which == 0 else T["peer_v"])[rs, :], T["uv_s"][rs, which * 1024:(which + 1) * 1024]
    if r < 256:
        fb = r % NCV
        k.dma("sp", cvf[:, fb, :], src_of(r)[0], "cvf%d" % fb, [], [("cvf", fb)])
    q = r - (NCV - 1)
    if 0 <= q < 256:
        fb, cb = q % NCV, q % 2
        k.cp("dve", cvb[:, cb, :], cvf[:, fb, :], [("cvf", fb)], [("cvb", cb)])
        k.dma("sp", src_of(q)[1], cvb[:, cb, :], "cvst%d" % cb, [("cvb", cb)], ["uv_s"])


def phase_BC(k, T, sbt, pst, xnT, identb, identf, load_w):
    nc, s = k.nc, k.s
    with ExitStack() as st:
        wB = sbt(st, "wB", [128, 8, 452], BF16)
        wC = sbt(st, "wC", [128, 8, 768], BF16)
        kT = sbt(st, "kT", [64, S], BF16)
        iknT = sbt(st, "iknT", [64, S], BF16)
        v_aug = sbt(st, "v_aug", [128, NBLK, 65], BF16)
        iws = sbt(st, "iws", [128, NBLK, 4], F32)
        gI = sbt(st, "gI", [128, 64], F32)
        bI = sbt(st, "bI", [128, 64], F32)
        cmask = sbt(st, "cmask", [128, 128], F32)
        e65 = sbt(st, "e65", [65, 64], F32)
        self_f = sbt(st, "sel_f", [64, 256], F32)
        sel_b = sbt(st, "sel_b", [64, 256], BF16)
        i8 = sbt(st, "i8", [128, 8, 128], BF16)
        bnd = sbt(st, "bnd", [128, 2, 8, 128], BF16)
        ikf = sbt(st, "ikf", [128, 64], F32)
        ikb = sbt(st, "ikb", [128, 64], BF16)
        stats = sbt(st, "stats", [128, 6], F32)
        mv = sbt(st, "mv", [128, 2], F32)
        irs = sbt(st, "irs", [128, 1], F32)
        qT = sbt(st, "qT", [64, 2, 8, 128], BF16)
        iqT = sbt(st, "iqT", [64, 2, 4, 128], BF16)
        score = sbt(st, "score", [128, S], F32)
        rl = sbt(st, "rl", [128, 2, 4, 256], F32)
        selneg = sbt(st, "selneg", [128, 2, S], BF16)
        cj = sbt(st, "cj", [128, S], BF16)
        lo = sbt(st, "lo", [128, 1], F32)
        hi = sbt(st, "hi", [128, 1], F32)
        mid = sbt(st, "mid", [128, 1], F32)
        cnt = sbt(st, "cnt", [128, 1], F32)
        wtab = sbt(st, "wtab", [128, NROUNDS + 2], F32)
        pw2 = sbt(st, "pw2", [128, NROUNDS + 2], F32)
        pT = sbt(st, "pT", [128, 2, 2, 512], BF16)
        oT = sbt(st, "oT", [65, 2, 512], F32)
        rb = sbt(st, "rb", [64, 2, 512], F32)
        ybn = sbt(st, "ybn", [64, 8, 128], BF16)
        ybo = sbt(st, "ybo", [128, 4, 128], BF16)
        pX = pst(st, "pX", [128, 2, 512], F32)
        pL = pst(st, "pL", [128, 2, 2, 512], F32)
        pO = pst(st, "pO", [128, 2, 512], F32)

        load_w(wB[:], "wB", T["w_in"][:, COL["ak"]:COL["ak"] + 452], 8, 452)
        load_w(wC[:, :, 0:512], "wC", T["w_in"][:, COL["aq"]:COL["aq"] + 512], 8, 512)
        load_w(wC[:, :, 512:768], "wC", T["w_in"][:, COL["iq"]:COL["iq"] + 256], 8, 256)
        k.dma("sp", gI[:], T["idx_k_norm_g"].partition_broadcast(128), "c0", [], ["gI"])
        k.dma("sp", bI[:], T["idx_k_norm_b"].partition_broadcast(128), "c0", [], ["bI"])
        k.dma("sp", cmask[:], T["c_cmask"], "c0", [], ["cmask"])
        k.dma("sp", e65[:], T["c_e65"], "c0", [], ["e65"])
        k.dma("sp", self_f[:], T["c_sel"], "c0", [], ["sel_f"])
        k.cp("dve", sel_b[:], self_f[:], ["sel_f"], ["sel_b"])
        for h in range(8):
            k.cp("dve", i8[:, h, :], identb[:], ["identb"], ["i8"])
        with ExitStack() as st2:
            bstage = sbt(st2, "bstage", [128, 3, 8, 128], F32)
            k.dma("sp", bstage[:, 0], T["c_bd"], "c0", [], ["bstage"])
            k.dma("sp", bstage[:, 1], T["c_bp"], "c0", [], ["bstage"])
            k.dma("sp", bstage[:, 2], T["c_b31"], "c0", [], ["bstage"])
            for j in range(2):
                k.tt("dve", bstage[:, j], bstage[:, j], bstage[:, 2], ALU.subtract, ["bstage"], ["bstage"])
                k.tsc("dve", bnd[:, j], bstage[:, j], 8.0, None, ALU.mult, None, ["bstage"], ["bnd"])
            s.barrier()
        k.memset("dve", v_aug[:, :, 64:65], 1.0, ["v_aug"])
        for r_ in range(NROUNDS + 2):
            k.memset("dve", pw2[:, r_:r_ + 1], 2.0 ** (-r_), ["pw2"])

        stage = k.cfg.get("bc_stage", 9)
        for b in range(k.cfg.get("nblk_b", NBLK) if stage >= 1 else 0):
            tb = slice(b * 128, (b + 1) * 128)
            for c in range(8):
                k.mm(pX[:, 0, 0:452], xnT[:, c, tb], wB[:, c, :], c == 0, c == 7, [("xnT", b), "wB"], [("pX", 0)])
            bv = k.cfg.get("b_var", 9)
            k.cp("act", v_aug[:, b, 0:64], pX[:, 0, 64:128], [("pX", 0)], ["v_aug"])
            if bv >= 2:
                k.cp("act", ikf[:], pX[:, 0, 384:448], [("pX", 0)], ["ikf"])
                k.tsc("dve", iws[:, b, :], pX[:, 0, 448:452], 0.0625, None, ALU.mult, None, [("pX", 0)], ["iws"])
            if bv >= 3:
                k.gen("dve", lambda e: e.bn_stats(out=stats[:], in_=ikf[:]), ["ikf"], ["stats"])
                k.gen("dve", lambda e: e.bn_aggr(out=mv[:], in_=stats[:]), ["stats"], ["mv"])
            if bv >= 4:
                k.act(irs[:], mv[:, 1:2], AF.Ln, ["mv"], ["irs"], bias=EPS)
                k.act(irs[:], irs[:], AF.Exp, ["irs"], ["irs"], scale=-0.5)
                k.tsc("dve", ikf[:], ikf[:], mv[:, 0:1], None, ALU.subtract, None, ["ikf", "mv"], ["ikf"])
                k.tsc("dve", ikf[:], ikf[:], irs[:, 0:1], None, ALU.mult, None, ["ikf", "irs"], ["ikf"])
                k.tt("dve", ikf[:], ikf[:], gI[:], ALU.mult, ["ikf", "gI"], ["ikf"])
                k.tt("dve", ikf[:], ikf[:], bI[:], ALU.add, ["ikf", "bI"], ["ikf"])
            if bv >= 5:
                ptk = pX[0:64, 1, 0:128]
                k.tr(ptk, ikf[:], identf[:], ["ikf", "identf"], [("pX", 1)])
                k.cp("act", iknT[:, tb], ptk, [("pX", 1)], [("iknT", b)])
        for c4 in range(8 if stage >= 2 else 0):
            ts_ = slice(c4 * 512, (c4 + 1) * 512)
            for c in range(8):
                k.mm(pX[0:64, 0, :], wB[:, c, 0:64], xnT[:, c, ts_], c == 0, c == 7,
                     [("xnT", 4 * c4, 4 * c4 + 4), "wB"], [("pX", 0)])
            k.cp("act", kT[:, ts_], pX[0:64, 0, :], [("pX", 0)], [("kT", 4 * c4, 4 * c4 + 4)])

        def c123(i):
            p = i % 2
            tb = slice(i * 128, (i + 1) * 128)
            L = (i + 1) * 128
            pq = pX[0:64, :, :].rearrange("p a (h t) -> p (a h) t", t=128)
            for h in range(8):
                for c in range(8):
                    k.mm(pq[:, h, :], wC[:, c, h * 64:(h + 1) * 64], xnT[:, c, tb], c == 0, c == 7,
                         [("xnT", i), "wC"], [("pX", h // 4)])
            k.cp("act", qT[:, p], pq, [("pX", 0), ("pX", 1)], [("qT", p)])
            for h in range(4):
                for c in range(8):
                    k.mm(pq[:, h, :], wC[:, c, 512 + h * 64:512 + (h + 1) * 64], xnT[:, c, tb], c == 0, c == 7,
                         [("xnT", i), "wC"], [("pX", 0)])
            k.cp("act", iqT[:, p], pq[:, 0:4, :], [("pX", 0)], [("iqT", p)])
            ps4 = pX[:].rearrange("p a (j w) -> p (a j) w", w=256)
            nch = (L + 255) // 256
            for ch in range(nch):
                s0 = ch * 256
                wk = min(256, L - s0)
                rp = ch % 2
                for j in range(4):
                    k.mm(ps4[:, j, 0:wk], iqT[:, p, j, :], iknT[:, s0:s0 + wk], True, True,
                         [("iqT", p), ("iknT", s0 // 128, (s0 + wk) // 128)], [("pX", j // 2)])
                k.act(rl[:, rp, :, 0:wk], ps4[:, :, 0:wk], AF.Relu, [("pX", 0), ("pX", 1)], [("rl", rp)])
                k.tsc("dve", score[:, s0:s0 + wk], rl[:, rp, 0, 0:wk], iws[:, i, 0:1], None, ALU.mult, None,
                      [("rl", rp), "iws"], ["score"])
                for j in range(1, 4):
                    k.stt("dve", score[:, s0:s0 + wk], rl[:, rp, j, 0:wk], iws[:, i, j:j + 1], score[:, s0:s0 + wk],
                          ALU.mult, ALU.add, [("rl", rp), "iws", "score"], ["score"])
            k.tred("dve", hi[:], score[:, 0:L], ALU.max, ["score"], ["hi"])
            k.tred("dve", lo[:], score[:, 0:L], ALU.min, ["score"], ["lo"])
            k.tsc("dve", lo[:], lo[:], -1.0, None, ALU.add, None, ["lo"], ["lo"])
            k.tt("dve", score[:, L - 128:L], score[:, L - 128:L], cmask[:], ALU.add, ["score", "cmask"], ["score"])
            if L > 256:
                k.tsc("dve", mid[:], lo[:], hi[:, 0:1], 0.5, ALU.add, ALU.mult, ["lo", "hi"], ["mid"])
                k.tsc("dve", hi[:], hi[:], lo[:, 0:1], 0.5, ALU.subtract, ALU.mult, ["lo", "hi"], ["hi"])
                k.tsc("dve", wtab[:], pw2[:], hi[:, 0:1], None, ALU.mult, None, ["pw2", "hi"], ["wtab"])
                for r_ in range(NROUNDS):
                    k.tsc("dve", cj[:, 0:L], score[:, 0:L], mid[:, 0:1], None, ALU.is_gt, ALU.add,
                          ["score", "mid"], ["cj", "cnt"], accum=cnt[:])
                    k.tsc("dve", cnt[:], cnt[:], 255.5, wtab[:, r_:r_ + 1], ALU.is_gt, ALU.mult, ["cnt", "wtab"], ["cnt"])
                    k.stt("dve", mid[:], mid[:], wtab[:, r_ + 1:r_ + 2], cnt[:], ALU.subtract, ALU.add,
                          ["mid", "wtab", "cnt"], ["mid"])
                k.tsc("dve", lo[:], mid[:], wtab[:, NROUNDS:NROUNDS + 1], None, ALU.subtract, None, ["mid", "wtab"], ["lo"])
            k.tsc("dve", selneg[:, p, 0:L], score[:, 0:L], lo[:, 0:1], NEG, ALU.is_le, ALU.mult,
                  ["score", "lo"], [("selneg", p)])

        def c4(i):
            p = i % 2

            def qk(j):
                lb_ = j % 2
                sj = slice(j * 128, (j + 1) * 128)
                near = (j == i) or (j == i - 1)
                for half in range(2):
                    hs = slice(4 * half, 4 * half + 4)
                    k.mm(pL[:, lb_, half, :], kT[:, sj], qT[:, p, hs, :], True, False,
                         [("kT", j), ("qT", p)], [("pL", lb_)])
                    k.mm(pL[:, lb_, half, :], selneg[:, p, sj], i8[:, hs, :], False, not near,
                         [("selneg", p), "i8"], [("pL", lb_)])
                    if near:
                        k.mm(pL[:, lb_, half, :], identb[:], bnd[:, 0 if j == i else 1, hs, :], False, True,
                             ["identb", "bnd"], [("pL", lb_)])

            qk(0)
            for j in range(i + 1):
                lb_ = j % 2
                if j + 1 <= i:
                    qk(j + 1)
                k.act(pT[:, lb_, :, :], pL[:, lb_, :, :], AF.Exp, [("pL", lb_)], [("pT", lb_)], scale=0.125)
                for half in range(2):
                    k.mm(pO[0:65, half, :], v_aug[:, j, :], pT[:, lb_, half, :], j == 0, j == i,
                         ["v_aug", ("pT", lb_)], ["pO"])
            k.cp("act", oT[:], pO[0:65, :, :], ["pO"], ["oT"])
            for half in range(2):
                k.mm(pX[0:64, half, :], e65[:], oT[:, half, :], True, True,
                     ["e65", "oT"], [("pX", half)])
            k.act(rb[:], pX[0:64, :, :], AF.Ln, [("pX", 0), ("pX", 1)], ["rb"])
            k.act(rb[:], rb[:], AF.Exp, ["rb"], ["rb"], scale=-1.0)
            k.tt("dve", ybn[:].rearrange("p (a h) t -> p a (h t)", a=2), oT[0:64, :, :], rb[:], ALU.mult, ["oT", "rb"], ["ybn"])
            yv = ybn[:].rearrange("p (c two) t -> p two c t", two=2)
            k.mm(pX[:, 0, :], sel_b[:, 0:128], yv[:, 0], True, False, ["sel_b", "ybn"], [("pX", 0)])
            k.mm(pX[:, 0, :], sel_b[:, 128:256], yv[:, 1], False, True, ["sel_b", "ybn"], [("pX", 0)])
            k.cp("act", ybo[:], pX[:, 0, :], [("pX", 0)], ["ybo"])
            k.dma("sp", T["yb_s"][:, :, i * 128:(i + 1) * 128], ybo[:], "ybo", ["ybo"], [])

        nblk = k.cfg.get("nblk_c", NBLK)
        if stage >= 3:
            c123(0)
        for i in range(nblk if stage >= 3 else 0):
            if i + 1 < nblk:
                c123(i + 1)
            if stage >= 4:
                c4(i)
        s.barrier()


def phase_D(k, T, sbt, pst, xnT, identb, identf, load_w):
    nc, s = k.nc, k.s
    NG = k.cfg.get("ngroups_d", 8)
    with ExitStack() as st:
        wD = sbt(st, "wD", [128, 8, 2048], BF16)
        lbs = sbt(st, "lbs", [128, 2, 4], F32)
        lbT = sbt(st, "lbT", [128, 4], F32)
        omlT = sbt(st, "omlT", [128, 4], F32)
        gnT = sbt(st, "gnT", [128, 4], F32)
        tri = sbt(st, "tri", [64, 64], F32)
        cmk = sbt(st, "cmk", [128, 512], F32)
        ones_b = sbt(st, "ones_b", [128, 128], BF16)
        v_sb = sbt(st, "v_sb", [64, 2, 8, 512], BF16)
        state_f = sbt(st, "state_f", [128, 4, 128], F32)
        state_b = sbt(st, "state_b", [128, 4, 128], BF16)
        NT = 10
        tf = sbt(st, "tf", [128, 2, NT, 512], F32)
        qeT = sbt(st, "qeT", [128, 2, 512], BF16)
        keT = sbt(st, "keT", [128, 2, 512], BF16)
        k2T = sbt(st, "k2T", [128, 2, 512], F32)
        ebl = sbt(st, "ebl", [128, 2, 8], F32)
        at_sb = sbt(st, "at_sb", [64, 2, 64], BF16)
        k2_sb = sbt(st, "k2_sb", [64, 2, 128], BF16)
        sq = sbt(st, "sq", [128, 512], BF16)
        yo = sbt(st, "yo", [128, 2, 512], BF16)
        cvf = sbt(st, "cvf", [128, NCV, 1024], F32)
        cvb = sbt(st, "cvb", [128, 2, 1024], BF16)
        cstep = {"r": 0}
        pP = pst(st, "pP", [128, 3, 512], F32)
        pOo = pst(st, "pOo", [128, 2, 512], F32)
        pM = pst(st, "pM", [128, 2, 512], F32)
        pV = pst(st, "pV", [128, 512], F32)

        for nm, c0 in (("hq", 0), ("hf", 512), ("hi", 1024), ("hog", 1536)):
            load_w(wD[:, :, c0:c0 + 512], "wD", T["w_in"][:, COL[nm]:COL[nm] + 512], 8, 512)
        k.dma("sp", lbs[:], T["hg_lb"].rearrange("r (h p) -> p r h", p=128), "c0", [], ["lbs"])
        k.dma("sp", gnT[:], T["hg_norm"].rearrange("o (h p) -> p (o h)", p=128), "c0", [], ["gnT"])
        k.dma("sp", tri[:], T["c_tri"], "c0", [], ["tri"])
        k.dma("sp", cmk[:], T["c_chunkmask"], "c0", [], ["cmk"])
        k.memset("dve", ones_b[:], 1.0, ["ones_b"])
        k.memset("dve", state_f[:], 0.0, [("state_f", 0, 4)])
        k.memset("dve", state_b[:], 0.0, [("state_b", 0, 4)])
        k.tt("dve", lbT[:], lbs[:, 1, :], lbs[:, 0, :], ALU.subtract, ["lbs"], ["lbT"])
        k.act(lbT[:], lbT[:], AF.Exp, ["lbT"], ["lbT"])
        k.tsc("dve", lbT[:], lbT[:], 1.0, None, ALU.add, None, ["lbT"], ["lbT"])
        k.recip(lbT[:], lbT[:], ["lbT"], ["lbT"])
        k.tsc("dve", omlT[:], lbT[:], -1.0, 1.0, ALU.mult, ALU.add, ["lbT"], ["omlT"])

        def vproj(g):
            gp = g % 2
            xk = ("xnT", 4 * g, 4 * g + 4)
            for c in range(8):
                tok = slice(g * 512 + c * 64, g * 512 + (c + 1) * 64)
                for kc in range(8):
                    k.mm(pV[0:64, :], xnT[:, kc, tok], wD[:, kc, 1024:1536], kc == 0, kc == 7, [xk, "wD"], ["pV"])
                k.cp("act", v_sb[:, gp, c, :], pV[0:64, :], ["pV"], [("v_sb", gp)])

        def prologue(g, h):
            g5 = slice(g * 512, (g + 1) * 512)
            xk = ("xnT", 4 * g, 4 * g + 4)
            u = (g * 4 + h) % 2
            t = lambda i, u=u: tf[:, u, i, :]
            tk = lambda i, u=u: ("tf", u * NT + i)
            for j, c0 in enumerate((0, 512, 1536)):
                for kc in range(8):
                    k.mm(pP[:, j, :], wD[:, kc, c0 + h * 128:c0 + (h + 1) * 128], xnT[:, kc, g5], kc == 0, kc == 7,
                         [xk, "wD"], [("pP", j)])
                    yield
            k.act(t(0), pP[:, 1, :], AF.Exp, [("pP", 1)], [tk(0)], scale=-1.0)
            yield
            k.act(t(0), t(0), AF.Ln, [tk(0)], [tk(0)], bias=1.0)
            yield
            k.act(t(0), t(0), AF.Exp, [tk(0)], [tk(0)], scale=-1.0)
            yield
            k.tsc("dve", t(0), t(0), omlT[:, h:h + 1], lbT[:, h:h + 1], ALU.mult, ALU.add,
                  [tk(0), "omlT", "lbT"], [tk(0)])
            yield
            k.act(t(1), t(0), AF.Ln, [tk(0)], [tk(1)])
            yield
            k.tsc("dve", t(2), t(0), -1.0, 1.0, ALU.mult, ALU.add, [tk(0)], [tk(2)])
            yield
            k.gen("dve", lambda e, o=t(3), d0=cmk[:], d1=t(1): e.tensor_tensor_scan(
                out=o, data0=d0, data1=d1, initial=0.0, op0=ALU.mult, op1=ALU.add),
                ["cmk", tk(1)], [tk(3)])
            yield
            k.act(t(4), t(3), AF.Exp, [tk(3)], [tk(4)])
            yield
            k.act(t(5), t(3), AF.Exp, [tk(3)], [tk(5)], scale=-1.0)
            yield
            for c in range(8):
                cs = slice(c * 64, (c + 1) * 64)
                k.act(tf[:, u, 6, cs], tf[:, u, 3, cs], AF.Exp, [tk(3)], [tk(6)], scale=-1.0,
                      bias=tf[:, u, 3, c * 64 + 63:c * 64 + 64])
                yield
            bl = tf[:, u, 3, :].rearrange("p (c t) -> p c t", t=64)[:, :, 63]
            k.act(ebl[:, u, :], bl, AF.Exp, [tk(3)], [("ebl", u)])
            yield
            k.tt("dve", k2T[:, u, :], t(2), t(6), ALU.mult, [tk(2), tk(6)], [("k2T", u)])
            yield
            k.tt("dve", keT[:, u, :], t(2), t(5), ALU.mult, [tk(2), tk(5)], [("keT", u)])
            yield
            k.act(t(7), pP[:, 0, :], AF.Exp, [("pP", 0)], [tk(7)], scale=-1.0)
            yield
            k.act(t(7), t(7), AF.Ln, [tk(7)], [tk(7)], bias=1.0)
            yield
            k.act(t(7), t(7), AF.Exp, [tk(7)], [tk(7)], scale=-1.0)
            yield
            k.tt("dve", t(7), t(7), pP[:, 0, :], ALU.mult, [tk(7), ("pP", 0)], [tk(7)])
            yield
            k.tt("dve", qeT[:, u, :], t(7), t(4), ALU.mult, [tk(7), tk(4)], [("qeT", u)])
            yield
            k.act(t(8), pP[:, 2, :], AF.Exp, [("pP", 2)], [tk(8)], scale=-1.0)
            yield
            k.act(t(8), t(8), AF.Ln, [tk(8)], [tk(8)], bias=1.0)
            yield
            k.act(t(8), t(8), AF.Exp, [tk(8)], [tk(8)], scale=-1.0)
            yield
            k.tt("dve", t(8), t(8), pP[:, 2, :], ALU.mult, [tk(8), ("pP", 2)], [tk(8)])

        def chunks(g, h, pg=None, qg=None):
            def pull(n=1):
                if qg is not None:
                    next(qg, None)
                if pg is not None:
                    for _ in range(n):
                        next(pg, None)
            gp = g % 2
            u = (g * 4 + h) % 2
            hs = slice(h * 128, (h + 1) * 128)
            for c in range(8):
                cs = slice(c * 64, (c + 1) * 64)
                a2 = c % 2
                k.mm(pM[0:64, 0, 0:64], keT[:, u, cs], qeT[:, u, cs], True, True,
                     [("keT", u), ("qeT", u)], [("pM", 0)])
                k.tt("dve", at_sb[:, a2, :], pM[0:64, 0, 0:64], tri[:], ALU.mult, [("pM", 0), "tri"], [("at_sb", a2)])
                pull(2)
                k.mm(pOo[:, u, cs], state_b[:, h, :], qeT[:, u, cs], True, False,
                     [("state_b", h), ("qeT", u)], [("pOo", u)])
                k.mm(pOo[:, u, cs], v_sb[:, gp, c, hs], at_sb[:, a2, :], False, True,
                     [("v_sb", gp), ("at_sb", a2)], [("pOo", u)])
                k.tr(pM[0:64, 0, 128:256], k2T[:, u, cs], identf[:], [("k2T", u), "identf"], [("pM", 0)])
                k.cp("act", k2_sb[:, a2, :], pM[0:64, 0, 128:256], [("pM", 0)], [("k2_sb", a2)])
                pull(2)
                k.mm(pM[:, 1, 0:128], k2_sb[:, a2, :], v_sb[:, gp, c, hs], True, True,
                     [("k2_sb", a2), ("v_sb", gp)], [("pM", 1)])
                k.stt("dve", state_f[:, h, :], state_f[:, h, :], ebl[:, u, c:c + 1], pM[:, 1, 0:128],
                      ALU.mult, ALU.add, [("state_f", h), ("ebl", u), ("pM", 1)], [("state_f", h)])
                k.cp("act", state_b[:, h, :], state_f[:, h, :], [("state_f", h)], [("state_b", h)])
                pull(2)
                if k.cfg.get("convert", True) and NG == 8:
                    convert_step(k, T, cvf, cvb, cstep["r"])
                    cstep["r"] += 1

        def post(g, h):
            g5 = slice(g * 512, (g + 1) * 512)
            u = (g * 4 + h) % 2
            t = lambda i, u=u: tf[:, u, i, :]
            tk = lambda i, u=u: ("tf", u * NT + i)
            k.act(sq[:], pOo[:, u, :], AF.Square, [("pOo", u)], ["sq"])
            yield
            k.mm(pV[:], ones_b[:], sq[:], True, True, ["ones_b", "sq"], ["pV"])
            yield
            k.act(t(9), pV[:], AF.Ln, ["pV"], [tk(9)], scale=1.0 / 128, bias=EPS)
            yield
            k.act(t(9), t(9), AF.Exp, [tk(9)], [tk(9)], scale=-0.5)
            yield
            k.stt("dve", t(9), pOo[:, u, :], gnT[:, h:h + 1], t(9), ALU.mult, ALU.mult,
                  [("pOo", u), "gnT", tk(9)], [tk(9)])
            yield
            k.tt("dve", yo[:, u, :], t(9), t(8), ALU.mult, [tk(9), tk(8)], [("yo", u)])
            yield
            k.dma("sp", T["ya_s"][:, h, g5], yo[:, u, :], "yo%d" % u, [("yo", u)], [])

        units = [(g, h) for g in range(NG) for h in range(4)]
        vproj(0)
        for _ in prologue(*units[0]):
            pass
        qg = None
        for n, (g, h) in enumerate(units):
            pg = None
            if n + 1 < len(units):
                g1, h1 = units[n + 1]
                if h1 == 0:
                    vproj(g1)
                pg = prologue(g1, h1)
            chunks(g, h, pg, qg)
            if qg is not None:
                for _ in qg:
                    pass
            if pg is not None:
                for _ in pg:
                    pass
            qg = post(g, h)
        for _ in qg:
            pass
        if k.cfg.get("convert", True) and NG == 8:
            while cstep["r"] < 256 + NCV:
                convert_step(k, T, cvf, cvb, cstep["r"])
                cstep["r"] += 1
        s.barrier()


def phase_E1(k, T, sbt, pst, xnT, load_w):
    nc, s = k.nc, k.s
    NG = k.cfg.get("ngroups_e1", 8)
    with ExitStack() as st:
        wG = sbt(st, "wG", [128, 8, 2048], BF16)
        wU = sbt(st, "wU", [128, 2, 4, 1024], BF16)
        yab = sbt(st, "yab", [128, 2, 2, 4, 512], BF16)
        sg = sbt(st, "sg", [128, 2, 2, 512], F32)
        tmp = sbt(st, "e1tmp", [128, 2, 2, 512], F32)
        hTg = sbt(st, "hTg", [128, 2, 8, 512], BF16)
        pE = pst(st, "pE", [128, 2, 4, 512], F32)
        load_w(wG[:, :, 0:1024], "wG", T["w_in"][:, COL["ga"]:COL["ga"] + 1024], 8, 1024)
        load_w(wG[:, :, 1024:2048], "wG", T["w_in"][:, COL["gb"]:COL["gb"] + 1024], 8, 1024)
        load_w(wU[:, 0], "wU", T["w_up_a"], 4, 1024)
        load_w(wU[:, 1], "wU", T["w_up_b"], 4, 1024)
        for g in range(NG):
            gp = g % 2
            g5 = slice(g * 512, (g + 1) * 512)
            xk = ("xnT", 4 * g, 4 * g + 4)
            k.dma("sp", yab[:, gp, 0], T["ya_s"][:, :, g5], "yab%d" % gp, [], [("yab", gp)])
            k.dma("sp", yab[:, gp, 1], T["yb_s"][:, :, g5], "yab%d" % gp, [], [("yab", gp)])
            for nn in range(8):
                pb = nn % 2
                ns = slice(nn * 128, (nn + 1) * 128)
                for ab in range(2):
                    for kc in range(8):
                        k.mm(pE[:, pb, ab, :], wG[:, kc, ab * 1024 + nn * 128:ab * 1024 + (nn + 1) * 128], xnT[:, kc, g5],
                             kc == 0, kc == 7, [xk, "wG"], [("pE", pb * 4 + ab)])
                    for c4 in range(4):
                        k.mm(pE[:, pb, 2 + ab, :], wU[:, ab, c4, ns], yab[:, gp, ab, c4, :], c4 == 0, c4 == 3,
                             [("yab", gp), "wU"], [("pE", pb * 4 + 2 + ab)])
                for ab in range(2):
                    k.act(sg[:, pb, ab, :], pE[:, pb, ab, :], AF.Exp, [("pE", pb * 4 + ab)], [("sg", pb * 2 + ab)], scale=-1.0)
                    k.act(sg[:, pb, ab, :], sg[:, pb, ab, :], AF.Ln, [("sg", pb * 2 + ab)], [("sg", pb * 2 + ab)], bias=1.0)
                    k.act(sg[:, pb, ab, :], sg[:, pb, ab, :], AF.Exp, [("sg", pb * 2 + ab)], [("sg", pb * 2 + ab)], scale=-1.0)
                    k.tt("dve", tmp[:, pb, ab, :], sg[:, pb, ab, :], pE[:, pb, 2 + ab, :], ALU.mult,
                         [("sg", pb * 2 + ab), ("pE", pb * 4 + 2 + ab)], [("e1tmp", pb * 2 + ab)])
                k.tt("dve", hTg[:, gp, nn, :], tmp[:, pb, 0, :], tmp[:, pb, 1, :], ALU.add,
                     [("e1tmp", pb * 2), ("e1tmp", pb * 2 + 1)], [("hTg", gp)])
            k.dma("sp", T["hT_s"][:, :, g5], hTg[:, gp], "hTg%d" % gp, [("hTg", gp)], [])
        s.barrier()


def peer_routing_alloc(sbt, st, pfx="r"):
    B = {}
    B["vals"] = sbt(st, pfx + "vals", [128, 16, 16], F32)
    B["idxs"] = sbt(st, pfx + "idxs", [128, 16, 16], U32)
    B["idxf"] = sbt(st, pfx + "idxf", [128, 16, 16], F32)
    B["s2"] = sbt(st, pfx + "s2", [128, 128], F32)
    B["cand"] = sbt(st, pfx + "cand", [128, 8, 256], F32)
    B["cand2"] = sbt(st, pfx + "cand2", [128, 8, 256], F32)
    B["tops"] = sbt(st, pfx + "tops", [128, 8, 16], F32)
    B["pos"] = sbt(st, pfx + "pos", [128, 8, 16], U32)
    B["ipos"] = sbt(st, pfx + "ipos", [128, 8, 16], U32)
    B["jpos"] = sbt(st, pfx + "jpos", [128, 8, 16], U32)
    B["iposf"] = sbt(st, pfx + "iposf", [128, 8, 16], F32)
    B["jposf"] = sbt(st, pfx + "jposf", [128, 8, 16], F32)
    B["eq"] = sbt(st, pfx + "eq", [128, 8, 16, 16], F32)
    B["sel1"] = sbt(st, pfx + "sel1", [128, 8, 16], F32)
    B["sel2"] = sbt(st, pfx + "sel2", [128, 8, 16], F32)
    B["gsum"] = sbt(st, pfx + "gsum", [128, 8], F32)
    return B


def peer_routing(k, B, s_sb, s_key, iota16, eidx, eidx_key, gw, gw_key, pfx="r"):
    vals, idxs, idxf, s2, cand, cand2 = B["vals"], B["idxs"], B["idxf"], B["s2"], B["cand"], B["cand2"]
    tops, pos, ipos, jpos, iposf, jposf = B["tops"], B["pos"], B["ipos"], B["jpos"], B["iposf"], B["jposf"]
    eq, sel1, sel2, gsum = B["eq"], B["sel1"], B["sel2"], B["gsum"]
    K_ = pfx
    for l in range(16):
        k.gen("dve", lambda e, l=l: e.max(out=vals[:, l, 0:8], in_=s_sb[:, l, :]), [s_key], [K_ + "vals"])
        k.gen("dve", lambda e, l=l: e.max_index(out=idxs[:, l, 0:8], in_max=vals[:, l, 0:8], in_values=s_sb[:, l, :]),
              [s_key, K_ + "vals"], [K_ + "idxs"])
        k.gen("dve", lambda e, l=l: e.match_replace(out=s2[:], in_to_replace=vals[:, l, 0:8], in_values=s_sb[:, l, :],
                                                    imm_value=-1e30), [s_key, K_ + "vals"], [K_ + "s2"])
        k.gen("dve", lambda e, l=l: e.max(out=vals[:, l, 8:16], in_=s2[:]), [K_ + "s2"], [K_ + "vals"])
        k.gen("dve", lambda e, l=l: e.max_index(out=idxs[:, l, 8:16], in_max=vals[:, l, 8:16], in_values=s2[:]),
              [K_ + "s2", K_ + "vals"], [K_ + "idxs"])
        yield
    k.cp("dve", idxf[:], idxs[:], [K_ + "idxs"], [K_ + "idxf"])
    v4 = vals[:].rearrange("p (h two) i -> p h two i", two=2)
    x4 = idxf[:].rearrange("p (h two) i -> p h two i", two=2)
    c4 = cand[:].rearrange("p h (i j) -> p h i j", j=16)
    k.tt("dve", c4, v4[:, :, 0, :].unsqueeze(3).broadcast_to([128, 8, 16, 16]),
         v4[:, :, 1, :].unsqueeze(2).broadcast_to([128, 8, 16, 16]), ALU.add, [K_ + "vals"], [K_ + "cand"])
    for h in range(8):
        k.gen("dve", lambda e, h=h: e.max(out=tops[:, h, 0:8], in_=cand[:, h, :]), [K_ + "cand"], [K_ + "tops"])
        k.gen("dve", lambda e, h=h: e.max_index(out=pos[:, h, 0:8], in_max=tops[:, h, 0:8], in_values=cand[:, h, :]),
              [K_ + "cand", K_ + "tops"], [K_ + "pos"])
        k.gen("dve", lambda e, h=h: e.match_replace(out=cand2[:, h, :], in_to_replace=tops[:, h, 0:8],
                                                    in_values=cand[:, h, :], imm_value=-1e30),
              [K_ + "cand", K_ + "tops"], [K_ + "cand2"])
        k.gen("dve", lambda e, h=h: e.max(out=tops[:, h, 8:16], in_=cand2[:, h, :]), [K_ + "cand2"], [K_ + "tops"])
        k.gen("dve", lambda e, h=h: e.max_index(out=pos[:, h, 8:16], in_max=tops[:, h, 8:16], in_values=cand2[:, h, :]),
              [K_ + "cand2", K_ + "tops"], [K_ + "pos"])
        yield
    k.tsc("dve", ipos[:], pos[:], 4, None, ALU.logical_shift_right, None, [K_ + "pos"], [K_ + "ipos"])
    k.tsc("dve", jpos[:], pos[:], 15, None, ALU.bitwise_and, None, [K_ + "pos"], [K_ + "jpos"])
    k.cp("dve", iposf[:], ipos[:], [K_ + "ipos"], [K_ + "iposf"])
    k.cp("dve", jposf[:], jpos[:], [K_ + "jpos"], [K_ + "jposf"])
    io4 = iota16[:].unsqueeze(1).unsqueeze(1).broadcast_to([128, 8, 16, 16])
    for (pf_, xi, sel) in ((iposf, 0, sel1), (jposf, 1, sel2)):
        k.tt("dve", eq[:], io4, pf_[:].unsqueeze(3).broadcast_to([128, 8, 16, 16]), ALU.is_equal,
             ["iota16", K_ + "iposf", K_ + "jposf"], [K_ + "eq"])
        k.tt("dve", eq[:], eq[:], x4[:, :, xi, :].unsqueeze(2).broadcast_to([128, 8, 16, 16]), ALU.mult,
             [K_ + "eq", K_ + "idxf"], [K_ + "eq"])
        k.tred("dve", sel[:], eq[:], ALU.add, [K_ + "eq"], [K_ + "sel"])
    yield
    k.stt("dve", sel1[:], sel1[:], 128.0, sel2[:], ALU.mult, ALU.add, [K_ + "sel"], [K_ + "sel"])
    k.cp("dve", eidx.rearrange("p (h i) -> p h i", i=16), sel1[:], [K_ + "sel"], [eidx_key])
    k.tt("dve", gw, tops[:], tops[:, :, 0:1].broadcast_to([128, 8, 16]), ALU.subtract, [K_ + "tops"], [gw_key])
    k.act(gw, gw, AF.Exp, [gw_key], [gw_key])
    k.tred("dve", gsum[:], gw, ALU.add, [gw_key], [K_ + "gsum"])
    k.recip(gsum[:], gsum[:], [K_ + "gsum"], [K_ + "gsum"])
    k.tt("dve", gw, gw, gsum[:].unsqueeze(2).broadcast_to([128, 8, 16]), ALU.mult, [gw_key, K_ + "gsum"], [gw_key])


def phase_E2(k, T, sbt, pst, identb, identf, load_w):
    nc, s = k.nc, k.s
    NB_ = k.cfg.get("nblk_e2", NBLK)
    NU = 11
    GS = 4
    with ExitStack() as st:
        wO = sbt(st, "wO", [128, 8, 1024], BF16)
        wQ = sbt(st, "wQ", [128, 8, 2048], BF16)
        keysT = sbt(st, "keysT", [128, 16, 128], BF16)
        g2 = sbt(st, "g2", [128, 1024], F32)
        g3 = sbt(st, "g3", [128, 1024], F32)
        iota16 = sbt(st, "iota16", [128, 16], F32)
        load_w(wO[:], "wO", T["w_out"], 8, 1024)
        load_w(wQ[:], "wQ", T["peer_wq"], 8, 2048)
        k.dma("sp", g2[:], T["norm_ffn"].partition_broadcast(128), "c0", [], ["g2"])
        k.dma("sp", g3[:], T["norm_final"].partition_broadcast(128), "c0", [], ["g3"])
        k.dma("sp", iota16[:], T["c_iota16"], "c0", [], ["iota16"])
        with ExitStack() as st2:
            kst = sbt(st2, "kst", [128, 2, 128], F32)
            pK = pst(st2, "pK", [128, 128], F32)
            for l in range(16):
                h_, p_ = l // 2, l % 2
                kb = l % 2
                k.dma("sp", kst[:, kb, :], T["peer_keys"][p_ * 8 + h_], "kst%d" % kb, [], [("kst", kb)])
                k.tr(pK[:], kst[:, kb, :], identf[:], [("kst", kb), "identf"], ["pK"])
                k.cp("act", keysT[:, l, :], pK[:], ["pK"], ["keysT"])
            s.barrier()

        hTb = sbt(st, "hTb", [128, 2, 8, 128], BF16)
        xb = sbt(st, "xb", [128, 1, 2, 512], F32)
        x1 = sbt(st, "x1", [128, 2, 2, 512], F32)
        junk = sbt(st, "junk", [128, 1024], BF16)
        junkb = sbt(st, "junkb", [128, 1024], BF16)
        junkb2 = sbt(st, "junkb2", [128, 1024], BF16)
        prodb = sbt(st, "prodb", [128, 3, 1024], BF16)
        ssq = sbt(st, "ssq", [128, 4], F32)
        xn2f = sbt(st, "xn2f", [128, 1024], F32)
        xn2b = sbt(st, "xn2b", [128, 2, 1024], BF16)
        xn2T = sbt(st, "xn2T", [128, 8, 128], BF16)
        qTs = sbt(st, "qTs", [128, 8, 128], BF16)
        s_sb = sbt(st, "s_sb", [128, 16, 128], F32)
        eidx = sbt(st, "eidx", [128, 2, 128], I32)
        gw = sbt(st, "gw", [128, 2, 8, 16], F32)
        uvg = sbt(st, "uvg", [128, NU, 2048], BF16)
        dg = sbt(st, "dg", [128, 4, 128], BF16)
        hcol = sbt(st, "hcol", [128, 128], F32)
        acol = sbt(st, "acol", [128, 128], F32)
        ob = sbt(st, "ob", [128, 2, 512], F32)
        RB = peer_routing_alloc(sbt, st)
        pY = pst(st, "pY", [128, 2, 512], F32)
        pG = pst(st, "pG", [128, 2, 512], F32)
        pT2 = pst(st, "pT2", [128, 8, 128], BF16)
        pQ = pst(st, "pQ", [128, 8, 128], F32)

        def front(b):
            p = b % 2
            tb = slice(b * 128, (b + 1) * 128)
            k.dma("sp", hTb[:, p], T["hT_s"][:, :, tb], "hTb%d" % p, [], [("hTb", p)])
            k.dma("sp", xb[:, 0], T["x"][tb, :].rearrange("t (a n) -> t a n", a=2), "xb0", [], [("xb", 0)])
            for half in range(2):
                for c in range(8):
                    k.mm(pY[:, half, :], hTb[:, p, c, :], wO[:, c, half * 512:(half + 1) * 512], c == 0, c == 7,
                         [("hTb", p), "wO"], ["pY"])
                yield
            k.tt("dve", x1[:, p], xb[:, 0], pY[:], ALU.add, [("xb", 0), "pY"], [("x1", p)])
            x1f = x1[:, p].rearrange("p a n -> p (a n)")
            k.act(junk[:], x1f, AF.Square, [("x1", p)], ["junk", ("ssq", p)], accum=ssq[:, p:p + 1])
            k.act(ssq[:, p:p + 1], ssq[:, p:p + 1], AF.Ln, [("ssq", p)], [("ssq", p)], scale=1.0 / D, bias=EPS)
            k.act(ssq[:, p:p + 1], ssq[:, p:p + 1], AF.Exp, [("ssq", p)], [("ssq", p)], scale=-0.5)
            k.stt("dve", xn2f[:], x1f, ssq[:, p:p + 1], g2[:], ALU.mult, ALU.mult, [("x1", p), ("ssq", p), "g2"], ["xn2f"])
            k.cp("act", xn2b[:, p, :], xn2f[:], ["xn2f"], [("xn2b", p)])
            for c in range(8):
                k.tr(pT2[:, c, :], xn2b[:, p, c * 128:(c + 1) * 128], identb[:], [("xn2b", p), "identb"], ["pT2"])
            k.cp("act", xn2T[:], pT2[:], ["pT2"], ["xn2T"])
            yield
            for l0 in (0, 8):
                for l in range(8):
                    for kc in range(8):
                        k.mm(pQ[:, l, :], wQ[:, kc, (l0 + l) * 128:(l0 + l + 1) * 128], xn2T[:, kc, :], kc == 0, kc == 7,
                             ["xn2T", "wQ"], [("pQ", l // 4)])
                    yield
                for hb in range(2):
                    ls = slice(hb * 4, hb * 4 + 4)
                    k.cp("act", qTs[:, ls, :], pQ[:, ls, :], [("pQ", hb)], [("qTs", hb)])
                yield
                for l in range(8):
                    k.mm(pQ[:, l, :], qTs[:, l, :], keysT[:, l0 + l, :], True, True, [("qTs", l // 4), "keysT"], [("pQ", l // 4)])
                for hb in range(2):
                    ls = slice(hb * 4, hb * 4 + 4)
                    k.cp("act", s_sb[:, l0 + hb * 4:l0 + hb * 4 + 4, :], pQ[:, ls, :], [("pQ", hb)], ["s_sb"])
                yield
            yield from peer_routing(k, RB, s_sb, "s_sb", iota16, eidx[:, p, :], ("eidx", p), gw[:, p], ("gw", p))

        def gath(b, fg):
            p = b % 2
            tb = slice(b * 128, (b + 1) * 128)
            x1f = x1[:, p].rearrange("p a n -> p (a n)")
            gwf = gw[:, p].rearrange("p h i -> p (h i)")
            for kk in range(128):
                ub = kk % NU
                db = kk % 4
                hk = ("hcol", kk % 8)
                ak = ("acol", kk % 8)
                k.s.dma("pool", lambda e, kk=kk, ub=ub, p=p: e.indirect_dma_start(
                    out=uvg[:, ub, :], out_offset=None, in_=T["uv_s"][:, :],
                    in_offset=bass.IndirectOffsetOnAxis(ap=eidx[:, p, kk:kk + 1], axis=0)),
                    "uvg%d" % ub, [("eidx", p), "uv_s"], [("uvg", ub)])
                k.stt("dve", junkb[:], uvg[:, ub, 0:1024], 1.0, xn2b[:, p, :], ALU.mult, ALU.mult,
                      [("uvg", ub), ("xn2b", p)], ["junkb", hk], accum=hcol[:, kk:kk + 1])
                k.act(acol[:, kk:kk + 1], hcol[:, kk:kk + 1], AF.Gelu, [hk], [ak])
                k.act(acol[:, kk:kk + 1], acol[:, kk:kk + 1], AF.Copy, [ak, ("gw", p)], [ak], scale=gwf[:, kk:kk + 1])
                k.act(dg[:, db, :], identb[:], AF.Copy, ["identb", ak], [("dg", db)], scale=acol[:, kk:kk + 1])
                for half in range(2):
                    k.mm(pG[:, half, :], dg[:, db, :], uvg[:, ub, 1024 + half * 512:1024 + (half + 1) * 512],
                         kk == 0, kk == 127, [("dg", db), ("uvg", ub)], ["pG"])
                if fg is not None:
                    next(fg, None)
            if fg is not None:
                for _ in fg:
                    pass
            k.tt("dve", x1[:, p], x1[:, p], pG[:], ALU.add, [("x1", p), "pG"], [("x1", p)])
            k.act(junk[:], x1f, AF.Square, [("x1", p)], ["junk", ("ssq", 2)], accum=ssq[:, 2:3])
            k.act(ssq[:, 2:3], ssq[:, 2:3], AF.Ln, [("ssq", 2)], [("ssq", 2)], scale=1.0 / D, bias=EPS)
            k.act(ssq[:, 2:3], ssq[:, 2:3], AF.Exp, [("ssq", 2)], [("ssq", 2)], scale=-0.5)
            k.stt("dve", ob[:].rearrange("p a n -> p (a n)"), x1f, ssq[:, 2:3], g3[:], ALU.mult, ALU.mult,
                  [("x1", p), ("ssq", 2), "g3"], ["ob"])
            k.dma("sp", T["out"][tb, :].rearrange("t (a n) -> t a n", a=2), ob[:], "ob", ["ob"], [])

        for _ in front(0):
            pass
        for b in range(NB_):
            gath(b, front(b + 1) if b + 1 < NB_ else None)
        s.barrier()


_NC_CACHE = {}


def _core_inputs(inp, b, consts):
    d = {
        "x": np.ascontiguousarray(inp["x"][b], dtype=np.float32),
        "norm_mix": np.asarray(inp["norm_mix"], np.float32).reshape(1, D),
        "w_in": np.ascontiguousarray(np.asarray(inp["w_in"], np.float32)[0]),
        "hg_lb": np.asarray(inp["hg_lb"], np.float32).reshape(2, 512),
        "hg_norm": np.asarray(inp["hg_norm"], np.float32).reshape(1, 512),
        "idx_k_norm_g": np.asarray(inp["idx_k_norm_g"], np.float32).reshape(1, 64),
        "idx_k_norm_b": np.asarray(inp["idx_k_norm_b"], np.float32).reshape(1, 64),
        "w_up_a": np.ascontiguousarray(np.asarray(inp["w_up_a"], np.float32)[0]),
        "w_up_b": np.ascontiguousarray(np.asarray(inp["w_up_b"], np.float32)[0]),
        "w_out": np.ascontiguousarray(np.asarray(inp["w_out"], np.float32)[0]),
        "norm_ffn": np.asarray(inp["norm_ffn"], np.float32).reshape(1, D),
        "peer_wq": np.ascontiguousarray(np.asarray(inp["peer_wq"], np.float32)[0]),
        "peer_keys": np.ascontiguousarray(np.asarray(inp["peer_keys"], np.float32)[0]).reshape(16, 128, 128),
        "peer_u": np.ascontiguousarray(np.asarray(inp["peer_u"], np.float32)[0]),
        "peer_v": np.ascontiguousarray(np.asarray(inp["peer_v"], np.float32)[0]),
        "norm_final": np.asarray(inp["norm_final"], np.float32).reshape(1, D),
    }
    d.update(consts)
    return d


def kernel(**inputs):
    if "nc" not in _NC_CACHE:
        _NC_CACHE["nc"] = build()
    nc = _NC_CACHE["nc"]
    consts = host_consts(np.asarray(inputs["rel_bias"], np.float32))
    shared = _core_inputs(inputs, 0, consts)
    in_maps = []
    for b in range(NCORES):
        d = dict(shared)
        d["x"] = np.ascontiguousarray(np.asarray(inputs["x"])[b], dtype=np.float32)
        in_maps.append(d)
    res = run_bass_kernel_spmd(nc, in_maps, core_ids=list(range(NCORES)))
    out = np.stack([np.asarray(r["out"], dtype=np.float32) for r in res.results], axis=0)
    return out
```

```python
import math
from contextlib import ExitStack

import numpy as np
import ml_dtypes
import concourse.bass as bass
import concourse.mybir as mybir
from concourse.bass_utils import run_bass_kernel_spmd

F32 = mybir.dt.float32
BF16 = mybir.dt.bfloat16
I32 = mybir.dt.int32
U32 = mybir.dt.uint32
ALU = mybir.AluOpType
AF = mybir.ActivationFunctionType
AX = mybir.AxisListType

S = 4096
D = 1024
NBLK = 32
NCORES = 8
COL = dict(hq=0, hf=512, hi=1024, hog=1536, aq=2048, ak=2560, av=2624, iq=2688, ik=2944,
           iw=3008, ga=3012, gb=4036)
IN_WIDTH = 5060
EPS = 1e-6
NEG = -30000.0
NROUNDS = 16


class Sched:
    ENG = ("pe", "dve", "act", "pool", "sp")

    def __init__(self, nc):
        self.nc = nc
        self.q = {e: [] for e in self.ENG}
        self.cnt = {}
        self.seen = {e: {} for e in self.ENG}
        self.w = {}
        self.r = {}
        self.excl = set()

    def _split(self, reads, writes):
        reads = self._units(reads)
        writes = self._units(writes)
        ex = [u for u in reads if u[0] in self.excl]
        if ex:
            reads = [u for u in reads if u[0] not in self.excl]
            writes = writes + [u for u in ex if u not in writes]
        return reads, writes

    @staticmethod
    def _units(specs):
        out = []
        for s in specs:
            if isinstance(s, str):
                out.append((s, 0))
            elif len(s) == 2:
                out.append((s[0], s[1]))
            else:
                for i in range(s[1], s[2]):
                    out.append((s[0], i))
        return out

    def _deps(self, eng, reads, writes):
        deps = {}

        def add(ev, kind):
            if ev is None:
                return
            sem, val = ev
            if sem == "E:" + eng and eng == "pe":
                return
            if deps.get(sem, 0) < val:
                deps[sem] = val

        for u in reads:
            add(self.w.get(u), "raw")
        for u in writes:
            add(self.w.get(u), "waw")
            for sem, val in self.r.get(u, {}).items():
                add((sem, val), "war")
        waits = []
        seen = self.seen[eng]
        for sem, val in deps.items():
            if seen.get(sem, 0) < val:
                seen[sem] = val
                waits.append((sem, val))
        return waits

    def _register(self, ev, reads, writes):
        sem, val = ev
        for u in reads:
            d = self.r.setdefault(u, {})
            if d.get(sem, 0) < val:
                d[sem] = val
        for u in writes:
            self.w[u] = ev
            self.r[u] = {}

    def op(self, eng, fn, reads=(), writes=()):
        reads, writes = self._split(reads, writes)
        waits = self._deps(eng, reads, writes)
        sem = "E:" + eng
        self.cnt[sem] = self.cnt.get(sem, 0) + 1
        ev = (sem, self.cnt[sem])
        self.q[eng].append((fn, waits, (sem, 1)))
        self._register(ev, reads, writes)
        return ev

    def dma(self, queue, fn, sem, reads=(), writes=()):
        reads, writes = self._split(reads, writes)
        waits = self._deps(queue, reads, writes)
        sem = "D:" + sem
        prev = self.cnt.get(sem, 0)
        if prev and self.seen[queue].get(sem, 0) < prev:
            self.seen[queue][sem] = prev
            waits.append((sem, prev))
        self.cnt[sem] = self.cnt.get(sem, 0) + 16
        ev = (sem, self.cnt[sem])
        self.q[queue].append((fn, waits, (sem, 16)))
        self._register(ev, reads, writes)
        return ev

    def barrier(self, engs=None):
        for eng in (engs or self.ENG):
            waits = []
            for sem, val in self.cnt.items():
                if sem == "E:" + eng:
                    continue
                if self.seen[eng].get(sem, 0) < val:
                    self.seen[eng][sem] = val
                    waits.append((sem, val))
            if waits:
                self.q[eng].append((None, waits, None))

    def emit(self):
        nc = self.nc
        with ExitStack() as st:
            handles = {}
            for name in self.cnt:
                handles[name] = st.enter_context(nc.semaphore(name.replace(":", "_")))
            block = st.enter_context(nc.Block())
            engobjs = {"pe": block.tensor, "dve": block.vector, "act": block.scalar,
                       "pool": block.gpsimd, "sp": block.sync}

            def make(ename):
                lst = self.q[ename]

                def body(e):
                    for fn, waits, inc in lst:
                        for sem, val in waits:
                            e.wait_ge(handles[sem], val)
                        if fn is not None:
                            ins = fn(e)
                            ins.then_inc(handles[inc[0]], inc[1])
                return body

            for ename in self.ENG:
                if self.q[ename]:
                    engobjs[ename](make(ename))


class K:
    def __init__(self, nc, cfg):
        self.nc = nc
        self.cfg = cfg
        self.s = Sched(nc)
        self.uid = 0

    def mm(self, out, lhsT, rhs, start, stop, r, w):
        self.s.op("pe", lambda e: e.matmul(out, lhsT=lhsT, rhs=rhs, start=start, stop=stop), r, w)

    def tr(self, out, in_, ident, r, w):
        self.s.op("pe", lambda e: e.transpose(out=out, in_=in_, identity=ident), r, w)

    def act(self, out, in_, func, r, w, scale=1.0, bias=0.0, accum=None):
        if accum is None:
            self.s.op("act", lambda e: e.activation(out=out, in_=in_, func=func, bias=bias, scale=scale), r, w)
        else:
            self.s.op("act", lambda e: e.activation(out=out, in_=in_, func=func, bias=bias, scale=scale,
                                                    accum_out=accum), r, w)

    def tsc(self, eng, out, in0, s1, s2, op0, op1, r, w, accum=None):
        if op1 is None:
            self.s.op(eng, lambda e: e.tensor_scalar(out=out, in0=in0, scalar1=s1, scalar2=None, op0=op0), r, w)
        elif accum is None:
            self.s.op(eng, lambda e: e.tensor_scalar(out=out, in0=in0, scalar1=s1, scalar2=s2, op0=op0, op1=op1), r, w)
        else:
            self.s.op(eng, lambda e: e.tensor_scalar(out=out, in0=in0, scalar1=s1, scalar2=s2, op0=op0, op1=op1,
                                                     accum_out=accum), r, w)

    def stt(self, eng, out, in0, scalar, in1, op0, op1, r, w, accum=None):
        if accum is None:
            self.s.op(eng, lambda e: e.scalar_tensor_tensor(out=out, in0=in0, scalar=scalar, in1=in1, op0=op0, op1=op1), r, w)
        else:
            self.s.op(eng, lambda e: e.scalar_tensor_tensor(out=out, in0=in0, scalar=scalar, in1=in1, op0=op0, op1=op1,
                                                            accum_out=accum), r, w)

    def tt(self, eng, out, in0, in1, op, r, w):
        self.s.op(eng, lambda e: e.tensor_tensor(out=out, in0=in0, in1=in1, op=op), r, w)

    def tred(self, eng, out, in_, op, r, w):
        self.s.op(eng, lambda e: e.tensor_reduce(out=out, in_=in_, axis=AX.X, op=op), r, w)

    def cp(self, eng, out, in_, r, w):
        if eng == "act":
            self.s.op("act", lambda e: e.copy(out=out, in_=in_), r, w)
        else:
            self.s.op(eng, lambda e: e.tensor_copy(out=out, in_=in_), r, w)

    def recip(self, out, in_, r, w):
        self.s.op("dve", lambda e: e.reciprocal(out=out, in_=in_), r, w)

    def memset(self, eng, ap, val, w):
        self.s.op(eng, lambda e: e.memset(ap, val), (), w)

    def dma(self, q, out, in_, sem, r, w):
        self.s.dma(q, lambda e: e.dma_start(out=out, in_=in_), sem, r, w)

    def gen(self, eng, fn, r, w):
        self.s.op(eng, fn, r, w)


def t5_bucket_np(n):
    n = np.maximum(n, 0)
    nf = np.maximum(n, 1).astype(np.float32)
    large = 16 + (np.log(nf / np.float32(16)) / np.float32(math.log(128 / 16)) * np.float32(16)).astype(np.int32)
    large = np.minimum(large, 31)
    return np.where(n < 16, n, large)


def host_consts(rel_bias):
    c = {}
    c["c_ident"] = np.eye(128, dtype=np.float32)
    tl = np.arange(128)
    c["c_cmask"] = np.where(tl[None, :] <= tl[:, None], 0.0, -1e30).astype(np.float32)
    t64 = np.arange(64)
    c["c_tri"] = (t64[:, None] <= t64[None, :]).astype(np.float32)
    cm = np.ones((128, 512), np.float32)
    cm[:, ::64] = 0.0
    c["c_chunkmask"] = cm
    e65 = np.zeros((65, 64), np.float32)
    e65[64, :] = 1.0
    c["c_e65"] = e65
    sel = np.zeros((64, 256), np.float32)
    sel[np.arange(64), np.arange(64)] = 1.0
    sel[np.arange(64), 128 + 64 + np.arange(64)] = 1.0
    c["c_sel"] = sel
    c["c_iota16"] = np.tile(np.arange(16, dtype=np.float32)[None, :], (128, 1))
    sl = np.arange(128)[:, None]
    tt_ = np.arange(128)[None, :]
    bd = t5_bucket_np(tt_ - sl)
    bp = t5_bucket_np(128 + tt_ - sl)
    rb = np.asarray(rel_bias, np.float32)
    c["c_bd"] = np.ascontiguousarray(rb[bd].transpose(0, 2, 1))
    c["c_bp"] = np.ascontiguousarray(rb[bp].transpose(0, 2, 1))
    c["c_b31"] = np.ascontiguousarray(np.broadcast_to(rb[31][None, :, None], (128, 8, 128)))
    return c


CONST_SHAPES = {
    "c_ident": [128, 128], "c_cmask": [128, 128], "c_tri": [64, 64], "c_chunkmask": [128, 512],
    "c_e65": [65, 64], "c_sel": [64, 256], "c_iota16": [128, 16],
    "c_bd": [128, 8, 128], "c_bp": [128, 8, 128], "c_b31": [128, 8, 128],
}

INPUT_SHAPES = {
    "x": [S, D], "norm_mix": [1, D], "w_in": [D, IN_WIDTH], "hg_lb": [2, 512], "hg_norm": [1, 512],
    "idx_k_norm_g": [1, 64], "idx_k_norm_b": [1, 64], "w_up_a": [512, D], "w_up_b": [512, D],
    "w_out": [D, D], "norm_ffn": [1, D], "peer_wq": [D, 2048], "peer_keys": [16, 128, 128],
    "peer_u": [16384, D], "peer_v": [16384, D], "norm_final": [1, D],
}

SCRATCH = {
    "ya_s": ([128, 4, S], BF16), "yb_s": ([128, 4, S], BF16), "hT_s": ([128, 8, S], BF16),
    "uv_s": ([16384, 2048], BF16),
}


def build(cfg=None):
    cfg = cfg or {}
    phases = cfg.get("phases", ["A", "BC", "D", "E1", "E2"])
    inject = cfg.get("inject", [])
    taps = cfg.get("taps", [])
    nc = bass.Bass("TRN2", target_bir_lowering=False)
    k = K(nc, cfg)
    s = k.s
    T = {}
    for name, shp in INPUT_SHAPES.items():
        T[name] = nc.dram_tensor(name, shp, F32, kind="ExternalInput").ap()
    for name, shp in CONST_SHAPES.items():
        T[name] = nc.dram_tensor(name, shp, F32, kind="ExternalInput").ap()
    for name, (shp, dt) in SCRATCH.items():
        kind = "ExternalInput" if name in inject else ("ExternalOutput" if name in taps else "Internal")
        T[name] = nc.dram_tensor(name, shp, dt, kind=kind).ap()
    T["out"] = nc.dram_tensor("out", [S, D], F32, kind="ExternalOutput").ap()

    with ExitStack() as top, nc.allow_low_precision("bf16 matmul operands, fp32 accumulation"), \
            nc.allow_non_contiguous_dma("small strided constant loads"):
        def sbt(st, name, shape, dt):
            return st.enter_context(nc.sbuf_tensor(name, shape, dt))

        def pst(st, name, shape, dt):
            s.excl.add(name)
            return st.enter_context(nc.psum_tensor(name, shape, dt))

        identf = sbt(top, "identf", [128, 128], F32)
        identb = sbt(top, "identb", [128, 128], BF16)
        wst = sbt(top, "wst", [128, 2, 8, 128], F32)
        k.dma("sp", identf[:], T["c_ident"], "c0", [], ["identf"])
        k.cp("dve", identb[:], identf[:], ["identf"], ["identb"])
        wstate = {"n": 0}

        def load_w(dst, dst_key, src, C, n):
            wflat = [wst[:, b].rearrange("p c n -> p (c n)") for b in range(2)]
            for c in range(C):
                for c0 in range(0, n, 1024):
                    wdt = min(1024, n - c0)
                    b = wstate["n"] % 2
                    wstate["n"] += 1
                    k.dma("sp", wflat[b][:, 0:wdt], src[c * 128:(c + 1) * 128, c0:c0 + wdt],
                          "wst%d" % b, [], [("wst", b)])
                    k.cp("dve", dst[:, c, c0:c0 + wdt], wflat[b][:, 0:wdt], [("wst", b)], [dst_key])

        with ExitStack() as mid:
            xnT = sbt(mid, "xnT", [128, 8, S], BF16)
            if "A" in phases:
                phase_A(k, T, sbt, pst, xnT, identb)
            if "BC" in phases:
                phase_BC(k, T, sbt, pst, xnT, identb, identf, load_w)
            if "D" in phases:
                phase_D(k, T, sbt, pst, xnT, identb, identf, load_w)
            if "E1" in phases:
                phase_E1(k, T, sbt, pst, xnT, load_w)
            s.barrier()
        if "E2" in phases:
            phase_E2(k, T, sbt, pst, identb, identf, load_w)
        s.barrier()
        s.emit()
    return nc


def phase_A(k, T, sbt, pst, xnT, identb):
    nc, s = k.nc, k.s
    with ExitStack() as st:
        xt = sbt(st, "A_xt", [128, 2, D], F32)
        gb = sbt(st, "A_gb", [128, D], F32)
        junk = sbt(st, "A_junk", [128, D], F32)
        ss = sbt(st, "A_ss", [128, 2], F32)
        rstd = sbt(st, "A_rstd", [128, 2], F32)
        xs = sbt(st, "A_xs", [128, 2, D], BF16)
        pt = pst(st, "A_pt", [128, 2, 8, 128], BF16)
        k.dma("sp", gb[:], T["norm_mix"].partition_broadcast(128), "c0", [], ["A_gb"])
        for b in range(k.cfg.get("nblk_a", NBLK)):
            p = b % 2
            k.dma("sp", xt[:, p, :], T["x"][b * 128:(b + 1) * 128, :], "A_x%d" % p, [], [("A_xt", p)])
            k.act(junk[:], xt[:, p, :], AF.Square, [("A_xt", p)], ["A_junk", ("A_ss", p)], accum=ss[:, p:p + 1])
            k.act(rstd[:, p:p + 1], ss[:, p:p + 1], AF.Ln, [("A_ss", p)], [("A_rstd", p)], scale=1.0 / D, bias=EPS)
            k.act(rstd[:, p:p + 1], rstd[:, p:p + 1], AF.Exp, [("A_rstd", p)], [("A_rstd", p)], scale=-0.5)
            k.stt("dve", xs[:, p, :], xt[:, p, :], rstd[:, p:p + 1], gb[:], ALU.mult, ALU.mult,
                  [("A_xt", p), ("A_rstd", p), "A_gb"], [("A_xs", p)])
            for c in range(8):
                k.tr(pt[:, p, c, :], xs[:, p, c * 128:(c + 1) * 128], identb[:], [("A_xs", p), "identb"], [("A_pt", p)])
            k.cp("act", xnT[:, :, b * 128:(b + 1) * 128], pt[:, p, :, :], [("A_pt", p)], [("xnT", b)])
        s.barrier()


NCV = 4


def convert_step(k, T, cvf, cvb, r):
    def src_of(q):
        tile_, which = q // 2, q % 2
        rs = slice(tile_ * 128, (tile_ + 1) * 128)
        return (T["peer_u"] if which == 0 else T["peer_v"])[rs, :], T["uv_s"][rs, which * 1024:(which + 1) * 1024]
    if r < 256:
        fb = r % NCV
        k.dma("sp", cvf[:, fb, :], src_of(r)[0], "cvf%d" % fb, [], [("cvf", fb)])
    q = r - (NCV - 1)
    if 0 <= q < 256:
        fb, cb = q % NCV, q % 2
        k.cp("act", cvb[:, cb, :], cvf[:, fb, :], [("cvf", fb)], [("cvb", cb)])
        k.dma("sp", src_of(q)[1], cvb[:, cb, :], "cvst%d" % cb, [("cvb", cb)], ["uv_s"])


def phase_BC(k, T, sbt, pst, xnT, identb, identf, load_w):
    nc, s = k.nc, k.s
    with ExitStack() as st:
        wB = sbt(st, "wB", [128, 8, 452], BF16)
        wC = sbt(st, "wC", [128, 8, 768], BF16)
        kT = sbt(st, "kT", [64, S], BF16)
        iknT = sbt(st, "iknT", [64, S], BF16)
        v_aug = sbt(st, "v_aug", [128, NBLK, 65], BF16)
        iws = sbt(st, "iws", [128, NBLK, 4], F32)
        gI = sbt(st, "gI", [128, 64], F32)
        bI = sbt(st, "bI", [128, 64], F32)
        cmask = sbt(st, "cmask", [128, 128], F32)
        e65 = sbt(st, "e65", [65, 64], F32)
        self_f = sbt(st, "sel_f", [64, 256], F32)
        sel_b = sbt(st, "sel_b", [64, 256], BF16)
        i8 = sbt(st, "i8", [128, 8, 128], BF16)
        bnd = sbt(st, "bnd", [128, 2, 8, 128], BF16)
        ikf = sbt(st, "ikf", [128, 64], F32)
        ikb = sbt(st, "ikb", [128, 64], BF16)
        stats = sbt(st, "stats", [128, 6], F32)
        mv = sbt(st, "mv", [128, 2], F32)
        irs = sbt(st, "irs", [128, 1], F32)
        qT = sbt(st, "qT", [64, 2, 8, 128], BF16)
        iqT = sbt(st, "iqT", [64, 2, 4, 128], BF16)
        score = sbt(st, "score", [128, S], F32)
        rl = sbt(st, "rl", [128, 2, 4, 256], F32)
        selneg = sbt(st, "selneg", [128, 2, S], BF16)
        cj = sbt(st, "cj", [128, S], BF16)
        lo = sbt(st, "lo", [128, 1], F32)
        hi = sbt(st, "hi", [128, 1], F32)
        mid = sbt(st, "mid", [128, 1], F32)
        cnt = sbt(st, "cnt", [128, 1], F32)
        wtab = sbt(st, "wtab", [128, NROUNDS + 2], F32)
        pw2 = sbt(st, "pw2", [128, NROUNDS + 2], F32)
        pT = sbt(st, "pT", [128, 2, 2, 512], BF16)
        oT = sbt(st, "oT", [65, 2, 512], F32)
        rb = sbt(st, "rb", [64, 2, 512], F32)
        ybn = sbt(st, "ybn", [64, 8, 128], BF16)
        ybo = sbt(st, "ybo", [128, 4, 128], BF16)
        pX = pst(st, "pX", [128, 2, 512], F32)
        pL = pst(st, "pL", [128, 2, 2, 512], F32)
        pO = pst(st, "pO", [128, 2, 512], F32)

        load_w(wB[:], "wB", T["w_in"][:, COL["ak"]:COL["ak"] + 452], 8, 452)
        load_w(wC[:, :, 0:512], "wC", T["w_in"][:, COL["aq"]:COL["aq"] + 512], 8, 512)
        load_w(wC[:, :, 512:768], "wC", T["w_in"][:, COL["iq"]:COL["iq"] + 256], 8, 256)
        k.dma("sp", gI[:], T["idx_k_norm_g"].partition_broadcast(128), "c0", [], ["gI"])
        k.dma("sp", bI[:], T["idx_k_norm_b"].partition_broadcast(128), "c0", [], ["bI"])
        k.dma("sp", cmask[:], T["c_cmask"], "c0", [], ["cmask"])
        k.dma("sp", e65[:], T["c_e65"], "c0", [], ["e65"])
        k.dma("sp", self_f[:], T["c_sel"], "c0", [], ["sel_f"])
        k.cp("dve", sel_b[:], self_f[:], ["sel_f"], ["sel_b"])
        for h in range(8):
            k.cp("dve", i8[:, h, :], identb[:], ["identb"], ["i8"])
        with ExitStack() as st2:
            bstage = sbt(st2, "bstage", [128, 3, 8, 128], F32)
            k.dma("sp", bstage[:, 0], T["c_bd"], "c0", [], ["bstage"])
            k.dma("sp", bstage[:, 1], T["c_bp"], "c0", [], ["bstage"])
            k.dma("sp", bstage[:, 2], T["c_b31"], "c0", [], ["bstage"])
            for j in range(2):
                k.tt("dve", bstage[:, j], bstage[:, j], bstage[:, 2], ALU.subtract, ["bstage"], ["bstage"])
                k.tsc("dve", bnd[:, j], bstage[:, j], 8.0, None, ALU.mult, None, ["bstage"], ["bnd"])
            s.barrier()
        k.memset("dve", v_aug[:, :, 64:65], 1.0, ["v_aug"])
        for r_ in range(NROUNDS + 2):
            k.memset("dve", pw2[:, r_:r_ + 1], 2.0 ** (-r_), ["pw2"])

        stage = k.cfg.get("bc_stage", 9)
        for b in range(k.cfg.get("nblk_b", NBLK) if stage >= 1 else 0):
            tb = slice(b * 128, (b + 1) * 128)
            for c in range(8):
                k.mm(pX[:, 0, 0:452], xnT[:, c, tb], wB[:, c, :], c == 0, c == 7, [("xnT", b), "wB"], [("pX", 0)])
            bv = k.cfg.get("b_var", 9)
            k.cp("act", v_aug[:, b, 0:64], pX[:, 0, 64:128], [("pX", 0)], ["v_aug"])
            if bv >= 2:
                k.cp("act", ikf[:], pX[:, 0, 384:448], [("pX", 0)], ["ikf"])
                k.tsc("dve", iws[:, b, :], pX[:, 0, 448:452], 0.0625, None, ALU.mult, None, [("pX", 0)], ["iws"])
            if bv >= 3:
                k.gen("dve", lambda e: e.bn_stats(out=stats[:], in_=ikf[:]), ["ikf"], ["stats"])
                k.gen("dve", lambda e: e.bn_aggr(out=mv[:], in_=stats[:]), ["stats"], ["mv"])
            if bv >= 4:
                k.act(irs[:], mv[:, 1:2], AF.Ln, ["mv"], ["irs"], bias=EPS)
                k.act(irs[:], irs[:], AF.Exp, ["irs"], ["irs"], scale=-0.5)
                k.tsc("dve", ikf[:], ikf[:], mv[:, 0:1], None, ALU.subtract, None, ["ikf", "mv"], ["ikf"])
                k.tsc("dve", ikf[:], ikf[:], irs[:, 0:1], None, ALU.mult, None, ["ikf", "irs"], ["ikf"])
                k.tt("dve", ikf[:], ikf[:], gI[:], ALU.mult, ["ikf", "gI"], ["ikf"])
                k.tt("dve", ikf[:], ikf[:], bI[:], ALU.add, ["ikf", "bI"], ["ikf"])
            if bv >= 5:
                ptk = pX[0:64, 1, 0:128]
                k.tr(ptk, ikf[:], identf[:], ["ikf", "identf"], [("pX", 1)])
                k.cp("act", iknT[:, tb], ptk, [("pX", 1)], [("iknT", b)])
        for c4 in range(8 if stage >= 2 else 0):
            ts_ = slice(c4 * 512, (c4 + 1) * 512)
            for c in range(8):
                k.mm(pX[0:64, 0, :], wB[:, c, 0:64], xnT[:, c, ts_], c == 0, c == 7,
                     [("xnT", 4 * c4, 4 * c4 + 4), "wB"], [("pX", 0)])
            k.cp("act", kT[:, ts_], pX[0:64, 0, :], [("pX", 0)], [("kT", 4 * c4, 4 * c4 + 4)])

        def c123(i):
            p = i % 2
            tb = slice(i * 128, (i + 1) * 128)
            L = (i + 1) * 128
            pq = pX[0:64, :, :].rearrange("p a (h t) -> p (a h) t", t=128)
            for h in range(8):
                for c in range(8):
                    k.mm(pq[:, h, :], wC[:, c, h * 64:(h + 1) * 64], xnT[:, c, tb], c == 0, c == 7,
                         [("xnT", i), "wC"], [("pX", h // 4)])
            k.cp("act", qT[:, p], pq, [("pX", 0), ("pX", 1)], [("qT", p)])
            for h in range(4):
                for c in range(8):
                    k.mm(pq[:, h, :], wC[:, c, 512 + h * 64:512 + (h + 1) * 64], xnT[:, c, tb], c == 0, c == 7,
                         [("xnT", i), "wC"], [("pX", 0)])
            k.cp("act", iqT[:, p], pq[:, 0:4, :], [("pX", 0)], [("iqT", p)])
            ps4 = pX[:].rearrange("p a (j w) -> p (a j) w", w=256)
            nch = (L + 255) // 256
            for ch in range(nch):
                s0 = ch * 256
                wk = min(256, L - s0)
                rp = ch % 2
                for j in range(4):
                    k.mm(ps4[:, j, 0:wk], iqT[:, p, j, :], iknT[:, s0:s0 + wk], True, True,
                         [("iqT", p), ("iknT", s0 // 128, (s0 + wk) // 128)], [("pX", j // 2)])
                k.act(rl[:, rp, :, 0:wk], ps4[:, :, 0:wk], AF.Relu, [("pX", 0), ("pX", 1)], [("rl", rp)])
                k.tsc("dve", score[:, s0:s0 + wk], rl[:, rp, 0, 0:wk], iws[:, i, 0:1], None, ALU.mult, None,
                      [("rl", rp), "iws"], ["score"])
                for j in range(1, 4):
                    k.stt("dve", score[:, s0:s0 + wk], rl[:, rp, j, 0:wk], iws[:, i, j:j + 1], score[:, s0:s0 + wk],
                          ALU.mult, ALU.add, [("rl", rp), "iws", "score"], ["score"])
            k.tred("dve", hi[:], score[:, 0:L], ALU.max, ["score"], ["hi"])
            k.tred("dve", lo[:], score[:, 0:L], ALU.min, ["score"], ["lo"])
            k.tsc("dve", lo[:], lo[:], -1.0, None, ALU.add, None, ["lo"], ["lo"])
            k.tt("dve", score[:, L - 128:L], score[:, L - 128:L], cmask[:], ALU.add, ["score", "cmask"], ["score"])
            if L > 256:
                k.tsc("dve", mid[:], lo[:], hi[:, 0:1], 0.5, ALU.add, ALU.mult, ["lo", "hi"], ["mid"])
                k.tsc("dve", hi[:], hi[:], lo[:, 0:1], 0.5, ALU.subtract, ALU.mult, ["lo", "hi"], ["hi"])
                k.tsc("dve", wtab[:], pw2[:], hi[:, 0:1], None, ALU.mult, None, ["pw2", "hi"], ["wtab"])
                for r_ in range(NROUNDS):
                    k.tsc("dve", cj[:, 0:L], score[:, 0:L], mid[:, 0:1], None, ALU.is_gt, ALU.add,
                          ["score", "mid"], ["cj", "cnt"], accum=cnt[:])
                    k.tsc("dve", cnt[:], cnt[:], 255.5, wtab[:, r_:r_ + 1], ALU.is_gt, ALU.mult, ["cnt", "wtab"], ["cnt"])
                    k.stt("dve", mid[:], mid[:], wtab[:, r_ + 1:r_ + 2], cnt[:], ALU.subtract, ALU.add,
                          ["mid", "wtab", "cnt"], ["mid"])
                k.tsc("dve", lo[:], mid[:], wtab[:, NROUNDS:NROUNDS + 1], None, ALU.subtract, None, ["mid", "wtab"], ["lo"])
            k.tsc("dve", selneg[:, p, 0:L], score[:, 0:L], lo[:, 0:1], NEG, ALU.is_le, ALU.mult,
                  ["score", "lo"], [("selneg", p)])

        def c4(i):
            p = i % 2

            def qk(j):
                lb_ = j % 2
                sj = slice(j * 128, (j + 1) * 128)
                near = (j == i) or (j == i - 1)
                for half in range(2):
                    hs = slice(4 * half, 4 * half + 4)
                    k.mm(pL[:, lb_, half, :], kT[:, sj], qT[:, p, hs, :], True, False,
                         [("kT", j), ("qT", p)], [("pL", lb_)])
                    k.mm(pL[:, lb_, half, :], selneg[:, p, sj], i8[:, hs, :], False, not near,
                         [("selneg", p), "i8"], [("pL", lb_)])
                    if near:
                        k.mm(pL[:, lb_, half, :], identb[:], bnd[:, 0 if j == i else 1, hs, :], False, True,
                             ["identb", "bnd"], [("pL", lb_)])

            qk(0)
            for j in range(i + 1):
                lb_ = j % 2
                if j + 1 <= i:
                    qk(j + 1)
                k.act(pT[:, lb_, :, :], pL[:, lb_, :, :], AF.Exp, [("pL", lb_)], [("pT", lb_)], scale=0.125)
                for half in range(2):
                    k.mm(pO[0:65, half, :], v_aug[:, j, :], pT[:, lb_, half, :], j == 0, j == i,
                         ["v_aug", ("pT", lb_)], ["pO"])
            k.cp("act", oT[:], pO[0:65, :, :], ["pO"], ["oT"])
            for half in range(2):
                k.mm(pX[0:64, half, :], e65[:], oT[:, half, :], True, True,
                     ["e65", "oT"], [("pX", half)])
            k.act(rb[:], pX[0:64, :, :], AF.Ln, [("pX", 0), ("pX", 1)], ["rb"])
            k.act(rb[:], rb[:], AF.Exp, ["rb"], ["rb"], scale=-1.0)
            k.tt("dve", ybn[:].rearrange("p (a h) t -> p a (h t)", a=2), oT[0:64, :, :], rb[:], ALU.mult, ["oT", "rb"], ["ybn"])
            yv = ybn[:].rearrange("p (c two) t -> p two c t", two=2)
            k.mm(pX[:, 0, :], sel_b[:, 0:128], yv[:, 0], True, False, ["sel_b", "ybn"], [("pX", 0)])
            k.mm(pX[:, 0, :], sel_b[:, 128:256], yv[:, 1], False, True, ["sel_b", "ybn"], [("pX", 0)])
            k.cp("act", ybo[:], pX[:, 0, :], [("pX", 0)], ["ybo"])
            k.dma("sp", T["yb_s"][:, :, i * 128:(i + 1) * 128], ybo[:], "ybo", ["ybo"], [])

        nblk = k.cfg.get("nblk_c", NBLK)
        if stage >= 3:
            c123(0)
        for i in range(nblk if stage >= 3 else 0):
            if i + 1 < nblk:
                c123(i + 1)
            if stage >= 4:
                c4(i)
        s.barrier()


def phase_D(k, T, sbt, pst, xnT, identb, identf, load_w):
    nc, s = k.nc, k.s
    NG = k.cfg.get("ngroups_d", 8)
    with ExitStack() as st:
        wD = sbt(st, "wD", [128, 8, 2048], BF16)
        lbs = sbt(st, "lbs", [128, 2, 4], F32)
        lbT = sbt(st, "lbT", [128, 4], F32)
        omlT = sbt(st, "omlT", [128, 4], F32)
        gnT = sbt(st, "gnT", [128, 4], F32)
        tri = sbt(st, "tri", [64, 64], F32)
        cmk = sbt(st, "cmk", [128, 512], F32)
        ones_b = sbt(st, "ones_b", [128, 128], BF16)
        v_sb = sbt(st, "v_sb", [64, 2, 8, 512], BF16)
        state_f = sbt(st, "state_f", [128, 4, 128], F32)
        state_b = sbt(st, "state_b", [128, 4, 128], BF16)
        NT = 10
        tf = sbt(st, "tf", [128, 2, NT, 512], F32)
        qeT = sbt(st, "qeT", [128, 2, 512], BF16)
        keT = sbt(st, "keT", [128, 2, 512], BF16)
        k2T = sbt(st, "k2T", [128, 2, 512], F32)
        ebl = sbt(st, "ebl", [128, 2, 8], F32)
        at_sb = sbt(st, "at_sb", [64, 2, 64], BF16)
        k2_sb = sbt(st, "k2_sb", [64, 2, 128], BF16)
        sq = sbt(st, "sq", [128, 512], BF16)
        yo = sbt(st, "yo", [128, 2, 512], BF16)
        cvf = sbt(st, "cvf", [128, NCV, 1024], F32)
        cvb = sbt(st, "cvb", [128, 2, 1024], BF16)
        cstep = {"r": 0}
        pP = pst(st, "pP", [128, 3, 512], F32)
        pOo = pst(st, "pOo", [128, 2, 512], F32)
        pM = pst(st, "pM", [128, 2, 512], F32)
        pV = pst(st, "pV", [128, 512], F32)

        load_w(wD[:], "wD", T["w_in"][:, 0:2048], 8, 2048)
        k.dma("sp", lbs[:], T["hg_lb"].rearrange("r (h p) -> p r h", p=128), "c0", [], ["lbs"])
        k.dma("sp", gnT[:], T["hg_norm"].rearrange("o (h p) -> p (o h)", p=128), "c0", [], ["gnT"])
        k.dma("sp", tri[:], T["c_tri"], "c0", [], ["tri"])
        k.dma("sp", cmk[:], T["c_chunkmask"], "c0", [], ["cmk"])
        k.memset("dve", ones_b[:], 1.0, ["ones_b"])
        k.memset("dve", state_f[:], 0.0, [("state_f", 0, 4)])
        k.memset("dve", state_b[:], 0.0, [("state_b", 0, 4)])
        k.tt("dve", lbT[:], lbs[:, 1, :], lbs[:, 0, :], ALU.subtract, ["lbs"], ["lbT"])
        k.act(lbT[:], lbT[:], AF.Exp, ["lbT"], ["lbT"])
        k.tsc("dve", lbT[:], lbT[:], 1.0, None, ALU.add, None, ["lbT"], ["lbT"])
        k.recip(lbT[:], lbT[:], ["lbT"], ["lbT"])
        k.tsc("dve", omlT[:], lbT[:], -1.0, 1.0, ALU.mult, ALU.add, ["lbT"], ["omlT"])

        def vproj(g):
            gp = g % 2
            xk = ("xnT", 4 * g, 4 * g + 4)
            for c in range(8):
                tok = slice(g * 512 + c * 64, g * 512 + (c + 1) * 64)
                for kc in range(8):
                    k.mm(pV[0:64, :], xnT[:, kc, tok], wD[:, kc, 1024:1536], kc == 0, kc == 7, [xk, "wD"], ["pV"])
                k.cp("act", v_sb[:, gp, c, :], pV[0:64, :], ["pV"], [("v_sb", gp)])

        def prologue(g, h):
            g5 = slice(g * 512, (g + 1) * 512)
            xk = ("xnT", 4 * g, 4 * g + 4)
            u = (g * 4 + h) % 2
            t = lambda i, u=u: tf[:, u, i, :]
            tk = lambda i, u=u: ("tf", u * NT + i)
            for j, c0 in enumerate((0, 512, 1536)):
                for kc in range(8):
                    k.mm(pP[:, j, :], wD[:, kc, c0 + h * 128:c0 + (h + 1) * 128], xnT[:, kc, g5], kc == 0, kc == 7,
                         [xk, "wD"], [("pP", j)])
                    yield
            k.act(t(0), pP[:, 1, :], AF.Exp, [("pP", 1)], [tk(0)], scale=-1.0)
            yield
            k.act(t(0), t(0), AF.Ln, [tk(0)], [tk(0)], bias=1.0)
            yield
            k.act(t(0), t(0), AF.Exp, [tk(0)], [tk(0)], scale=-1.0)
            yield
            k.tsc("dve", t(0), t(0), omlT[:, h:h + 1], lbT[:, h:h + 1], ALU.mult, ALU.add,
                  [tk(0), "omlT", "lbT"], [tk(0)])
            yield
            k.act(t(1), t(0), AF.Ln, [tk(0)], [tk(1)])
            yield
            k.tsc("dve", t(2), t(0), -1.0, 1.0, ALU.mult, ALU.add, [tk(0)], [tk(2)])
            yield
            k.gen("dve", lambda e, o=t(3), d0=cmk[:], d1=t(1): e.tensor_tensor_scan(
                out=o, data0=d0, data1=d1, initial=0.0, op0=ALU.mult, op1=ALU.add),
                ["cmk", tk(1)], [tk(3)])
            yield
            k.act(t(4), t(3), AF.Exp, [tk(3)], [tk(4)])
            yield
            k.act(t(5), t(3), AF.Exp, [tk(3)], [tk(5)], scale=-1.0)
            yield
            for c in range(8):
                cs = slice(c * 64, (c + 1) * 64)
                k.act(tf[:, u, 6, cs], tf[:, u, 3, cs], AF.Exp, [tk(3)], [tk(6)], scale=-1.0,
                      bias=tf[:, u, 3, c * 64 + 63:c * 64 + 64])
                yield
            bl = tf[:, u, 3, :].rearrange("p (c t) -> p c t", t=64)[:, :, 63]
            k.act(ebl[:, u, :], bl, AF.Exp, [tk(3)], [("ebl", u)])
            yield
            k.tt("dve", k2T[:, u, :], t(2), t(6), ALU.mult, [tk(2), tk(6)], [("k2T", u)])
            yield
            k.tt("dve", keT[:, u, :], t(2), t(5), ALU.mult, [tk(2), tk(5)], [("keT", u)])
            yield
            k.act(t(7), pP[:, 0, :], AF.Exp, [("pP", 0)], [tk(7)], scale=-1.0)
            yield
            k.act(t(7), t(7), AF.Ln, [tk(7)], [tk(7)], bias=1.0)
            yield
            k.act(t(7), t(7), AF.Exp, [tk(7)], [tk(7)], scale=-1.0)
            yield
            k.tt("dve", t(7), t(7), pP[:, 0, :], ALU.mult, [tk(7), ("pP", 0)], [tk(7)])
            yield
            k.tt("dve", qeT[:, u, :], t(7), t(4), ALU.mult, [tk(7), tk(4)], [("qeT", u)])
            yield
            k.act(t(8), pP[:, 2, :], AF.Exp, [("pP", 2)], [tk(8)], scale=-1.0)
            yield
            k.act(t(8), t(8), AF.Ln, [tk(8)], [tk(8)], bias=1.0)
            yield
            k.act(t(8), t(8), AF.Exp, [tk(8)], [tk(8)], scale=-1.0)
            yield
            k.tt("dve", t(8), t(8), pP[:, 2, :], ALU.mult, [tk(8), ("pP", 2)], [tk(8)])

        def chunks(g, h, pg=None, qg=None):
            def pull(n=1):
                if qg is not None:
                    next(qg, None)
                if pg is not None:
                    for _ in range(n):
                        next(pg, None)
            gp = g % 2
            u = (g * 4 + h) % 2
            hs = slice(h * 128, (h + 1) * 128)
            for c in range(8):
                cs = slice(c * 64, (c + 1) * 64)
                a2 = c % 2
                k.mm(pM[0:64, 0, 0:64], keT[:, u, cs], qeT[:, u, cs], True, True,
                     [("keT", u), ("qeT", u)], [("pM", 0)])
                k.tt("dve", at_sb[:, a2, :], pM[0:64, 0, 0:64], tri[:], ALU.mult, [("pM", 0), "tri"], [("at_sb", a2)])
                pull(2)
                k.mm(pOo[:, u, cs], state_b[:, h, :], qeT[:, u, cs], True, False,
                     [("state_b", h), ("qeT", u)], [("pOo", u)])
                k.mm(pOo[:, u, cs], v_sb[:, gp, c, hs], at_sb[:, a2, :], False, True,
                     [("v_sb", gp), ("at_sb", a2)], [("pOo", u)])
                k.tr(pM[0:64, 0, 128:256], k2T[:, u, cs], identf[:], [("k2T", u), "identf"], [("pM", 0)])
                k.cp("act", k2_sb[:, a2, :], pM[0:64, 0, 128:256], [("pM", 0)], [("k2_sb", a2)])
                pull(2)
                k.mm(pM[:, 1, 0:128], k2_sb[:, a2, :], v_sb[:, gp, c, hs], True, True,
                     [("k2_sb", a2), ("v_sb", gp)], [("pM", 1)])
                k.stt("dve", state_f[:, h, :], state_f[:, h, :], ebl[:, u, c:c + 1], pM[:, 1, 0:128],
                      ALU.mult, ALU.add, [("state_f", h), ("ebl", u), ("pM", 1)], [("state_f", h)])
                k.cp("act", state_b[:, h, :], state_f[:, h, :], [("state_f", h)], [("state_b", h)])
                pull(2)
                if k.cfg.get("convert", True) and NG == 8:
                    convert_step(k, T, cvf, cvb, cstep["r"])
                    cstep["r"] += 1

        def post(g, h):
            g5 = slice(g * 512, (g + 1) * 512)
            u = (g * 4 + h) % 2
            t = lambda i, u=u: tf[:, u, i, :]
            tk = lambda i, u=u: ("tf", u * NT + i)
            k.act(sq[:], pOo[:, u, :], AF.Square, [("pOo", u)], ["sq"])
            yield
            k.mm(pV[:], ones_b[:], sq[:], True, True, ["ones_b", "sq"], ["pV"])
            yield
            k.act(t(9), pV[:], AF.Ln, ["pV"], [tk(9)], scale=1.0 / 128, bias=EPS)
            yield
            k.act(t(9), t(9), AF.Exp, [tk(9)], [tk(9)], scale=-0.5)
            yield
            k.stt("dve", t(9), pOo[:, u, :], gnT[:, h:h + 1], t(9), ALU.mult, ALU.mult,
                  [("pOo", u), "gnT", tk(9)], [tk(9)])
            yield
            k.tt("dve", yo[:, u, :], t(9), t(8), ALU.mult, [tk(9), tk(8)], [("yo", u)])
            yield
            k.dma("sp", T["ya_s"][:, h, g5], yo[:, u, :], "yo%d" % u, [("yo", u)], [])

        units = [(g, h) for g in range(NG) for h in range(4)]
        vproj(0)
        for _ in prologue(*units[0]):
            pass
        qg = None
        for n, (g, h) in enumerate(units):
            pg = None
            if n + 1 < len(units):
                g1, h1 = units[n + 1]
                if h1 == 0:
                    vproj(g1)
                pg = prologue(g1, h1)
            chunks(g, h, pg, qg)
            if qg is not None:
                for _ in qg:
                    pass
            if pg is not None:
                for _ in pg:
                    pass
            qg = post(g, h)
        for _ in qg:
            pass
        if k.cfg.get("convert", True) and NG == 8:
            while cstep["r"] < 256 + NCV:
                convert_step(k, T, cvf, cvb, cstep["r"])
                cstep["r"] += 1
        s.barrier()


def phase_E1(k, T, sbt, pst, xnT, load_w):
    nc, s = k.nc, k.s
    NG = k.cfg.get("ngroups_e1", 8)
    with ExitStack() as st:
        wG = sbt(st, "wG", [128, 8, 2048], BF16)
        wU = sbt(st, "wU", [128, 2, 4, 1024], BF16)
        yab = sbt(st, "yab", [128, 2, 2, 4, 512], BF16)
        sg = sbt(st, "sg", [128, 2, 2, 512], F32)
        tmp = sbt(st, "e1tmp", [128, 2, 2, 512], F32)
        hTg = sbt(st, "hTg", [128, 2, 8, 512], BF16)
        pE = pst(st, "pE", [128, 2, 4, 512], F32)
        load_w(wG[:], "wG", T["w_in"][:, COL["ga"]:COL["ga"] + 2048], 8, 2048)
        load_w(wU[:, 0], "wU", T["w_up_a"], 4, 1024)
        load_w(wU[:, 1], "wU", T["w_up_b"], 4, 1024)
        for g in range(NG):
            gp = g % 2
            g5 = slice(g * 512, (g + 1) * 512)
            xk = ("xnT", 4 * g, 4 * g + 4)
            k.dma("sp", yab[:, gp, 0], T["ya_s"][:, :, g5], "yab%d" % gp, [], [("yab", gp)])
            k.dma("sp", yab[:, gp, 1], T["yb_s"][:, :, g5], "yab%d" % gp, [], [("yab", gp)])
            for nn in range(8):
                pb = nn % 2
                ns = slice(nn * 128, (nn + 1) * 128)
                for ab in range(2):
                    for kc in range(8):
                        k.mm(pE[:, pb, ab, :], wG[:, kc, ab * 1024 + nn * 128:ab * 1024 + (nn + 1) * 128], xnT[:, kc, g5],
                             kc == 0, kc == 7, [xk, "wG"], [("pE", pb * 4 + ab)])
                    for c4 in range(4):
                        k.mm(pE[:, pb, 2 + ab, :], wU[:, ab, c4, ns], yab[:, gp, ab, c4, :], c4 == 0, c4 == 3,
                             [("yab", gp), "wU"], [("pE", pb * 4 + 2 + ab)])
                for ab in range(2):
                    k.act(sg[:, pb, ab, :], pE[:, pb, ab, :], AF.Exp, [("pE", pb * 4 + ab)], [("sg", pb * 2 + ab)], scale=-1.0)
                    k.act(sg[:, pb, ab, :], sg[:, pb, ab, :], AF.Ln, [("sg", pb * 2 + ab)], [("sg", pb * 2 + ab)], bias=1.0)
                    k.act(sg[:, pb, ab, :], sg[:, pb, ab, :], AF.Exp, [("sg", pb * 2 + ab)], [("sg", pb * 2 + ab)], scale=-1.0)
                    k.tt("dve", tmp[:, pb, ab, :], sg[:, pb, ab, :], pE[:, pb, 2 + ab, :], ALU.mult,
                         [("sg", pb * 2 + ab), ("pE", pb * 4 + 2 + ab)], [("e1tmp", pb * 2 + ab)])
                k.tt("dve", hTg[:, gp, nn, :], tmp[:, pb, 0, :], tmp[:, pb, 1, :], ALU.add,
                     [("e1tmp", pb * 2), ("e1tmp", pb * 2 + 1)], [("hTg", gp)])
            k.dma("sp", T["hT_s"][:, :, g5], hTg[:, gp], "hTg%d" % gp, [("hTg", gp)], [])
        s.barrier()


def peer_routing_alloc(sbt, st, pfx="r"):
    B = {}
    B["vals"] = sbt(st, pfx + "vals", [128, 16, 16], F32)
    B["idxs"] = sbt(st, pfx + "idxs", [128, 16, 16], U32)
    B["idxf"] = sbt(st, pfx + "idxf", [128, 16, 16], F32)
    B["s2"] = sbt(st, pfx + "s2", [128, 128], F32)
    B["cand"] = sbt(st, pfx + "cand", [128, 8, 256], F32)
    B["cand2"] = sbt(st, pfx + "cand2", [128, 8, 256], F32)
    B["tops"] = sbt(st, pfx + "tops", [128, 8, 16], F32)
    B["pos"] = sbt(st, pfx + "pos", [128, 8, 16], U32)
    B["ipos"] = sbt(st, pfx + "ipos", [128, 8, 16], U32)
    B["jpos"] = sbt(st, pfx + "jpos", [128, 8, 16], U32)
    B["iposf"] = sbt(st, pfx + "iposf", [128, 8, 16], F32)
    B["jposf"] = sbt(st, pfx + "jposf", [128, 8, 16], F32)
    B["eq"] = sbt(st, pfx + "eq", [128, 8, 16, 16], F32)
    B["sel1"] = sbt(st, pfx + "sel1", [128, 8, 16], F32)
    B["sel2"] = sbt(st, pfx + "sel2", [128, 8, 16], F32)
    B["gsum"] = sbt(st, pfx + "gsum", [128, 8], F32)
    return B


def peer_routing(k, B, s_sb, s_key, iota16, eidx, eidx_key, gw, gw_key, pfx="r"):
    vals, idxs, idxf, s2, cand, cand2 = B["vals"], B["idxs"], B["idxf"], B["s2"], B["cand"], B["cand2"]
    tops, pos, ipos, jpos, iposf, jposf = B["tops"], B["pos"], B["ipos"], B["jpos"], B["iposf"], B["jposf"]
    eq, sel1, sel2, gsum = B["eq"], B["sel1"], B["sel2"], B["gsum"]
    K_ = pfx
    for l in range(16):
        k.gen("dve", lambda e, l=l: e.max(out=vals[:, l, 0:8], in_=s_sb[:, l, :]), [s_key], [K_ + "vals"])
        k.gen("dve", lambda e, l=l: e.max_index(out=idxs[:, l, 0:8], in_max=vals[:, l, 0:8], in_values=s_sb[:, l, :]),
              [s_key, K_ + "vals"], [K_ + "idxs"])
        k.gen("dve", lambda e, l=l: e.match_replace(out=s2[:], in_to_replace=vals[:, l, 0:8], in_values=s_sb[:, l, :],
                                                    imm_value=-1e30), [s_key, K_ + "vals"], [K_ + "s2"])
        k.gen("dve", lambda e, l=l: e.max(out=vals[:, l, 8:16], in_=s2[:]), [K_ + "s2"], [K_ + "vals"])
        k.gen("dve", lambda e, l=l: e.max_index(out=idxs[:, l, 8:16], in_max=vals[:, l, 8:16], in_values=s2[:]),
              [K_ + "s2", K_ + "vals"], [K_ + "idxs"])
        yield
    k.cp("dve", idxf[:], idxs[:], [K_ + "idxs"], [K_ + "idxf"])
    v4 = vals[:].rearrange("p (h two) i -> p h two i", two=2)
    x4 = idxf[:].rearrange("p (h two) i -> p h two i", two=2)
    c4 = cand[:].rearrange("p h (i j) -> p h i j", j=16)
    k.tt("dve", c4, v4[:, :, 0, :].unsqueeze(3).broadcast_to([128, 8, 16, 16]),
         v4[:, :, 1, :].unsqueeze(2).broadcast_to([128, 8, 16, 16]), ALU.add, [K_ + "vals"], [K_ + "cand"])
    for h in range(8):
        k.gen("dve", lambda e, h=h: e.max(out=tops[:, h, 0:8], in_=cand[:, h, :]), [K_ + "cand"], [K_ + "tops"])
        k.gen("dve", lambda e, h=h: e.max_index(out=pos[:, h, 0:8], in_max=tops[:, h, 0:8], in_values=cand[:, h, :]),
              [K_ + "cand", K_ + "tops"], [K_ + "pos"])
        k.gen("dve", lambda e, h=h: e.match_replace(out=cand2[:, h, :], in_to_replace=tops[:, h, 0:8],
                                                    in_values=cand[:, h, :], imm_value=-1e30),
              [K_ + "cand", K_ + "tops"], [K_ + "cand2"])
        k.gen("dve", lambda e, h=h: e.max(out=tops[:, h, 8:16], in_=cand2[:, h, :]), [K_ + "cand2"], [K_ + "tops"])
        k.gen("dve", lambda e, h=h: e.max_index(out=pos[:, h, 8:16], in_max=tops[:, h, 8:16], in_values=cand2[:, h, :]),
              [K_ + "cand2", K_ + "tops"], [K_ + "pos"])
        yield
    k.tsc("dve", ipos[:], pos[:], 4, None, ALU.logical_shift_right, None, [K_ + "pos"], [K_ + "ipos"])
    k.tsc("dve", jpos[:], pos[:], 15, None, ALU.bitwise_and, None, [K_ + "pos"], [K_ + "jpos"])
    k.cp("dve", iposf[:], ipos[:], [K_ + "ipos"], [K_ + "iposf"])
    k.cp("dve", jposf[:], jpos[:], [K_ + "jpos"], [K_ + "jposf"])
    io4 = iota16[:].unsqueeze(1).unsqueeze(1).broadcast_to([128, 8, 16, 16])
    for (pf_, xi, sel) in ((iposf, 0, sel1), (jposf, 1, sel2)):
        k.tt("dve", eq[:], io4, pf_[:].unsqueeze(3).broadcast_to([128, 8, 16, 16]), ALU.is_equal,
             ["iota16", K_ + "iposf", K_ + "jposf"], [K_ + "eq"])
        k.tt("dve", eq[:], eq[:], x4[:, :, xi, :].unsqueeze(2).broadcast_to([128, 8, 16, 16]), ALU.mult,
             [K_ + "eq", K_ + "idxf"], [K_ + "eq"])
        k.tred("dve", sel[:], eq[:], ALU.add, [K_ + "eq"], [K_ + "sel"])
    yield
    k.stt("dve", sel1[:], sel1[:], 128.0, sel2[:], ALU.mult, ALU.add, [K_ + "sel"], [K_ + "sel"])
    k.cp("dve", eidx.rearrange("p (h i) -> p h i", i=16), sel1[:], [K_ + "sel"], [eidx_key])
    k.tt("dve", gw, tops[:], tops[:, :, 0:1].broadcast_to([128, 8, 16]), ALU.subtract, [K_ + "tops"], [gw_key])
    k.act(gw, gw, AF.Exp, [gw_key], [gw_key])
    k.tred("dve", gsum[:], gw, ALU.add, [gw_key], [K_ + "gsum"])
    k.recip(gsum[:], gsum[:], [K_ + "gsum"], [K_ + "gsum"])
    k.tt("dve", gw, gw, gsum[:].unsqueeze(2).broadcast_to([128, 8, 16]), ALU.mult, [gw_key, K_ + "gsum"], [gw_key])


def phase_E2(k, T, sbt, pst, identb, identf, load_w):
    nc, s = k.nc, k.s
    NB_ = k.cfg.get("nblk_e2", NBLK)
    NU = 11
    GS = 4
    with ExitStack() as st:
        wO = sbt(st, "wO", [128, 8, 1024], BF16)
        wQ = sbt(st, "wQ", [128, 8, 2048], BF16)
        keysT = sbt(st, "keysT", [128, 16, 128], BF16)
        g2 = sbt(st, "g2", [128, 1024], F32)
        g3 = sbt(st, "g3", [128, 1024], F32)
        iota16 = sbt(st, "iota16", [128, 16], F32)
        load_w(wO[:], "wO", T["w_out"], 8, 1024)
        load_w(wQ[:], "wQ", T["peer_wq"], 8, 2048)
        k.dma("sp", g2[:], T["norm_ffn"].partition_broadcast(128), "c0", [], ["g2"])
        k.dma("sp", g3[:], T["norm_final"].partition_broadcast(128), "c0", [], ["g3"])
        k.dma("sp", iota16[:], T["c_iota16"], "c0", [], ["iota16"])
        with ExitStack() as st2:
            kst = sbt(st2, "kst", [128, 2, 128], F32)
            pK = pst(st2, "pK", [128, 128], F32)
            for l in range(16):
                h_, p_ = l // 2, l % 2
                kb = l % 2
                k.dma("sp", kst[:, kb, :], T["peer_keys"][p_ * 8 + h_], "kst%d" % kb, [], [("kst", kb)])
                k.tr(pK[:], kst[:, kb, :], identf[:], [("kst", kb), "identf"], ["pK"])
                k.cp("act", keysT[:, l, :], pK[:], ["pK"], ["keysT"])
            s.barrier()

        hTb = sbt(st, "hTb", [128, 2, 8, 128], BF16)
        xb = sbt(st, "xb", [128, 1, 2, 512], F32)
        x1 = sbt(st, "x1", [128, 2, 2, 512], F32)
        junk = sbt(st, "junk", [128, 1024], BF16)
        junkb = sbt(st, "junkb", [128, 1024], BF16)
        junkb2 = sbt(st, "junkb2", [128, 1024], BF16)
        prodb = sbt(st, "prodb", [128, 3, 1024], BF16)
        ssq = sbt(st, "ssq", [128, 4], F32)
        xn2f = sbt(st, "xn2f", [128, 1024], F32)
        xn2b = sbt(st, "xn2b", [128, 2, 1024], BF16)
        xn2T = sbt(st, "xn2T", [128, 8, 128], BF16)
        qTs = sbt(st, "qTs", [128, 8, 128], BF16)
        s_sb = sbt(st, "s_sb", [128, 16, 128], F32)
        eidx = sbt(st, "eidx", [128, 2, 128], I32)
        gw = sbt(st, "gw", [128, 2, 8, 16], F32)
        uvg = sbt(st, "uvg", [128, NU, 2048], BF16)
        dg = sbt(st, "dg", [128, 4, 128], BF16)
        hcol = sbt(st, "hcol", [128, 128], F32)
        acol = sbt(st, "acol", [128, 128], F32)
        ob = sbt(st, "ob", [128, 2, 512], F32)
        RB = peer_routing_alloc(sbt, st)
        pY = pst(st, "pY", [128, 2, 512], F32)
        pG = pst(st, "pG", [128, 2, 512], F32)
        pT2 = pst(st, "pT2", [128, 8, 128], BF16)
        pQ = pst(st, "pQ", [128, 8, 128], F32)

        def front(b):
            p = b % 2
            tb = slice(b * 128, (b + 1) * 128)
            k.dma("sp", hTb[:, p], T["hT_s"][:, :, tb], "hTb%d" % p, [], [("hTb", p)])
            k.dma("sp", xb[:, 0], T["x"][tb, :].rearrange("t (a n) -> t a n", a=2), "xb0", [], [("xb", 0)])
            for half in range(2):
                for c in range(8):
                    k.mm(pY[:, half, :], hTb[:, p, c, :], wO[:, c, half * 512:(half + 1) * 512], c == 0, c == 7,
                         [("hTb", p), "wO"], ["pY"])
                yield
            k.tt("dve", x1[:, p], xb[:, 0], pY[:], ALU.add, [("xb", 0), "pY"], [("x1", p)])
            x1f = x1[:, p].rearrange("p a n -> p (a n)")
            k.act(junk[:], x1f, AF.Square, [("x1", p)], ["junk", ("ssq", p)], accum=ssq[:, p:p + 1])
            k.act(ssq[:, p:p + 1], ssq[:, p:p + 1], AF.Ln, [("ssq", p)], [("ssq", p)], scale=1.0 / D, bias=EPS)
            k.act(ssq[:, p:p + 1], ssq[:, p:p + 1], AF.Exp, [("ssq", p)], [("ssq", p)], scale=-0.5)
            k.stt("dve", xn2f[:], x1f, ssq[:, p:p + 1], g2[:], ALU.mult, ALU.mult, [("x1", p), ("ssq", p), "g2"], ["xn2f"])
            k.cp("act", xn2b[:, p, :], xn2f[:], ["xn2f"], [("xn2b", p)])
            for c in range(8):
                k.tr(pT2[:, c, :], xn2b[:, p, c * 128:(c + 1) * 128], identb[:], [("xn2b", p), "identb"], ["pT2"])
            k.cp("act", xn2T[:], pT2[:], ["pT2"], ["xn2T"])
            yield
            for l0 in (0, 8):
                for l in range(8):
                    for kc in range(8):
                        k.mm(pQ[:, l, :], wQ[:, kc, (l0 + l) * 128:(l0 + l + 1) * 128], xn2T[:, kc, :], kc == 0, kc == 7,
                             ["xn2T", "wQ"], [("pQ", l // 4)])
                    yield
                for hb in range(2):
                    ls = slice(hb * 4, hb * 4 + 4)
                    k.cp("act", qTs[:, ls, :], pQ[:, ls, :], [("pQ", hb)], [("qTs", hb)])
                yield
                for l in range(8):
                    k.mm(pQ[:, l, :], qTs[:, l, :], keysT[:, l0 + l, :], True, True, [("qTs", l // 4), "keysT"], [("pQ", l // 4)])
                for hb in range(2):
                    ls = slice(hb * 4, hb * 4 + 4)
                    k.cp("act", s_sb[:, l0 + hb * 4:l0 + hb * 4 + 4, :], pQ[:, ls, :], [("pQ", hb)], ["s_sb"])
                yield
            yield from peer_routing(k, RB, s_sb, "s_sb", iota16, eidx[:, p, :], ("eidx", p), gw[:, p], ("gw", p))

        def gath(b, fg):
            p = b % 2
            tb = slice(b * 128, (b + 1) * 128)
            x1f = x1[:, p].rearrange("p a n -> p (a n)")
            gwf = gw[:, p].rearrange("p h i -> p (h i)")
            for kk in range(128):
                ub = kk % NU
                db = kk % 4
                hk = ("hcol", kk % 8)
                ak = ("acol", kk % 8)
                k.s.dma("pool", lambda e, kk=kk, ub=ub, p=p: e.indirect_dma_start(
                    out=uvg[:, ub, :], out_offset=None, in_=T["uv_s"][:, :],
                    in_offset=bass.IndirectOffsetOnAxis(ap=eidx[:, p, kk:kk + 1], axis=0)),
                    "uvg%d" % ub, [("eidx", p), "uv_s"], [("uvg", ub)])
                k.stt("dve", junkb[:], uvg[:, ub, 0:1024], 1.0, xn2b[:, p, :], ALU.mult, ALU.mult,
                      [("uvg", ub), ("xn2b", p)], ["junkb", hk], accum=hcol[:, kk:kk + 1])
                k.act(acol[:, kk:kk + 1], hcol[:, kk:kk + 1], AF.Gelu, [hk], [ak])
                k.act(acol[:, kk:kk + 1], acol[:, kk:kk + 1], AF.Copy, [ak, ("gw", p)], [ak], scale=gwf[:, kk:kk + 1])
                k.act(dg[:, db, :], identb[:], AF.Copy, ["identb", ak], [("dg", db)], scale=acol[:, kk:kk + 1])
                for half in range(2):
                    k.mm(pG[:, half, :], dg[:, db, :], uvg[:, ub, 1024 + half * 512:1024 + (half + 1) * 512],
                         kk == 0, kk == 127, [("dg", db), ("uvg", ub)], ["pG"])
                if fg is not None:
                    next(fg, None)
            if fg is not None:
                for _ in fg:
                    pass
            k.tt("dve", x1[:, p], x1[:, p], pG[:], ALU.add, [("x1", p), "pG"], [("x1", p)])
            k.act(junk[:], x1f, AF.Square, [("x1", p)], ["junk", ("ssq", 2)], accum=ssq[:, 2:3])
            k.act(ssq[:, 2:3], ssq[:, 2:3], AF.Ln, [("ssq", 2)], [("ssq", 2)], scale=1.0 / D, bias=EPS)
            k.act(ssq[:, 2:3], ssq[:, 2:3], AF.Exp, [("ssq", 2)], [("ssq", 2)], scale=-0.5)
            k.stt("dve", ob[:].rearrange("p a n -> p (a n)"), x1f, ssq[:, 2:3], g3[:], ALU.mult, ALU.mult,
                  [("x1", p), ("ssq", 2), "g3"], ["ob"])
            k.dma("sp", T["out"][tb, :].rearrange("t (a n) -> t a n", a=2), ob[:], "ob", ["ob"], [])

        for _ in front(0):
            pass
        for b in range(NB_):
            gath(b, front(b + 1) if b + 1 < NB_ else None)
        s.barrier()


_NC_CACHE = {}


def _core_inputs(inp, b, consts):
    d = {
        "x": np.ascontiguousarray(inp["x"][b], dtype=np.float32),
        "norm_mix": np.asarray(inp["norm_mix"], np.float32).reshape(1, D),
        "w_in": np.ascontiguousarray(np.asarray(inp["w_in"], np.float32)[0]),
        "hg_lb": np.asarray(inp["hg_lb"], np.float32).reshape(2, 512),
        "hg_norm": np.asarray(inp["hg_norm"], np.float32).reshape(1, 512),
        "idx_k_norm_g": np.asarray(inp["idx_k_norm_g"], np.float32).reshape(1, 64),
        "idx_k_norm_b": np.asarray(inp["idx_k_norm_b"], np.float32).reshape(1, 64),
        "w_up_a": np.ascontiguousarray(np.asarray(inp["w_up_a"], np.float32)[0]),
        "w_up_b": np.ascontiguousarray(np.asarray(inp["w_up_b"], np.float32)[0]),
        "w_out": np.ascontiguousarray(np.asarray(inp["w_out"], np.float32)[0]),
        "norm_ffn": np.asarray(inp["norm_ffn"], np.float32).reshape(1, D),
        "peer_wq": np.ascontiguousarray(np.asarray(inp["peer_wq"], np.float32)[0]),
        "peer_keys": np.ascontiguousarray(np.asarray(inp["peer_keys"], np.float32)[0]).reshape(16, 128, 128),
        "peer_u": np.ascontiguousarray(np.asarray(inp["peer_u"], np.float32)[0]),
        "peer_v": np.ascontiguousarray(np.asarray(inp["peer_v"], np.float32)[0]),
        "norm_final": np.asarray(inp["norm_final"], np.float32).reshape(1, D),
    }
    d.update(consts)
    return d


def kernel(**inputs):
    if "nc" not in _NC_CACHE:
        _NC_CACHE["nc"] = build()
    nc = _NC_CACHE["nc"]
    consts = host_consts(np.asarray(inputs["rel_bias"], np.float32))
    shared = _core_inputs(inputs, 0, consts)
    in_maps = []
    for b in range(NCORES):
        d = dict(shared)
        d["x"] = np.ascontiguousarray(np.asarray(inputs["x"])[b], dtype=np.float32)
        in_maps.append(d)
    res = run_bass_kernel_spmd(nc, in_maps, core_ids=list(range(NCORES)))
    out = np.stack([np.asarray(r["out"], dtype=np.float32) for r in res.results], axis=0)
    return out
```

```python
import math
from contextlib import ExitStack

import numpy as np
import ml_dtypes
import concourse.bass as bass
import concourse.mybir as mybir
from concourse.bass_utils import run_bass_kernel_spmd

F32 = mybir.dt.float32
BF16 = mybir.dt.bfloat16
I32 = mybir.dt.int32
U32 = mybir.dt.uint32
ALU = mybir.AluOpType
AF = mybir.ActivationFunctionType
AX = mybir.AxisListType

S = 4096
D = 1024
NBLK = 32
NCORES = 8
COL = dict(hq=0, hf=512, hi=1024, hog=1536, aq=2048, ak=2560, av=2624, iq=2688, ik=2944,
           iw=3008, ga=3012, gb=4036)
IN_WIDTH = 5060
EPS = 1e-6
NEG = -30000.0
NROUNDS = 16


class Sched:
    ENG = ("pe", "dve", "act", "pool", "sp")

    def __init__(self, nc):
        self.nc = nc
        self.q = {e: [] for e in self.ENG}
        self.cnt = {}
        self.seen = {e: {} for e in self.ENG}
        self.w = {}
        self.r = {}
        self.excl = set()

    def _split(self, reads, writes):
        reads = self._units(reads)
        writes = self._units(writes)
        ex = [u for u in reads if u[0] in self.excl]
        if ex:
            reads = [u for u in reads if u[0] not in self.excl]
            writes = writes + [u for u in ex if u not in writes]
        return reads, writes

    @staticmethod
    def _units(specs):
        out = []
        for s in specs:
            if isinstance(s, str):
                out.append((s, 0))
            elif len(s) == 2:
                out.append((s[0], s[1]))
            else:
                for i in range(s[1], s[2]):
                    out.append((s[0], i))
        return out

    def _deps(self, eng, reads, writes):
        deps = {}

        def add(ev, kind):
            if ev is None:
                return
            sem, val = ev
            if sem == "E:" + eng and eng == "pe":
                return
            if deps.get(sem, 0) < val:
                deps[sem] = val

        for u in reads:
            add(self.w.get(u), "raw")
        for u in writes:
            add(self.w.get(u), "waw")
            for sem, val in self.r.get(u, {}).items():
                add((sem, val), "war")
        waits = []
        seen = self.seen[eng]
        for sem, val in deps.items():
            if seen.get(sem, 0) < val:
                seen[sem] = val
                waits.append((sem, val))
        return waits

    def _register(self, ev, reads, writes):
        sem, val = ev
        for u in reads:
            d = self.r.setdefault(u, {})
            if d.get(sem, 0) < val:
                d[sem] = val
        for u in writes:
            self.w[u] = ev
            self.r[u] = {}

    def op(self, eng, fn, reads=(), writes=()):
        reads, writes = self._split(reads, writes)
        waits = self._deps(eng, reads, writes)
        sem = "E:" + eng
        self.cnt[sem] = self.cnt.get(sem, 0) + 1
        ev = (sem, self.cnt[sem])
        self.q[eng].append((fn, waits, (sem, 1)))
        self._register(ev, reads, writes)
        return ev

    def dma(self, queue, fn, sem, reads=(), writes=()):
        reads, writes = self._split(reads, writes)
        waits = self._deps(queue, reads, writes)
        sem = "D:" + sem
        prev = self.cnt.get(sem, 0)
        if prev and self.seen[queue].get(sem, 0) < prev:
            self.seen[queue][sem] = prev
            waits.append((sem, prev))
        self.cnt[sem] = self.cnt.get(sem, 0) + 16
        ev = (sem, self.cnt[sem])
        self.q[queue].append((fn, waits, (sem, 16)))
        self._register(ev, reads, writes)
        return ev

    def barrier(self, engs=None):
        for eng in (engs or self.ENG):
            waits = []
            for sem, val in self.cnt.items():
                if sem == "E:" + eng:
                    continue
                if self.seen[eng].get(sem, 0) < val:
                    self.seen[eng][sem] = val
                    waits.append((sem, val))
            if waits:
                self.q[eng].append((None, waits, None))

    def emit(self):
        nc = self.nc
        with ExitStack() as st:
            handles = {}
            for name in self.cnt:
                handles[name] = st.enter_context(nc.semaphore(name.replace(":", "_")))
            block = st.enter_context(nc.Block())
            engobjs = {"pe": block.tensor, "dve": block.vector, "act": block.scalar,
                       "pool": block.gpsimd, "sp": block.sync}

            def make(ename):
                lst = self.q[ename]

                def body(e):
                    for fn, waits, inc in lst:
                        for sem, val in waits:
                            e.wait_ge(handles[sem], val)
                        if fn is not None:
                            ins = fn(e)
                            ins.then_inc(handles[inc[0]], inc[1])
                return body

            for ename in self.ENG:
                if self.q[ename]:
                    engobjs[ename](make(ename))


class K:
    def __init__(self, nc, cfg):
        self.nc = nc
        self.cfg = cfg
        self.s = Sched(nc)
        self.uid = 0

    def mm(self, out, lhsT, rhs, start, stop, r, w):
        self.s.op("pe", lambda e: e.matmul(out, lhsT=lhsT, rhs=rhs, start=start, stop=stop), r, w)

    def tr(self, out, in_, ident, r, w):
        self.s.op("pe", lambda e: e.transpose(out=out, in_=in_, identity=ident), r, w)

    def act(self, out, in_, func, r, w, scale=1.0, bias=0.0, accum=None):
        if accum is None:
            self.s.op("act", lambda e: e.activation(out=out, in_=in_, func=func, bias=bias, scale=scale), r, w)
        else:
            self.s.op("act", lambda e: e.activation(out=out, in_=in_, func=func, bias=bias, scale=scale,
                                                    accum_out=accum), r, w)

    def tsc(self, eng, out, in0, s1, s2, op0, op1, r, w, accum=None):
        if op1 is None:
            self.s.op(eng, lambda e: e.tensor_scalar(out=out, in0=in0, scalar1=s1, scalar2=None, op0=op0), r, w)
        elif accum is None:
            self.s.op(eng, lambda e: e.tensor_scalar(out=out, in0=in0, scalar1=s1, scalar2=s2, op0=op0, op1=op1), r, w)
        else:
            self.s.op(eng, lambda e: e.tensor_scalar(out=out, in0=in0, scalar1=s1, scalar2=s2, op0=op0, op1=op1,
                                                     accum_out=accum), r, w)

    def stt(self, eng, out, in0, scalar, in1, op0, op1, r, w, accum=None):
        if accum is None:
            self.s.op(eng, lambda e: e.scalar_tensor_tensor(out=out, in0=in0, scalar=scalar, in1=in1, op0=op0, op1=op1), r, w)
        else:
            self.s.op(eng, lambda e: e.scalar_tensor_tensor(out=out, in0=in0, scalar=scalar, in1=in1, op0=op0, op1=op1,
                                                            accum_out=accum), r, w)

    def tt(self, eng, out, in0, in1, op, r, w):
        self.s.op(eng, lambda e: e.tensor_tensor(out=out, in0=in0, in1=in1, op=op), r, w)

    def tred(self, eng, out, in_, op, r, w):
        self.s.op(eng, lambda e: e.tensor_reduce(out=out, in_=in_, axis=AX.X, op=op), r, w)

    def cp(self, eng, out, in_, r, w):
        if eng == "act":
            self.s.op("act", lambda e: e.copy(out=out, in_=in_), r, w)
        else:
            self.s.op(eng, lambda e: e.tensor_copy(out=out, in_=in_), r, w)

    def recip(self, out, in_, r, w):
        self.s.op("dve", lambda e: e.reciprocal(out=out, in_=in_), r, w)

    def memset(self, eng, ap, val, w):
        self.s.op(eng, lambda e: e.memset(ap, val), (), w)

    def dma(self, q, out, in_, sem, r, w):
        self.s.dma(q, lambda e: e.dma_start(out=out, in_=in_), sem, r, w)

    def gen(self, eng, fn, r, w):
        self.s.op(eng, fn, r, w)


def t5_bucket_np(n):
    n = np.maximum(n, 0)
    nf = np.maximum(n, 1).astype(np.float32)
    large = 16 + (np.log(nf / np.float32(16)) / np.float32(math.log(128 / 16)) * np.float32(16)).astype(np.int32)
    large = np.minimum(large, 31)
    return np.where(n < 16, n, large)


def host_consts(rel_bias):
    c = {}
    c["c_ident"] = np.eye(128, dtype=np.float32)
    tl = np.arange(128)
    c["c_cmask"] = np.where(tl[None, :] <= tl[:, None], 0.0, -1e30).astype(np.float32)
    t64 = np.arange(64)
    c["c_tri"] = (t64[:, None] <= t64[None, :]).astype(np.float32)
    cm = np.ones((128, 512), np.float32)
    cm[:, ::64] = 0.0
    c["c_chunkmask"] = cm
    e65 = np.zeros((65, 64), np.float32)
    e65[64, :] = 1.0
    c["c_e65"] = e65
    sel = np.zeros((64, 256), np.float32)
    sel[np.arange(64), np.arange(64)] = 1.0
    sel[np.arange(64), 128 + 64 + np.arange(64)] = 1.0
    c["c_sel"] = sel
    c["c_iota16"] = np.tile(np.arange(16, dtype=np.float32)[None, :], (128, 1))
    sl = np.arange(128)[:, None]
    tt_ = np.arange(128)[None, :]
    bd = t5_bucket_np(tt_ - sl)
    bp = t5_bucket_np(128 + tt_ - sl)
    rb = np.asarray(rel_bias, np.float32)
    c["c_bd"] = np.ascontiguousarray(rb[bd].transpose(0, 2, 1))
    c["c_bp"] = np.ascontiguousarray(rb[bp].transpose(0, 2, 1))
    c["c_b31"] = np.ascontiguousarray(np.broadcast_to(rb[31][None, :, None], (128, 8, 128)))
    return c


CONST_SHAPES = {
    "c_ident": [128, 128], "c_cmask": [128, 128], "c_tri": [64, 64], "c_chunkmask": [128, 512],
    "c_e65": [65, 64], "c_sel": [64, 256], "c_iota16": [128, 16],
    "c_bd": [128, 8, 128], "c_bp": [128, 8, 128], "c_b31": [128, 8, 128],
}

INPUT_SHAPES = {
    "x": [S, D], "norm_mix": [1, D], "w_in": [D, IN_WIDTH], "hg_lb": [2, 512], "hg_norm": [1, 512],
    "idx_k_norm_g": [1, 64], "idx_k_norm_b": [1, 64], "w_up_a": [512, D], "w_up_b": [512, D],
    "w_out": [D, D], "norm_ffn": [1, D], "peer_wq": [D, 2048], "peer_keys": [16, 128, 128],
    "peer_u": [16384, D], "peer_v": [16384, D], "norm_final": [1, D],
}

SCRATCH = {
    "ya_s": ([128, 4, S], BF16), "yb_s": ([128, 4, S], BF16), "hT_s": ([128, 8, S], BF16),
    "uv_s": ([16384, 2048], BF16),
}


def build(cfg=None):
    cfg = cfg or {}
    phases = cfg.get("phases", ["A", "BC", "D", "E1", "E2"])
    inject = cfg.get("inject", [])
    taps = cfg.get("taps", [])
    nc = bass.Bass("TRN2", target_bir_lowering=False)
    k = K(nc, cfg)
    s = k.s
    T = {}
    for name, shp in INPUT_SHAPES.items():
        T[name] = nc.dram_tensor(name, shp, F32, kind="ExternalInput").ap()
    for name, shp in CONST_SHAPES.items():
        T[name] = nc.dram_tensor(name, shp, F32, kind="ExternalInput").ap()
    for name, (shp, dt) in SCRATCH.items():
        kind = "ExternalInput" if name in inject else ("ExternalOutput" if name in taps else "Internal")
        T[name] = nc.dram_tensor(name, shp, dt, kind=kind).ap()
    T["out"] = nc.dram_tensor("out", [S, D], F32, kind="ExternalOutput").ap()

    with ExitStack() as top, nc.allow_low_precision("bf16 matmul operands, fp32 accumulation"), \
            nc.allow_non_contiguous_dma("small strided constant loads"):
        def sbt(st, name, shape, dt):
            return st.enter_context(nc.sbuf_tensor(name, shape, dt))

        def pst(st, name, shape, dt):
            s.excl.add(name)
            return st.enter_context(nc.psum_tensor(name, shape, dt))

        identf = sbt(top, "identf", [128, 128], F32)
        identb = sbt(top, "identb", [128, 128], BF16)
        wst = sbt(top, "wst", [128, 2, 8, 128], F32)
        k.dma("sp", identf[:], T["c_ident"], "c0", [], ["identf"])
        k.cp("dve", identb[:], identf[:], ["identf"], ["identb"])
        wstate = {"n": 0}

        def load_w(dst, dst_key, src, C, n):
            for c0 in range(0, n, 128):
                wdt = min(128, n - c0)
                b = wstate["n"] % 2
                wstate["n"] += 1
                k.dma("sp", wst[:, b, 0:C, 0:wdt], src[:, c0:c0 + wdt].rearrange("(c p) n -> p c n", p=128),
                      "wst%d" % b, [], [("wst", b)])
                k.cp("pool", dst[:, :, c0:c0 + wdt], wst[:, b, 0:C, 0:wdt], [("wst", b)], [dst_key])

        with ExitStack() as mid:
            xnT = sbt(mid, "xnT", [128, 8, S], BF16)
            if "A" in phases:
                phase_A(k, T, sbt, pst, xnT, identb)
            if "BC" in phases:
                phase_BC(k, T, sbt, pst, xnT, identb, identf, load_w)
            if "D" in phases:
                phase_D(k, T, sbt, pst, xnT, identb, identf, load_w)
            if "E1" in phases:
                phase_E1(k, T, sbt, pst, xnT, load_w)
            s.barrier()
        if "E2" in phases:
            phase_E2(k, T, sbt, pst, identb, identf, load_w)
        s.barrier()
        s.emit()
    return nc


def phase_A(k, T, sbt, pst, xnT, identb):
    nc, s = k.nc, k.s
    with ExitStack() as st:
        xt = sbt(st, "A_xt", [128, 2, D], F32)
        gb = sbt(st, "A_gb", [128, D], F32)
        junk = sbt(st, "A_junk", [128, D], F32)
        ss = sbt(st, "A_ss", [128, 2], F32)
        rstd = sbt(st, "A_rstd", [128, 2], F32)
        xs = sbt(st, "A_xs", [128, 2, D], BF16)
        pt = pst(st, "A_pt", [128, 2, 8, 128], BF16)
        k.dma("sp", gb[:], T["norm_mix"].partition_broadcast(128), "c0", [], ["A_gb"])
        for b in range(k.cfg.get("nblk_a", NBLK)):
            p = b % 2
            k.dma("sp", xt[:, p, :], T["x"][b * 128:(b + 1) * 128, :], "A_x%d" % p, [], [("A_xt", p)])
            k.act(junk[:], xt[:, p, :], AF.Square, [("A_xt", p)], ["A_junk", ("A_ss", p)], accum=ss[:, p:p + 1])
            k.act(rstd[:, p:p + 1], ss[:, p:p + 1], AF.Ln, [("A_ss", p)], [("A_rstd", p)], scale=1.0 / D, bias=EPS)
            k.act(rstd[:, p:p + 1], rstd[:, p:p + 1], AF.Exp, [("A_rstd", p)], [("A_rstd", p)], scale=-0.5)
            k.stt("dve", xs[:, p, :], xt[:, p, :], rstd[:, p:p + 1], gb[:], ALU.mult, ALU.mult,
                  [("A_xt", p), ("A_rstd", p), "A_gb"], [("A_xs", p)])
            for c in range(8):
                k.tr(pt[:, p, c, :], xs[:, p, c * 128:(c + 1) * 128], identb[:], [("A_xs", p), "identb"], [("A_pt", p)])
            k.cp("act", xnT[:, :, b * 128:(b + 1) * 128], pt[:, p, :, :], [("A_pt", p)], [("xnT", b)])
        s.barrier()


NCV = 4


def convert_step(k, T, cvf, cvb, r):
    def src_of(q):
        tile_, which = q // 2, q % 2
        rs = slice(tile_ * 128, (tile_ + 1) * 128)
        return (T["peer_u"] if which == 0 else T["peer_v"])[rs, :], T["uv_s"][rs, which * 1024:(which + 1) * 1024]
    if r < 256:
        fb = r % NCV
        k.dma("sp", cvf[:, fb, :], src_of(r)[0], "cvf%d" % fb, [], [("cvf", fb)])
    q = r - (NCV - 1)
    if 0 <= q < 256:
        fb, cb = q % NCV, q % 2
        k.cp("act", cvb[:, cb, :], cvf[:, fb, :], [("cvf", fb)], [("cvb", cb)])
        k.dma("sp", src_of(q)[1], cvb[:, cb, :], "cvst%d" % cb, [("cvb", cb)], ["uv_s"])


def phase_BC(k, T, sbt, pst, xnT, identb, identf, load_w):
    nc, s = k.nc, k.s
    with ExitStack() as st:
        wB = sbt(st, "wB", [128, 8, 452], BF16)
        wC = sbt(st, "wC", [128, 8, 768], BF16)
        kT = sbt(st, "kT", [64, S], BF16)
        iknT = sbt(st, "iknT", [64, S], BF16)
        v_aug = sbt(st, "v_aug", [128, NBLK, 65], BF16)
        iws = sbt(st, "iws", [128, NBLK, 4], F32)
        gI = sbt(st, "gI", [128, 64], F32)
        bI = sbt(st, "bI", [128, 64], F32)
        cmask = sbt(st, "cmask", [128, 128], F32)
        e65 = sbt(st, "e65", [65, 64], F32)
        self_f = sbt(st, "sel_f", [64, 256], F32)
        sel_b = sbt(st, "sel_b", [64, 256], BF16)
        i8 = sbt(st, "i8", [128, 8, 128], BF16)
        bnd = sbt(st, "bnd", [128, 2, 8, 128], BF16)
        ikf = sbt(st, "ikf", [128, 64], F32)
        ikb = sbt(st, "ikb", [128, 64], BF16)
        stats = sbt(st, "stats", [128, 6], F32)
        mv = sbt(st, "mv", [128, 2], F32)
        irs = sbt(st, "irs", [128, 1], F32)
        qT = sbt(st, "qT", [64, 2, 8, 128], BF16)
        iqT = sbt(st, "iqT", [64, 2, 4, 128], BF16)
        score = sbt(st, "score", [128, S], F32)
        rl = sbt(st, "rl", [128, 2, 4, 256], F32)
        selneg = sbt(st, "selneg", [128, 2, S], BF16)
        cj = sbt(st, "cj", [128, S], BF16)
        lo = sbt(st, "lo", [128, 1], F32)
        hi = sbt(st, "hi", [128, 1], F32)
        mid = sbt(st, "mid", [128, 1], F32)
        cnt = sbt(st, "cnt", [128, 1], F32)
        wtab = sbt(st, "wtab", [128, NROUNDS + 2], F32)
        pw2 = sbt(st, "pw2", [128, NROUNDS + 2], F32)
        pT = sbt(st, "pT", [128, 2, 2, 512], BF16)
        oT = sbt(st, "oT", [65, 2, 512], F32)
        rb = sbt(st, "rb", [64, 2, 512], F32)
        ybn = sbt(st, "ybn", [64, 8, 128], BF16)
        ybo = sbt(st, "ybo", [128, 4, 128], BF16)
        pX = pst(st, "pX", [128, 2, 512], F32)
        pL = pst(st, "pL", [128, 2, 2, 512], F32)
        pO = pst(st, "pO", [128, 2, 512], F32)

        load_w(wB[:], "wB", T["w_in"][:, COL["ak"]:COL["ak"] + 452], 8, 452)
        load_w(wC[:, :, 0:512], "wC", T["w_in"][:, COL["aq"]:COL["aq"] + 512], 8, 512)
        load_w(wC[:, :, 512:768], "wC", T["w_in"][:, COL["iq"]:COL["iq"] + 256], 8, 256)
        k.dma("sp", gI[:], T["idx_k_norm_g"].partition_broadcast(128), "c0", [], ["gI"])
        k.dma("sp", bI[:], T["idx_k_norm_b"].partition_broadcast(128), "c0", [], ["bI"])
        k.dma("sp", cmask[:], T["c_cmask"], "c0", [], ["cmask"])
        k.dma("sp", e65[:], T["c_e65"], "c0", [], ["e65"])
        k.dma("sp", self_f[:], T["c_sel"], "c0", [], ["sel_f"])
        k.cp("dve", sel_b[:], self_f[:], ["sel_f"], ["sel_b"])
        for h in range(8):
            k.cp("dve", i8[:, h, :], identb[:], ["identb"], ["i8"])
        with ExitStack() as st2:
            bstage = sbt(st2, "bstage", [128, 3, 8, 128], F32)
            k.dma("sp", bstage[:, 0], T["c_bd"], "c0", [], ["bstage"])
            k.dma("sp", bstage[:, 1], T["c_bp"], "c0", [], ["bstage"])
            k.dma("sp", bstage[:, 2], T["c_b31"], "c0", [], ["bstage"])
            for j in range(2):
                k.tt("dve", bstage[:, j], bstage[:, j], bstage[:, 2], ALU.subtract, ["bstage"], ["bstage"])
                k.tsc("dve", bnd[:, j], bstage[:, j], 8.0, None, ALU.mult, None, ["bstage"], ["bnd"])
            s.barrier()
        k.memset("dve", v_aug[:, :, 64:65], 1.0, ["v_aug"])
        for r_ in range(NROUNDS + 2):
            k.memset("dve", pw2[:, r_:r_ + 1], 2.0 ** (-r_), ["pw2"])

        stage = k.cfg.get("bc_stage", 9)
        for b in range(k.cfg.get("nblk_b", NBLK) if stage >= 1 else 0):
            tb = slice(b * 128, (b + 1) * 128)
            for c in range(8):
                k.mm(pX[:, 0, 0:452], xnT[:, c, tb], wB[:, c, :], c == 0, c == 7, [("xnT", b), "wB"], [("pX", 0)])
            bv = k.cfg.get("b_var", 9)
            k.cp("act", v_aug[:, b, 0:64], pX[:, 0, 64:128], [("pX", 0)], ["v_aug"])
            if bv >= 2:
                k.cp("act", ikf[:], pX[:, 0, 384:448], [("pX", 0)], ["ikf"])
                k.tsc("dve", iws[:, b, :], pX[:, 0, 448:452], 0.0625, None, ALU.mult, None, [("pX", 0)], ["iws"])
            if bv >= 3:
                k.gen("dve", lambda e: e.bn_stats(out=stats[:], in_=ikf[:]), ["ikf"], ["stats"])
                k.gen("dve", lambda e: e.bn_aggr(out=mv[:], in_=stats[:]), ["stats"], ["mv"])
            if bv >= 4:
                k.act(irs[:], mv[:, 1:2], AF.Ln, ["mv"], ["irs"], bias=EPS)
                k.act(irs[:], irs[:], AF.Exp, ["irs"], ["irs"], scale=-0.5)
                k.tsc("dve", ikf[:], ikf[:], mv[:, 0:1], None, ALU.subtract, None, ["ikf", "mv"], ["ikf"])
                k.tsc("dve", ikf[:], ikf[:], irs[:, 0:1], None, ALU.mult, None, ["ikf", "irs"], ["ikf"])
                k.tt("dve", ikf[:], ikf[:], gI[:], ALU.mult, ["ikf", "gI"], ["ikf"])
                k.tt("dve", ikf[:], ikf[:], bI[:], ALU.add, ["ikf", "bI"], ["ikf"])
            if bv >= 5:
                ptk = pX[0:64, 1, 0:128]
                k.tr(ptk, ikf[:], identf[:], ["ikf", "identf"], [("pX", 1)])
                k.cp("act", iknT[:, tb], ptk, [("pX", 1)], [("iknT", b)])
        for c4 in range(8 if stage >= 2 else 0):
            ts_ = slice(c4 * 512, (c4 + 1) * 512)
            for c in range(8):
                k.mm(pX[0:64, 0, :], wB[:, c, 0:64], xnT[:, c, ts_], c == 0, c == 7,
                     [("xnT", 4 * c4, 4 * c4 + 4), "wB"], [("pX", 0)])
            k.cp("act", kT[:, ts_], pX[0:64, 0, :], [("pX", 0)], [("kT", 4 * c4, 4 * c4 + 4)])

        def c123(i):
            p = i % 2
            tb = slice(i * 128, (i + 1) * 128)
            L = (i + 1) * 128
            pq = pX[0:64, :, :].rearrange("p a (h t) -> p (a h) t", t=128)
            for h in range(8):
                for c in range(8):
                    k.mm(pq[:, h, :], wC[:, c, h * 64:(h + 1) * 64], xnT[:, c, tb], c == 0, c == 7,
                         [("xnT", i), "wC"], [("pX", h // 4)])
            k.cp("act", qT[:, p], pq, [("pX", 0), ("pX", 1)], [("qT", p)])
            for h in range(4):
                for c in range(8):
                    k.mm(pq[:, h, :], wC[:, c, 512 + h * 64:512 + (h + 1) * 64], xnT[:, c, tb], c == 0, c == 7,
                         [("xnT", i), "wC"], [("pX", 0)])
            k.cp("act", iqT[:, p], pq[:, 0:4, :], [("pX", 0)], [("iqT", p)])
            ps4 = pX[:].rearrange("p a (j w) -> p (a j) w", w=256)
            nch = (L + 255) // 256
            for ch in range(nch):
                s0 = ch * 256
                wk = min(256, L - s0)
                rp = ch % 2
                for j in range(4):
                    k.mm(ps4[:, j, 0:wk], iqT[:, p, j, :], iknT[:, s0:s0 + wk], True, True,
                         [("iqT", p), ("iknT", s0 // 128, (s0 + wk) // 128)], [("pX", j // 2)])
                k.act(rl[:, rp, :, 0:wk], ps4[:, :, 0:wk], AF.Relu, [("pX", 0), ("pX", 1)], [("rl", rp)])
                k.tsc("dve", score[:, s0:s0 + wk], rl[:, rp, 0, 0:wk], iws[:, i, 0:1], None, ALU.mult, None,
                      [("rl", rp), "iws"], ["score"])
                for j in range(1, 4):
                    k.stt("dve", score[:, s0:s0 + wk], rl[:, rp, j, 0:wk], iws[:, i, j:j + 1], score[:, s0:s0 + wk],
                          ALU.mult, ALU.add, [("rl", rp), "iws", "score"], ["score"])
            k.tred("dve", hi[:], score[:, 0:L], ALU.max, ["score"], ["hi"])
            k.tred("dve", lo[:], score[:, 0:L], ALU.min, ["score"], ["lo"])
            k.tsc("dve", lo[:], lo[:], -1.0, None, ALU.add, None, ["lo"], ["lo"])
            k.tt("dve", score[:, L - 128:L], score[:, L - 128:L], cmask[:], ALU.add, ["score", "cmask"], ["score"])
            if L > 256:
                k.tsc("dve", mid[:], lo[:], hi[:, 0:1], 0.5, ALU.add, ALU.mult, ["lo", "hi"], ["mid"])
                k.tsc("dve", hi[:], hi[:], lo[:, 0:1], 0.5, ALU.subtract, ALU.mult, ["lo", "hi"], ["hi"])
                k.tsc("dve", wtab[:], pw2[:], hi[:, 0:1], None, ALU.mult, None, ["pw2", "hi"], ["wtab"])
                for r_ in range(NROUNDS):
                    k.tsc("dve", cj[:, 0:L], score[:, 0:L], mid[:, 0:1], None, ALU.is_gt, ALU.add,
                          ["score", "mid"], ["cj", "cnt"], accum=cnt[:])
                    k.tsc("dve", cnt[:], cnt[:], 255.5, wtab[:, r_:r_ + 1], ALU.is_gt, ALU.mult, ["cnt", "wtab"], ["cnt"])
                    k.stt("dve", mid[:], mid[:], wtab[:, r_ + 1:r_ + 2], cnt[:], ALU.subtract, ALU.add,
                          ["mid", "wtab", "cnt"], ["mid"])
                k.tsc("dve", lo[:], mid[:], wtab[:, NROUNDS:NROUNDS + 1], None, ALU.subtract, None, ["mid", "wtab"], ["lo"])
            k.tsc("dve", selneg[:, p, 0:L], score[:, 0:L], lo[:, 0:1], NEG, ALU.is_le, ALU.mult,
                  ["score", "lo"], [("selneg", p)])

        def c4(i):
            p = i % 2

            def qk(j):
                lb_ = j % 2
                sj = slice(j * 128, (j + 1) * 128)
                near = (j == i) or (j == i - 1)
                for half in range(2):
                    hs = slice(4 * half, 4 * half + 4)
                    k.mm(pL[:, lb_, half, :], kT[:, sj], qT[:, p, hs, :], True, False,
                         [("kT", j), ("qT", p)], [("pL", lb_)])
                    k.mm(pL[:, lb_, half, :], selneg[:, p, sj], i8[:, hs, :], False, not near,
                         [("selneg", p), "i8"], [("pL", lb_)])
                    if near:
                        k.mm(pL[:, lb_, half, :], identb[:], bnd[:, 0 if j == i else 1, hs, :], False, True,
                             ["identb", "bnd"], [("pL", lb_)])

            qk(0)
            for j in range(i + 1):
                lb_ = j % 2
                if j + 1 <= i:
                    qk(j + 1)
                k.act(pT[:, lb_, :, :], pL[:, lb_, :, :], AF.Exp, [("pL", lb_)], [("pT", lb_)], scale=0.125)
                for half in range(2):
                    k.mm(pO[0:65, half, :], v_aug[:, j, :], pT[:, lb_, half, :], j == 0, j == i,
                         ["v_aug", ("pT", lb_)], ["pO"])
            k.cp("act", oT[:], pO[0:65, :, :], ["pO"], ["oT"])
            for half in range(2):
                k.mm(pX[0:64, half, :], e65[:], oT[:, half, :], True, True,
                     ["e65", "oT"], [("pX", half)])
            k.act(rb[:], pX[0:64, :, :], AF.Ln, [("pX", 0), ("pX", 1)], ["rb"])
            k.act(rb[:], rb[:], AF.Exp, ["rb"], ["rb"], scale=-1.0)
            k.tt("dve", ybn[:].rearrange("p (a h) t -> p a (h t)", a=2), oT[0:64, :, :], rb[:], ALU.mult, ["oT", "rb"], ["ybn"])
            yv = ybn[:].rearrange("p (c two) t -> p two c t", two=2)
            k.mm(pX[:, 0, :], sel_b[:, 0:128], yv[:, 0], True, False, ["sel_b", "ybn"], [("pX", 0)])
            k.mm(pX[:, 0, :], sel_b[:, 128:256], yv[:, 1], False, True, ["sel_b", "ybn"], [("pX", 0)])
            k.cp("act", ybo[:], pX[:, 0, :], [("pX", 0)], ["ybo"])
            k.dma("sp", T["yb_s"][:, :, i * 128:(i + 1) * 128], ybo[:], "ybo", ["ybo"], [])

        nblk = k.cfg.get("nblk_c", NBLK)
        if stage >= 3:
            c123(0)
        for i in range(nblk if stage >= 3 else 0):
            if i + 1 < nblk:
                c123(i + 1)
            if stage >= 4:
                c4(i)
        s.barrier()


def phase_D(k, T, sbt, pst, xnT, identb, identf, load_w):
    nc, s = k.nc, k.s
    NG = k.cfg.get("ngroups_d", 8)
    with ExitStack() as st:
        wD = sbt(st, "wD", [128, 8, 2048], BF16)
        lbs = sbt(st, "lbs", [128, 2, 4], F32)
        lbT = sbt(st, "lbT", [128, 4], F32)
        omlT = sbt(st, "omlT", [128, 4], F32)
        gnT = sbt(st, "gnT", [128, 4], F32)
        tri = sbt(st, "tri", [64, 64], F32)
        cmk = sbt(st, "cmk", [128, 512], F32)
        ones_b = sbt(st, "ones_b", [128, 128], BF16)
        v_sb = sbt(st, "v_sb", [64, 2, 8, 512], BF16)
        state_f = sbt(st, "state_f", [128, 4, 128], F32)
        state_b = sbt(st, "state_b", [128, 4, 128], BF16)
        NT = 10
        tf = sbt(st, "tf", [128, 2, NT, 512], F32)
        qeT = sbt(st, "qeT", [128, 2, 512], BF16)
        keT = sbt(st, "keT", [128, 2, 512], BF16)
        k2T = sbt(st, "k2T", [128, 2, 512], F32)
        ebl = sbt(st, "ebl", [128, 2, 8], F32)
        at_sb = sbt(st, "at_sb", [64, 2, 64], BF16)
        k2_sb = sbt(st, "k2_sb", [64, 2, 128], BF16)
        sq = sbt(st, "sq", [128, 512], BF16)
        yo = sbt(st, "yo", [128, 2, 512], BF16)
        cvf = sbt(st, "cvf", [128, NCV, 1024], F32)
        cvb = sbt(st, "cvb", [128, 2, 1024], BF16)
        cstep = {"r": 0}
        pP = pst(st, "pP", [128, 3, 512], F32)
        pOo = pst(st, "pOo", [128, 2, 512], F32)
        pM = pst(st, "pM", [128, 2, 512], F32)
        pV = pst(st, "pV", [128, 512], F32)

        for nm, c0 in (("hq", 0), ("hf", 512), ("hi", 1024), ("hog", 1536)):
            load_w(wD[:, :, c0:c0 + 512], "wD", T["w_in"][:, COL[nm]:COL[nm] + 512], 8, 512)
        k.dma("sp", lbs[:], T["hg_lb"].rearrange("r (h p) -> p r h", p=128), "c0", [], ["lbs"])
        k.dma("sp", gnT[:], T["hg_norm"].rearrange("o (h p) -> p (o h)", p=128), "c0", [], ["gnT"])
        k.dma("sp", tri[:], T["c_tri"], "c0", [], ["tri"])
        k.dma("sp", cmk[:], T["c_chunkmask"], "c0", [], ["cmk"])
        k.memset("dve", ones_b[:], 1.0, ["ones_b"])
        k.memset("dve", state_f[:], 0.0, [("state_f", 0, 4)])
        k.memset("dve", state_b[:], 0.0, [("state_b", 0, 4)])
        k.tt("dve", lbT[:], lbs[:, 1, :], lbs[:, 0, :], ALU.subtract, ["lbs"], ["lbT"])
        k.act(lbT[:], lbT[:], AF.Exp, ["lbT"], ["lbT"])
        k.tsc("dve", lbT[:], lbT[:], 1.0, None, ALU.add, None, ["lbT"], ["lbT"])
        k.recip(lbT[:], lbT[:], ["lbT"], ["lbT"])
        k.tsc("dve", omlT[:], lbT[:], -1.0, 1.0, ALU.mult, ALU.add, ["lbT"], ["omlT"])

        def vproj(g):
            gp = g % 2
            xk = ("xnT", 4 * g, 4 * g + 4)
            for c in range(8):
                tok = slice(g * 512 + c * 64, g * 512 + (c + 1) * 64)
                for kc in range(8):
                    k.mm(pV[0:64, :], xnT[:, kc, tok], wD[:, kc, 1024:1536], kc == 0, kc == 7, [xk, "wD"], ["pV"])
                k.cp("act", v_sb[:, gp, c, :], pV[0:64, :], ["pV"], [("v_sb", gp)])

        def prologue(g, h):
            g5 = slice(g * 512, (g + 1) * 512)
            xk = ("xnT", 4 * g, 4 * g + 4)
            u = (g * 4 + h) % 2
            t = lambda i, u=u: tf[:, u, i, :]
            tk = lambda i, u=u: ("tf", u * NT + i)
            for j, c0 in enumerate((0, 512, 1536)):
                for kc in range(8):
                    k.mm(pP[:, j, :], wD[:, kc, c0 + h * 128:c0 + (h + 1) * 128], xnT[:, kc, g5], kc == 0, kc == 7,
                         [xk, "wD"], [("pP", j)])
                    yield
            k.act(t(0), pP[:, 1, :], AF.Exp, [("pP", 1)], [tk(0)], scale=-1.0)
            yield
            k.act(t(0), t(0), AF.Ln, [tk(0)], [tk(0)], bias=1.0)
            yield
            k.act(t(0), t(0), AF.Exp, [tk(0)], [tk(0)], scale=-1.0)
            yield
            k.tsc("dve", t(0), t(0), omlT[:, h:h + 1], lbT[:, h:h + 1], ALU.mult, ALU.add,
                  [tk(0), "omlT", "lbT"], [tk(0)])
            yield
            k.act(t(1), t(0), AF.Ln, [tk(0)], [tk(1)])
            yield
            k.tsc("dve", t(2), t(0), -1.0, 1.0, ALU.mult, ALU.add, [tk(0)], [tk(2)])
            yield
            k.gen("dve", lambda e, o=t(3), d0=cmk[:], d1=t(1): e.tensor_tensor_scan(
                out=o, data0=d0, data1=d1, initial=0.0, op0=ALU.mult, op1=ALU.add),
                ["cmk", tk(1)], [tk(3)])
            yield
            k.act(t(4), t(3), AF.Exp, [tk(3)], [tk(4)])
            yield
            k.act(t(5), t(3), AF.Exp, [tk(3)], [tk(5)], scale=-1.0)
            yield
            for c in range(8):
                cs = slice(c * 64, (c + 1) * 64)
                k.act(tf[:, u, 6, cs], tf[:, u, 3, cs], AF.Exp, [tk(3)], [tk(6)], scale=-1.0,
                      bias=tf[:, u, 3, c * 64 + 63:c * 64 + 64])
                yield
            bl = tf[:, u, 3, :].rearrange("p (c t) -> p c t", t=64)[:, :, 63]
            k.act(ebl[:, u, :], bl, AF.Exp, [tk(3)], [("ebl", u)])
            yield
            k.tt("dve", k2T[:, u, :], t(2), t(6), ALU.mult, [tk(2), tk(6)], [("k2T", u)])
            yield
            k.tt("dve", keT[:, u, :], t(2), t(5), ALU.mult, [tk(2), tk(5)], [("keT", u)])
            yield
            k.act(t(7), pP[:, 0, :], AF.Exp, [("pP", 0)], [tk(7)], scale=-1.0)
            yield
            k.act(t(7), t(7), AF.Ln, [tk(7)], [tk(7)], bias=1.0)
            yield
            k.act(t(7), t(7), AF.Exp, [tk(7)], [tk(7)], scale=-1.0)
            yield
            k.tt("dve", t(7), t(7), pP[:, 0, :], ALU.mult, [tk(7), ("pP", 0)], [tk(7)])
            yield
            k.tt("dve", qeT[:, u, :], t(7), t(4), ALU.mult, [tk(7), tk(4)], [("qeT", u)])
            yield
            k.act(t(8), pP[:, 2, :], AF.Exp, [("pP", 2)], [tk(8)], scale=-1.0)
            yield
            k.act(t(8), t(8), AF.Ln, [tk(8)], [tk(8)], bias=1.0)
            yield
            k.act(t(8), t(8), AF.Exp, [tk(8)], [tk(8)], scale=-1.0)
            yield
            k.tt("dve", t(8), t(8), pP[:, 2, :], ALU.mult, [tk(8), ("pP", 2)], [tk(8)])

        def chunks(g, h, pg=None, qg=None):
            def pull(n=1):
                if qg is not None:
                    next(qg, None)
                if pg is not None:
                    for _ in range(n):
                        next(pg, None)
            gp = g % 2
            u = (g * 4 + h) % 2
            hs = slice(h * 128, (h + 1) * 128)
            for c in range(8):
                cs = slice(c * 64, (c + 1) * 64)
                a2 = c % 2
                k.mm(pM[0:64, 0, 0:64], keT[:, u, cs], qeT[:, u, cs], True, True,
                     [("keT", u), ("qeT", u)], [("pM", 0)])
                k.tt("dve", at_sb[:, a2, :], pM[0:64, 0, 0:64], tri[:], ALU.mult, [("pM", 0), "tri"], [("at_sb", a2)])
                pull(2)
                k.mm(pOo[:, u, cs], state_b[:, h, :], qeT[:, u, cs], True, False,
                     [("state_b", h), ("qeT", u)], [("pOo", u)])
                k.mm(pOo[:, u, cs], v_sb[:, gp, c, hs], at_sb[:, a2, :], False, True,
                     [("v_sb", gp), ("at_sb", a2)], [("pOo", u)])
                k.tr(pM[0:64, 0, 128:256], k2T[:, u, cs], identf[:], [("k2T", u), "identf"], [("pM", 0)])
                k.cp("act", k2_sb[:, a2, :], pM[0:64, 0, 128:256], [("pM", 0)], [("k2_sb", a2)])
                pull(2)
                k.mm(pM[:, 1, 0:128], k2_sb[:, a2, :], v_sb[:, gp, c, hs], True, True,
                     [("k2_sb", a2), ("v_sb", gp)], [("pM", 1)])
                k.stt("dve", state_f[:, h, :], state_f[:, h, :], ebl[:, u, c:c + 1], pM[:, 1, 0:128],
                      ALU.mult, ALU.add, [("state_f", h), ("ebl", u), ("pM", 1)], [("state_f", h)])
                k.cp("act", state_b[:, h, :], state_f[:, h, :], [("state_f", h)], [("state_b", h)])
                pull(2)
                if k.cfg.get("convert", True) and NG == 8:
                    convert_step(k, T, cvf, cvb, cstep["r"])
                    cstep["r"] += 1

        def post(g, h):
            g5 = slice(g * 512, (g + 1) * 512)
            u = (g * 4 + h) % 2
            t = lambda i, u=u: tf[:, u, i, :]
            tk = lambda i, u=u: ("tf", u * NT + i)
            k.act(sq[:], pOo[:, u, :], AF.Square, [("pOo", u)], ["sq"])
            yield
            k.mm(pV[:], ones_b[:], sq[:], True, True, ["ones_b", "sq"], ["pV"])
            yield
            k.act(t(9), pV[:], AF.Ln, ["pV"], [tk(9)], scale=1.0 / 128, bias=EPS)
            yield
            k.act(t(9), t(9), AF.Exp, [tk(9)], [tk(9)], scale=-0.5)
            yield
            k.stt("dve", t(9), pOo[:, u, :], gnT[:, h:h + 1], t(9), ALU.mult, ALU.mult,
                  [("pOo", u), "gnT", tk(9)], [tk(9)])
            yield
            k.tt("dve", yo[:, u, :], t(9), t(8), ALU.mult, [tk(9), tk(8)], [("yo", u)])
            yield
            k.dma("sp", T["ya_s"][:, h, g5], yo[:, u, :], "yo%d" % u, [("yo", u)], [])

        units = [(g, h) for g in range(NG) for h in range(4)]
        vproj(0)
        for _ in prologue(*units[0]):
            pass
        qg = None
        for n, (g, h) in enumerate(units):
            pg = None
            if n + 1 < len(units):
                g1, h1 = units[n + 1]
                if h1 == 0:
                    vproj(g1)
                pg = prologue(g1, h1)
            chunks(g, h, pg, qg)
            if qg is not None:
                for _ in qg:
                    pass
            if pg is not None:
                for _ in pg:
                    pass
            qg = post(g, h)
        for _ in qg:
            pass
        if k.cfg.get("convert", True) and NG == 8:
            while cstep["r"] < 256 + NCV:
                convert_step(k, T, cvf, cvb, cstep["r"])
                cstep["r"] += 1
        s.barrier()


def phase_E1(k, T, sbt, pst, xnT, load_w):
    nc, s = k.nc, k.s
    NG = k.cfg.get("ngroups_e1", 8)
    with ExitStack() as st:
        wG = sbt(st, "wG", [128, 8, 2048], BF16)
        wU = sbt(st, "wU", [128, 2, 4, 1024], BF16)
        yab = sbt(st, "yab", [128, 2, 2, 4, 512], BF16)
        sg = sbt(st, "sg", [128, 2, 2, 512], F32)
        tmp = sbt(st, "e1tmp", [128, 2, 2, 512], F32)
        hTg = sbt(st, "hTg", [128, 2, 8, 512], BF16)
        pE = pst(st, "pE", [128, 2, 4, 512], F32)
        load_w(wG[:, :, 0:1024], "wG", T["w_in"][:, COL["ga"]:COL["ga"] + 1024], 8, 1024)
        load_w(wG[:, :, 1024:2048], "wG", T["w_in"][:, COL["gb"]:COL["gb"] + 1024], 8, 1024)
        load_w(wU[:, 0], "wU", T["w_up_a"], 4, 1024)
        load_w(wU[:, 1], "wU", T["w_up_b"], 4, 1024)
        for g in range(NG):
            gp = g % 2
            g5 = slice(g * 512, (g + 1) * 512)
            xk = ("xnT", 4 * g, 4 * g + 4)
            k.dma("sp", yab[:, gp, 0], T["ya_s"][:, :, g5], "yab%d" % gp, [], [("yab", gp)])
            k.dma("sp", yab[:, gp, 1], T["yb_s"][:, :, g5], "yab%d" % gp, [], [("yab", gp)])
            for nn in range(8):
                pb = nn % 2
                ns = slice(nn * 128, (nn + 1) * 128)
                for ab in range(2):
                    for kc in range(8):
                        k.mm(pE[:, pb, ab, :], wG[:, kc, ab * 1024 + nn * 128:ab * 1024 + (nn + 1) * 128], xnT[:, kc, g5],
                             kc == 0, kc == 7, [xk, "wG"], [("pE", pb * 4 + ab)])
                    for c4 in range(4):
                        k.mm(pE[:, pb, 2 + ab, :], wU[:, ab, c4, ns], yab[:, gp, ab, c4, :], c4 == 0, c4 == 3,
                             [("yab", gp), "wU"], [("pE", pb * 4 + 2 + ab)])
                for ab in range(2):
                    k.act(sg[:, pb, ab, :], pE[:, pb, ab, :], AF.Exp, [("pE", pb * 4 + ab)], [("sg", pb * 2 + ab)], scale=-1.0)
                    k.act(sg[:, pb, ab, :], sg[:, pb, ab, :], AF.Ln, [("sg", pb * 2 + ab)], [("sg", pb * 2 + ab)], bias=1.0)
                    k.act(sg[:, pb, ab, :], sg[:, pb, ab, :], AF.Exp, [("sg", pb * 2 + ab)], [("sg", pb * 2 + ab)], scale=-1.0)
                    k.tt("dve", tmp[:, pb, ab, :], sg[:, pb, ab, :], pE[:, pb, 2 + ab, :], ALU.mult,
                         [("sg", pb * 2 + ab), ("pE", pb * 4 + 2 + ab)], [("e1tmp", pb * 2 + ab)])
                k.tt("dve", hTg[:, gp, nn, :], tmp[:, pb, 0, :], tmp[:, pb, 1, :], ALU.add,
                     [("e1tmp", pb * 2), ("e1tmp", pb * 2 + 1)], [("hTg", gp)])
            k.dma("sp", T["hT_s"][:, :, g5], hTg[:, gp], "hTg%d" % gp, [("hTg", gp)], [])
        s.barrier()


def peer_routing_alloc(sbt, st, pfx="r"):
    B = {}
    B["vals"] = sbt(st, pfx + "vals", [128, 16, 16], F32)
    B["idxs"] = sbt(st, pfx + "idxs", [128, 16, 16], U32)
    B["idxf"] = sbt(st, pfx + "idxf", [128, 16, 16], F32)
    B["s2"] = sbt(st, pfx + "s2", [128, 128], F32)
    B["cand"] = sbt(st, pfx + "cand", [128, 8, 256], F32)
    B["cand2"] = sbt(st, pfx + "cand2", [128, 8, 256], F32)
    B["tops"] = sbt(st, pfx + "tops", [128, 8, 16], F32)
    B["pos"] = sbt(st, pfx + "pos", [128, 8, 16], U32)
    B["ipos"] = sbt(st, pfx + "ipos", [128, 8, 16], U32)
    B["jpos"] = sbt(st, pfx + "jpos", [128, 8, 16], U32)
    B["iposf"] = sbt(st, pfx + "iposf", [128, 8, 16], F32)
    B["jposf"] = sbt(st, pfx + "jposf", [128, 8, 16], F32)
    B["eq"] = sbt(st, pfx + "eq", [128, 8, 16, 16], F32)
    B["sel1"] = sbt(st, pfx + "sel1", [128, 8, 16], F32)
    B["sel2"] = sbt(st, pfx + "sel2", [128, 8, 16], F32)
    B["gsum"] = sbt(st, pfx + "gsum", [128, 8], F32)
    return B


def peer_routing(k, B, s_sb, s_key, iota16, eidx, eidx_key, gw, gw_key, pfx="r"):
    vals, idxs, idxf, s2, cand, cand2 = B["vals"], B["idxs"], B["idxf"], B["s2"], B["cand"], B["cand2"]
    tops, pos, ipos, jpos, iposf, jposf = B["tops"], B["pos"], B["ipos"], B["jpos"], B["iposf"], B["jposf"]
    eq, sel1, sel2, gsum = B["eq"], B["sel1"], B["sel2"], B["gsum"]
    K_ = pfx
    for l in range(16):
        k.gen("dve", lambda e, l=l: e.max(out=vals[:, l, 0:8], in_=s_sb[:, l, :]), [s_key], [K_ + "vals"])
        k.gen("dve", lambda e, l=l: e.max_index(out=idxs[:, l, 0:8], in_max=vals[:, l, 0:8], in_values=s_sb[:, l, :]),
              [s_key, K_ + "vals"], [K_ + "idxs"])
        k.gen("dve", lambda e, l=l: e.match_replace(out=s2[:], in_to_replace=vals[:, l, 0:8], in_values=s_sb[:, l, :],
                                                    imm_value=-1e30), [s_key, K_ + "vals"], [K_ + "s2"])
        k.gen("dve", lambda e, l=l: e.max(out=vals[:, l, 8:16], in_=s2[:]), [K_ + "s2"], [K_ + "vals"])
        k.gen("dve", lambda e, l=l: e.max_index(out=idxs[:, l, 8:16], in_max=vals[:, l, 8:16], in_values=s2[:]),
              [K_ + "s2", K_ + "vals"], [K_ + "idxs"])
        yield
    k.cp("dve", idxf[:], idxs[:], [K_ + "idxs"], [K_ + "idxf"])
    v4 = vals[:].rearrange("p (h two) i -> p h two i", two=2)
    x4 = idxf[:].rearrange("p (h two) i -> p h two i", two=2)
    c4 = cand[:].rearrange("p h (i j) -> p h i j", j=16)
    k.tt("dve", c4, v4[:, :, 0, :].unsqueeze(3).broadcast_to([128, 8, 16, 16]),
         v4[:, :, 1, :].unsqueeze(2).broadcast_to([128, 8, 16, 16]), ALU.add, [K_ + "vals"], [K_ + "cand"])
    for h in range(8):
        k.gen("dve", lambda e, h=h: e.max(out=tops[:, h, 0:8], in_=cand[:, h, :]), [K_ + "cand"], [K_ + "tops"])
        k.gen("dve", lambda e, h=h: e.max_index(out=pos[:, h, 0:8], in_max=tops[:, h, 0:8], in_values=cand[:, h, :]),
              [K_ + "cand", K_ + "tops"], [K_ + "pos"])
        k.gen("dve", lambda e, h=h: e.match_replace(out=cand2[:, h, :], in_to_replace=tops[:, h, 0:8],
                                                    in_values=cand[:, h, :], imm_value=-1e30),
              [K_ + "cand", K_ + "tops"], [K_ + "cand2"])
        k.gen("dve", lambda e, h=h: e.max(out=tops[:, h, 8:16], in_=cand2[:, h, :]), [K_ + "cand2"], [K_ + "tops"])
        k.gen("dve", lambda e, h=h: e.max_index(out=pos[:, h, 8:16], in_max=tops[:, h, 8:16], in_values=cand2[:, h, :]),
              [K_ + "cand2", K_ + "tops"], [K_ + "pos"])
        yield
    k.tsc("dve", ipos[:], pos[:], 4, None, ALU.logical_shift_right, None, [K_ + "pos"], [K_ + "ipos"])
    k.tsc("dve", jpos[:], pos[:], 15, None, ALU.bitwise_and, None, [K_ + "pos"], [K_ + "jpos"])
    k.cp("dve", iposf[:], ipos[:], [K_ + "ipos"], [K_ + "iposf"])
    k.cp("dve", jposf[:], jpos[:], [K_ + "jpos"], [K_ + "jposf"])
    io4 = iota16[:].unsqueeze(1).unsqueeze(1).broadcast_to([128, 8, 16, 16])
    for (pf_, xi, sel) in ((iposf, 0, sel1), (jposf, 1, sel2)):
        k.tt("dve", eq[:], io4, pf_[:].unsqueeze(3).broadcast_to([128, 8, 16, 16]), ALU.is_equal,
             ["iota16", K_ + "iposf", K_ + "jposf"], [K_ + "eq"])
        k.tt("dve", eq[:], eq[:], x4[:, :, xi, :].unsqueeze(2).broadcast_to([128, 8, 16, 16]), ALU.mult,
             [K_ + "eq", K_ + "idxf"], [K_ + "eq"])
        k.tred("dve", sel[:], eq[:], ALU.add, [K_ + "eq"], [K_ + "sel"])
    yield
    k.stt("dve", sel1[:], sel1[:], 128.0, sel2[:], ALU.mult, ALU.add, [K_ + "sel"], [K_ + "sel"])
    k.cp("dve", eidx.rearrange("p (h i) -> p h i", i=16), sel1[:], [K_ + "sel"], [eidx_key])
    k.tt("dve", gw, tops[:], tops[:, :, 0:1].broadcast_to([128, 8, 16]), ALU.subtract, [K_ + "tops"], [gw_key])
    k.act(gw, gw, AF.Exp, [gw_key], [gw_key])
    k.tred("dve", gsum[:], gw, ALU.add, [gw_key], [K_ + "gsum"])
    k.recip(gsum[:], gsum[:], [K_ + "gsum"], [K_ + "gsum"])
    k.tt("dve", gw, gw, gsum[:].unsqueeze(2).broadcast_to([128, 8, 16]), ALU.mult, [gw_key, K_ + "gsum"], [gw_key])


def phase_E2(k, T, sbt, pst, identb, identf, load_w):
    nc, s = k.nc, k.s
    NB_ = k.cfg.get("nblk_e2", NBLK)
    NU = 12
    GS = 4
    with ExitStack() as st:
        wO = sbt(st, "wO", [128, 8, 1024], BF16)
        wQ = sbt(st, "wQ", [128, 8, 2048], BF16)
        keysT = sbt(st, "keysT", [128, 16, 128], BF16)
        g2 = sbt(st, "g2", [128, 1024], F32)
        g3 = sbt(st, "g3", [128, 1024], F32)
        iota16 = sbt(st, "iota16", [128, 16], F32)
        load_w(wO[:], "wO", T["w_out"], 8, 1024)
        load_w(wQ[:], "wQ", T["peer_wq"], 8, 2048)
        k.dma("sp", g2[:], T["norm_ffn"].partition_broadcast(128), "c0", [], ["g2"])
        k.dma("sp", g3[:], T["norm_final"].partition_broadcast(128), "c0", [], ["g3"])
        k.dma("sp", iota16[:], T["c_iota16"], "c0", [], ["iota16"])
        with ExitStack() as st2:
            kst = sbt(st2, "kst", [128, 2, 128], F32)
            pK = pst(st2, "pK", [128, 128], F32)
            for l in range(16):
                h_, p_ = l // 2, l % 2
                kb = l % 2
                k.dma("sp", kst[:, kb, :], T["peer_keys"][p_ * 8 + h_], "kst%d" % kb, [], [("kst", kb)])
                k.tr(pK[:], kst[:, kb, :], identf[:], [("kst", kb), "identf"], ["pK"])
                k.cp("act", keysT[:, l, :], pK[:], ["pK"], ["keysT"])
            s.barrier()

        hTb = sbt(st, "hTb", [128, 2, 8, 128], BF16)
        xb = sbt(st, "xb", [128, 1, 2, 512], F32)
        x1 = sbt(st, "x1", [128, 2, 2, 512], F32)
        junk = sbt(st, "junk", [128, 1024], BF16)
        junkb = sbt(st, "junkb", [128, 1024], BF16)
        junkb2 = sbt(st, "junkb2", [128, 1024], BF16)
        prodb = sbt(st, "prodb", [128, 3, 1024], BF16)
        ssq = sbt(st, "ssq", [128, 4], F32)
        xn2f = sbt(st, "xn2f", [128, 1024], F32)
        xn2b = sbt(st, "xn2b", [128, 2, 1024], BF16)
        xn2T = sbt(st, "xn2T", [128, 8, 128], BF16)
        qTs = sbt(st, "qTs", [128, 8, 128], BF16)
        s_sb = sbt(st, "s_sb", [128, 16, 128], F32)
        eidx = sbt(st, "eidx", [128, 2, 128], I32)
        gw = sbt(st, "gw", [128, 2, 8, 16], F32)
        uvg = sbt(st, "uvg", [128, NU, 2048], BF16)
        dg = sbt(st, "dg", [128, 4, 128], BF16)
        hcol = sbt(st, "hcol", [128, 128], F32)
        acol = sbt(st, "acol", [128, 128], F32)
        ob = sbt(st, "ob", [128, 2, 512], F32)
        RB = peer_routing_alloc(sbt, st)
        pY = pst(st, "pY", [128, 2, 512], F32)
        pG = pst(st, "pG", [128, 2, 512], F32)
        pT2 = pst(st, "pT2", [128, 8, 128], BF16)
        pQ = pst(st, "pQ", [128, 8, 128], F32)

        def front(b):
            p = b % 2
            tb = slice(b * 128, (b + 1) * 128)
            k.dma("sp", hTb[:, p], T["hT_s"][:, :, tb], "hTb%d" % p, [], [("hTb", p)])
            k.dma("sp", xb[:, 0], T["x"][tb, :].rearrange("t (a n) -> t a n", a=2), "xb0", [], [("xb", 0)])
            for half in range(2):
                for c in range(8):
                    k.mm(pY[:, half, :], hTb[:, p, c, :], wO[:, c, half * 512:(half + 1) * 512], c == 0, c == 7,
                         [("hTb", p), "wO"], ["pY"])
                yield
            k.tt("dve", x1[:, p], xb[:, 0], pY[:], ALU.add, [("xb", 0), "pY"], [("x1", p)])
            x1f = x1[:, p].rearrange("p a n -> p (a n)")
            k.act(junk[:], x1f, AF.Square, [("x1", p)], ["junk", ("ssq", p)], accum=ssq[:, p:p + 1])
            k.act(ssq[:, p:p + 1], ssq[:, p:p + 1], AF.Ln, [("ssq", p)], [("ssq", p)], scale=1.0 / D, bias=EPS)
            k.act(ssq[:, p:p + 1], ssq[:, p:p + 1], AF.Exp, [("ssq", p)], [("ssq", p)], scale=-0.5)
            k.stt("dve", xn2f[:], x1f, ssq[:, p:p + 1], g2[:], ALU.mult, ALU.mult, [("x1", p), ("ssq", p), "g2"], ["xn2f"])
            k.cp("act", xn2b[:, p, :], xn2f[:], ["xn2f"], [("xn2b", p)])
            for c in range(8):
                k.tr(pT2[:, c, :], xn2b[:, p, c * 128:(c + 1) * 128], identb[:], [("xn2b", p), "identb"], ["pT2"])
            k.cp("act", xn2T[:], pT2[:], ["pT2"], ["xn2T"])
            yield
            for l0 in (0, 8):
                for l in range(8):
                    for kc in range(8):
                        k.mm(pQ[:, l, :], wQ[:, kc, (l0 + l) * 128:(l0 + l + 1) * 128], xn2T[:, kc, :], kc == 0, kc == 7,
                             ["xn2T", "wQ"], [("pQ", l // 4)])
                    yield
                for hb in range(2):
                    ls = slice(hb * 4, hb * 4 + 4)
                    k.cp("act", qTs[:, ls, :], pQ[:, ls, :], [("pQ", hb)], [("qTs", hb)])
                yield
                for l in range(8):
                    k.mm(pQ[:, l, :], qTs[:, l, :], keysT[:, l0 + l, :], True, True, [("qTs", l // 4), "keysT"], [("pQ", l // 4)])
                for hb in range(2):
                    ls = slice(hb * 4, hb * 4 + 4)
                    k.cp("act", s_sb[:, l0 + hb * 4:l0 + hb * 4 + 4, :], pQ[:, ls, :], [("pQ", hb)], ["s_sb"])
                yield
            yield from peer_routing(k, RB, s_sb, "s_sb", iota16, eidx[:, p, :], ("eidx", p), gw[:, p], ("gw", p))

        def gath(b, fg):
            p = b % 2
            tb = slice(b * 128, (b + 1) * 128)
            x1f = x1[:, p].rearrange("p a n -> p (a n)")
            gwf = gw[:, p].rearrange("p h i -> p (h i)")
            for kk in range(128):
                ub = kk % NU
                db = kk % 4
                hk = ("hcol", kk % 8)
                ak = ("acol", kk % 8)
                k.s.dma("pool", lambda e, kk=kk, ub=ub, p=p: e.indirect_dma_start(
                    out=uvg[:, ub, :], out_offset=None, in_=T["uv_s"][:, :],
                    in_offset=bass.IndirectOffsetOnAxis(ap=eidx[:, p, kk:kk + 1], axis=0)),
                    "uvg%d" % ub, [("eidx", p), "uv_s"], [("uvg", ub)])
                k.stt("dve", junkb[:], uvg[:, ub, 0:1024], 1.0, xn2b[:, p, :], ALU.mult, ALU.mult,
                      [("uvg", ub), ("xn2b", p)], ["junkb", hk], accum=hcol[:, kk:kk + 1])
                k.act(acol[:, kk:kk + 1], hcol[:, kk:kk + 1], AF.Gelu, [hk], [ak])
                k.act(acol[:, kk:kk + 1], acol[:, kk:kk + 1], AF.Copy, [ak, ("gw", p)], [ak], scale=gwf[:, kk:kk + 1])
                k.act(dg[:, db, :], identb[:], AF.Copy, ["identb", ak], [("dg", db)], scale=acol[:, kk:kk + 1])
                for half in range(2):
                    k.mm(pG[:, half, :], dg[:, db, :], uvg[:, ub, 1024 + half * 512:1024 + (half + 1) * 512],
                         kk == 0, kk == 127, [("dg", db), ("uvg", ub)], ["pG"])
                if fg is not None:
                    next(fg, None)
            if fg is not None:
                for _ in fg:
                    pass
            k.tt("dve", x1[:, p], x1[:, p], pG[:], ALU.add, [("x1", p), "pG"], [("x1", p)])
            k.act(junk[:], x1f, AF.Square, [("x1", p)], ["junk", ("ssq", 2)], accum=ssq[:, 2:3])
            k.act(ssq[:, 2:3], ssq[:, 2:3], AF.Ln, [("ssq", 2)], [("ssq", 2)], scale=1.0 / D, bias=EPS)
            k.act(ssq[:, 2:3], ssq[:, 2:3], AF.Exp, [("ssq", 2)], [("ssq", 2)], scale=-0.5)
            k.stt("dve", ob[:].rearrange("p a n -> p (a n)"), x1f, ssq[:, 2:3], g3[:], ALU.mult, ALU.mult,
                  [("x1", p), ("ssq", 2), "g3"], ["ob"])
            k.dma("sp", T["out"][tb, :].rearrange("t (a n) -> t a n", a=2), ob[:], "ob", ["ob"], [])

        for _ in front(0):
            pass
        for b in range(NB_):
            gath(b, front(b + 1) if b + 1 < NB_ else None)
        s.barrier()


_NC_CACHE = {}


def _core_inputs(inp, b, consts):
    d = {
        "x": np.ascontiguousarray(inp["x"][b], dtype=np.float32),
        "norm_mix": np.asarray(inp["norm_mix"], np.float32).reshape(1, D),
        "w_in": np.ascontiguousarray(np.asarray(inp["w_in"], np.float32)[0]),
        "hg_lb": np.asarray(inp["hg_lb"], np.float32).reshape(2, 512),
        "hg_norm": np.asarray(inp["hg_norm"], np.float32).reshape(1, 512),
        "idx_k_norm_g": np.asarray(inp["idx_k_norm_g"], np.float32).reshape(1, 64),
        "idx_k_norm_b": np.asarray(inp["idx_k_norm_b"], np.float32).reshape(1, 64),
        "w_up_a": np.ascontiguousarray(np.asarray(inp["w_up_a"], np.float32)[0]),
        "w_up_b": np.ascontiguousarray(np.asarray(inp["w_up_b"], np.float32)[0]),
        "w_out": np.ascontiguousarray(np.asarray(inp["w_out"], np.float32)[0]),
        "norm_ffn": np.asarray(inp["norm_ffn"], np.float32).reshape(1, D),
        "peer_wq": np.ascontiguousarray(np.asarray(inp["peer_wq"], np.float32)[0]),
        "peer_keys": np.ascontiguousarray(np.asarray(inp["peer_keys"], np.float32)[0]).reshape(16, 128, 128),
        "peer_u": np.ascontiguousarray(np.asarray(inp["peer_u"], np.float32)[0]),
        "peer_v": np.ascontiguousarray(np.asarray(inp["peer_v"], np.float32)[0]),
        "norm_final": np.asarray(inp["norm_final"], np.float32).reshape(1, D),
    }
    d.update(consts)
    return d


def kernel(**inputs):
    if "nc" not in _NC_CACHE:
        _NC_CACHE["nc"] = build()
    nc = _NC_CACHE["nc"]
    consts = host_consts(np.asarray(inputs["rel_bias"], np.float32))
    shared = _core_inputs(inputs, 0, consts)
    in_maps = []
    for b in range(NCORES):
        d = dict(shared)
        d["x"] = np.ascontiguousarray(np.asarray(inputs["x"])[b], dtype=np.float32)
        in_maps.append(d)
    res = run_bass_kernel_spmd(nc, in_maps, core_ids=list(range(NCORES)))
    out = np.stack([np.asarray(r["out"], dtype=np.float32) for r in res.results], axis=0)
    return out
```
